# Optimizing a Trainium2 kernel written in Bass

```python
import jax, jax.numpy as jnp
from jax import lax
import numpy as np

D_MODEL = 2048
BATCH = 1
SEQ = 8192
DEPTH = 2
DEC_BATCH = 8
DEC_SEQ = 2048
PAST_LEN = 128

GRID_W = 64
N_META = 16
ATTN_HEADS = 16
HEAD_DIM = 64
ATTN_WIDTH = ATTN_HEADS * HEAD_DIM
POOL_WIDTH = D_MODEL - ATTN_WIDTH
POOL_WINDOWS = (2, 4, 8, 16)
N_POOL_GROUPS = len(POOL_WINDOWS)
POOL_GROUP = POOL_WIDTH // N_POOL_GROUPS
IN_WIDTH = 3 * ATTN_WIDTH + POOL_WIDTH
NA_ROWS_MAX = 8
NA_COLS = 16
D_FF = 5632
N_EXPERTS = 8
TOP_K = 2
N_DENSE = (DEPTH + 1) // 2
N_MOE = DEPTH // 2
EPS = 1e-6

kernel_name = "hybrid_natten_pool_encoder"


def rmsnorm(x, g):
    xf = x.astype(jnp.float32)
    y = xf * lax.rsqrt(jnp.mean(xf * xf, axis=-1, keepdims=True) + EPS)
    return (y * g.astype(jnp.float32)).astype(x.dtype)


def swiglu(h, w_gate, w_up, w_down):
    return (jax.nn.silu(h @ w_gate) * (h @ w_up)) @ w_down


def neighbourhood_attention(q, k, v, rpb, meta_bias):
    B, L, H, Dh = q.shape
    T = L - N_META
    rows = T // GRID_W
    kh = min(NA_ROWS_MAX, rows)
    scale = Dh ** -0.5
    f32 = jnp.float32
    qm, km, vm = q[:, :N_META], k[:, :N_META], v[:, :N_META]
    qg = q[:, N_META:].reshape(B, rows, GRID_W, H, Dh)
    kg = k[:, N_META:].reshape(B, rows, GRID_W, H, Dh)
    vg = v[:, N_META:].reshape(B, rows, GRID_W, H, Dh)

    row_start = jnp.clip(jnp.arange(rows) - kh // 2, 0, rows - kh)
    col_pos = jnp.arange(GRID_W)
    col_start = jnp.clip(col_pos - NA_COLS // 2, 0, GRID_W - NA_COLS)
    col_idx = col_start[:, None] + jnp.arange(NA_COLS)
    col_off = col_idx - col_pos[:, None] + (NA_COLS - 1)
    rpb_cols = rpb.astype(f32)[:, :, col_off]
    meta_b = meta_bias.astype(f32)[:, None, :]

    def row_block(r):
        r0 = row_start[r]
        kb = lax.dynamic_slice_in_dim(kg, r0, kh, axis=1)
        vb = lax.dynamic_slice_in_dim(vg, r0, kh, axis=1)
        kw = kb[:, :, col_idx]
        vw = vb[:, :, col_idx]
        row_off = r0 + jnp.arange(kh) - r + (NA_ROWS_MAX - 1)
        bias = jnp.transpose(rpb_cols[:, row_off], (0, 2, 1, 3))
        qr = qg[:, r]
        s_loc = jnp.einsum('bqhd,bjqkhd->bhqjk', qr, kw).astype(f32) * scale + bias
        s_meta = jnp.einsum('bqhd,bmhd->bhqm', qr, km).astype(f32) * scale + meta_b
        s = jnp.concatenate([s_loc.reshape(B, H, GRID_W, kh * NA_COLS), s_meta], axis=-1)
        p = jax.nn.softmax(s, axis=-1).astype(v.dtype)
        p_loc = p[..., :kh * NA_COLS].reshape(B, H, GRID_W, kh, NA_COLS)
        p_meta = p[..., kh * NA_COLS:]
        return (jnp.einsum('bhqjk,bjqkhd->bqhd', p_loc, vw)
                + jnp.einsum('bhqm,bmhd->bqhd', p_meta, vm))

    o_grid = lax.map(row_block, jnp.arange(rows))
    o_grid = jnp.moveaxis(o_grid, 0, 1).reshape(B, T, H, Dh)
    s_mm = jnp.einsum('bqhd,bmhd->bhqm', qm, km).astype(f32) * scale + meta_b
    o_meta = jnp.einsum('bhqm,bmhd->bqhd', jax.nn.softmax(s_mm, axis=-1).astype(v.dtype), vm)
    return jnp.concatenate([o_meta, o_grid], axis=1)


def multiscale_pool(u, pool_w, pool_scale):
    B, L, C = u.shape
    f32 = jnp.float32
    uf = u.astype(f32).reshape(B, L, N_POOL_GROUPS, POOL_GROUP)
    cs = jnp.concatenate([jnp.zeros((B, 1, N_POOL_GROUPS, POOL_GROUP), f32),
                          jnp.cumsum(uf, axis=1)], axis=1)
    t = jnp.arange(L)
    means = []
    for g, w in enumerate(POOL_WINDOWS):
        lo = jnp.clip(t - w // 2, 0, L)
        hi = jnp.clip(t + w // 2, 0, L)
        cnt = (hi - lo).astype(f32)
        csg = cs[:, :, g]
        means.append((csg[:, hi] - csg[:, lo]) / cnt[None, :, None])
    pooled = (jnp.stack(means, axis=2) - uf).astype(u.dtype)
    mixed = jnp.einsum('blgc,gcd->blgd', pooled, pool_w)
    return mixed.reshape(B, L, C) * pool_scale


def token_mixer(h, w_in, rpb, meta_bias, pool_w, pool_scale, g_attn_out, g_pool_out, w_out):
    B, L, _ = h.shape
    z = h @ w_in
    q = z[..., :ATTN_WIDTH].reshape(B, L, ATTN_HEADS, HEAD_DIM)
    k = z[..., ATTN_WIDTH:2 * ATTN_WIDTH].reshape(B, L, ATTN_HEADS, HEAD_DIM)
    v = z[..., 2 * ATTN_WIDTH:3 * ATTN_WIDTH].reshape(B, L, ATTN_HEADS, HEAD_DIM)
    u = z[..., 3 * ATTN_WIDTH:]
    o_attn = neighbourhood_attention(q, k, v, rpb, meta_bias).reshape(B, L, ATTN_WIDTH)
    o_pool = multiscale_pool(u, pool_w, pool_scale)
    o = jnp.concatenate([rmsnorm(o_attn, g_attn_out), rmsnorm(o_pool, g_pool_out)], axis=-1)
    return o @ w_out


def moe_ffn(h, router, w_gate, w_up, w_down):
    B, L, D = h.shape
    hf = h.reshape(B * L, D)
    logits = (hf @ router).astype(jnp.float32)
    top_v, top_i = lax.top_k(logits, TOP_K)
    gates = jax.nn.softmax(top_v, axis=-1)
    comb = jnp.sum(jax.nn.one_hot(top_i, N_EXPERTS, dtype=jnp.float32) * gates[..., None], axis=1)
    comb = comb.astype(h.dtype)
    y = jnp.zeros_like(hf)
    for e in range(N_EXPERTS):
        y = y + comb[:, e:e + 1] * swiglu(hf, w_gate[e], w_up[e], w_down[e])
    return y.reshape(B, L, D)


def trunk(x, meta_tokens, ln_mix, w_in, rpb, meta_bias, pool_w, pool_scale, g_attn_out,
          g_pool_out, w_out, ln_ffn, ffn_w_gate, ffn_w_up, ffn_w_down, router, moe_w_gate,
          moe_w_up, moe_w_down, g_final):
    B = x.shape[0]
    meta = jnp.broadcast_to(meta_tokens[None].astype(x.dtype), (B, N_META, D_MODEL))
    h = jnp.concatenate([meta, x], axis=1)
    for layer in range(DEPTH):
        h = h + token_mixer(rmsnorm(h, ln_mix[layer]), w_in[layer], rpb[layer],
                            meta_bias[layer], pool_w[layer], pool_scale[layer],
                            g_attn_out[layer], g_pool_out[layer], w_out[layer])
        hn = rmsnorm(h, ln_ffn[layer])
        i = layer // 2
        if layer % 2 == 0:
            h = h + swiglu(hn, ffn_w_gate[i], ffn_w_up[i], ffn_w_down[i])
        else:
            h = h + moe_ffn(hn, router[i], moe_w_gate[i], moe_w_up[i], moe_w_down[i])
    return rmsnorm(h, g_final)[:, N_META:]


def setup_inputs(seed: int = 0) -> dict:
    key = jax.random.key(seed)
    ks = jax.random.split(key, 24)
    f32 = jnp.float32
    nrm = lambda k, shape, s: jax.random.normal(k, shape, f32) * s
    gain = lambda k, shape: 1.0 + 0.02 * jax.random.normal(k, shape, f32)
    return {
        "x_prompt": nrm(ks[0], (BATCH, SEQ, D_MODEL), 1.0),
        "x_sample": nrm(ks[1], (DEC_BATCH, DEC_SEQ, D_MODEL), 1.0),
        "meta_tokens": nrm(ks[2], (N_META, D_MODEL), 1.0),
        "ln_mix": gain(ks[3], (DEPTH, D_MODEL)),
        "w_in": nrm(ks[4], (DEPTH, D_MODEL, IN_WIDTH), D_MODEL ** -0.5),
        "rpb": nrm(ks[5], (DEPTH, ATTN_HEADS, 2 * NA_ROWS_MAX - 1, 2 * NA_COLS - 1), 0.1),
        "meta_bias": nrm(ks[6], (DEPTH, ATTN_HEADS, N_META), 0.1),
        "pool_w": nrm(ks[7], (DEPTH, N_POOL_GROUPS, POOL_GROUP, POOL_GROUP), POOL_GROUP ** -0.5),
        "pool_scale": gain(ks[8], (DEPTH, POOL_WIDTH)),
        "g_attn_out": gain(ks[9], (DEPTH, ATTN_WIDTH)),
        "g_pool_out": gain(ks[10], (DEPTH, POOL_WIDTH)),
        "w_out": nrm(ks[11], (DEPTH, D_MODEL, D_MODEL), D_MODEL ** -0.5),
        "ln_ffn": gain(ks[12], (DEPTH, D_MODEL)),
        "ffn_w_gate": nrm(ks[13], (N_DENSE, D_MODEL, D_FF), D_MODEL ** -0.5),
        "ffn_w_up": nrm(ks[14], (N_DENSE, D_MODEL, D_FF), D_MODEL ** -0.5),
        "ffn_w_down": nrm(ks[15], (N_DENSE, D_FF, D_MODEL), D_FF ** -0.5),
        "router": nrm(ks[16], (N_MOE, D_MODEL, N_EXPERTS), D_MODEL ** -0.5),
        "moe_w_gate": nrm(ks[17], (N_MOE, N_EXPERTS, D_MODEL, D_FF), D_MODEL ** -0.5),
        "moe_w_up": nrm(ks[18], (N_MOE, N_EXPERTS, D_MODEL, D_FF), D_MODEL ** -0.5),
        "moe_w_down": nrm(ks[19], (N_MOE, N_EXPERTS, D_FF, D_MODEL), D_FF ** -0.5),
        "g_final": gain(ks[20], (D_MODEL,)),
    }


def reference(x_prompt, x_sample, meta_tokens, ln_mix, w_in, rpb, meta_bias, pool_w, pool_scale,
              g_attn_out, g_pool_out, w_out, ln_ffn, ffn_w_gate, ffn_w_up, ffn_w_down, router,
              moe_w_gate, moe_w_up, moe_w_down, g_final):
    y_prompt = trunk(x_prompt, meta_tokens, ln_mix, w_in, rpb, meta_bias, pool_w, pool_scale,
                     g_attn_out, g_pool_out, w_out, ln_ffn, ffn_w_gate, ffn_w_up, ffn_w_down,
                     router, moe_w_gate, moe_w_up, moe_w_down, g_final)
    y_sample = trunk(x_sample, meta_tokens, ln_mix, w_in, rpb, meta_bias, pool_w, pool_scale,
                     g_attn_out, g_pool_out, w_out, ln_ffn, ffn_w_gate, ffn_w_up, ffn_w_down,
                     router, moe_w_gate, moe_w_up, moe_w_down, g_final)
    return (y_prompt, y_sample)
```

```python
import numpy as np
import concourse.bass as bass
import concourse.mybir as mybir
from concourse.bass_utils import run_bass_kernel_spmd

F32, BF16 = mybir.dt.float32, mybir.dt.bfloat16
AF = mybir.ActivationFunctionType
ALU = mybir.AluOpType

D = 2048
DFF = 5632
NE = 8
NR = 66
NSLOT = NR * 64
NEG = -30000.0
EPS = 1e-6
ROW_PM = 33
ROW_P0 = 35
DBG = {"stop": None, "dump": []}

L0_ALL = list(range(66))
L0_OUT = list(range(0, 34)) + list(range(39, 62)) + [65]
L1_KV = L0_OUT
L1_OUT = list(range(1, 33)) + list(range(43, 59))


def groups_of(rows, g=16):
    return [rows[i:i + g] for i in range(0, len(rows), g)]


def runs_of(rows):
    out = []
    for j, r in enumerate(rows):
        if out and out[-1][0] + out[-1][1] == r:
            out[-1][1] += 1
        else:
            out.append([r, 1, j])
    return [tuple(x) for x in out]


def att_plan():
    plan = []
    col = 2
    for layer in range(2):
        rows = []
        if layer == 0:
            rows += [0]
        rows += list(range(1, 33))
        if layer == 0:
            rows += [ROW_PM]
        prange = range(4, 27) if layer == 0 else range(8, 24)
        rows += [ROW_P0 + i for i in prange]
        for row in rows:
            if row == 0 or row == ROW_PM:
                plan.append((layer, row, 0, 0, row, col))
                continue
            if row <= 32:
                g = row - 1
                r0 = min(max(g - 4, 0), 24)
                plan.append((layer, row, 1 + r0, 4, 0, col))
                col += 4
                continue
            i = row - ROW_P0
            st, nb = i - 4, 4
            if i in (8, 9):
                nb = 6
            elif i in (10, 11):
                nb = 5
            elif i in (21, 22):
                st, nb = 16, 5
            elif i == 23:
                st, nb = 16, 6
            plan.append((layer, row, ROW_P0 + st, nb, ROW_PM, col))
            col += nb
    return plan, col


PLAN, NMASK = att_plan()


def host_tables(c):
    rowmask = np.zeros((128, NMASK), np.float32)
    rowmask[16:, 1] = NEG
    for (layer, row, st, nb, mrow, col) in PLAN:
        if row < ROW_P0:
            continue
        i = row - ROW_P0
        r = 16 * c - 8 + i
        for b in range(nb):
            for kh in range(2):
                j = st - ROW_P0 + 2 * b + kh
                rk = 16 * c - 8 + j
                if r < 0 or r > 127:
                    ok = not (c == 0 and r == -1)
                else:
                    r0 = min(max(r - 4, 0), 120)
                    ok = (r0 <= rk <= r0 + 7)
                if not ok:
                    rowmask[kh * 64:(kh + 1) * 64, col + b] = NEG
    valid = np.zeros((NSLOT,), np.float32)
    pos = np.zeros((NSLOT,), np.int64)
    slen = np.ones((NSLOT,), np.int64)
    sl = np.arange(64)
    valid[48:64] = 1; pos[48:64] = np.arange(16); slen[0:64] = 2064
    for k in range(1, 33):
        valid[k * 64:(k + 1) * 64] = 1
        pos[k * 64:(k + 1) * 64] = 16 + (k - 1) * 64 + sl
        slen[k * 64:(k + 1) * 64] = 2064
    LP = 16 + 8192
    b0 = ROW_PM * 64
    valid[b0 + 48:b0 + 64] = 1; pos[b0 + 48:b0 + 64] = np.arange(16); slen[b0:b0 + 64] = LP
    b0 = 34 * 64
    valid[b0:b0 + 64] = 1; pos[b0:b0 + 64] = 16 + sl; slen[b0:b0 + 64] = LP
    for i in range(30):
        r = 16 * c - 8 + i
        b0 = (ROW_P0 + i) * 64
        slen[b0:b0 + 64] = LP
        if 0 <= r < 128:
            valid[b0:b0 + 64] = 1
            pos[b0:b0 + 64] = 16 + r * 64 + sl
        elif r == -1:
            valid[b0 + 48:b0 + 64] = 1
            pos[b0 + 48:b0 + 64] = np.arange(16)
    invcnt = np.zeros((4, NSLOT), np.float32)
    for g, w in enumerate((2, 4, 8, 16)):
        lo = np.maximum(pos - w // 2, 0)
        hi = np.minimum(pos + w // 2, slen)
        cnt = np.where(valid > 0, hi - lo, w)
        invcnt[g] = 1.0 / cnt.astype(np.float32)
    colmask = np.zeros((128, 64), np.float32)
    for qc in range(64):
        cs = min(max(qc - 8, 0), 48)
        for kc in range(64):
            if not (cs <= kc < cs + 16):
                colmask[kc, qc] = NEG
                colmask[64 + kc, qc] = NEG
    jj = np.zeros((128, 64), np.float32)
    for q in range(64):
        jj[q, 63 - q] = 1.0
        jj[64 + q, 63 - q] = 1.0
    return dict(rowmask=rowmask, valid=valid.reshape(1, NSLOT), invcnt=invcnt, colmask=colmask, jj=jj,
                ident=np.eye(128, dtype=np.float32))


class Buf:
    __slots__ = ("w", "r")

    def __init__(self):
        self.w = None
        self.r = {}


class Q:
    def __init__(self, name, sems, is_dma):
        self.name, self.sems, self.is_dma = name, sems, is_dma
        self.cnt = [0] * len(sems)
        self.i = 0
        self.ops = []
        self.known = {}


class Prog:
    def __init__(self, nc, es):
        self.nc = nc
        self.q = {}
        for name, n, dma in (("sp", 8, True), ("pool", 8, True), ("act", 1, False), ("dve", 1, False), ("pe", 1, False)):
            sems = [es.enter_context(nc.semaphore(f"s_{name}{k}")) for k in range(n)]
            self.q[name] = Q(name, sems, dma)
        self.dbufs = {}
        self.allbufs = []

    def buf(self, persistent=False):
        b = Buf()
        if not persistent:
            self.allbufs.append(b)
        return b

    def D(self, name, key=0):
        k = (name, key)
        b = self.dbufs.get(k)
        if b is None:
            b = self.dbufs[k] = self.buf()
        return b

    def Ds(self, name, keys):
        return [self.D(name, k) for k in keys]

    def op(self, qn, fn, reads=(), writes=()):
        q = self.q[qn]
        waits = {}

        def need(ev):
            if ev is None:
                return
            sem, val = ev
            if qn == "pe" and sem is q.sems[0]:
                return
            k = id(sem)
            if k not in waits or waits[k][1] < val:
                waits[k] = (sem, val)

        for b in reads:
            need(b.w)
        for b in writes:
            need(b.w)
            for ev in b.r.values():
                need(ev)
        if q.is_dma:
            k = q.i % len(q.sems)
            q.i += 1
            sem = q.sems[k]
            if q.cnt[k] > 0:
                need((sem, q.cnt[k]))
            q.cnt[k] += 16
            ev = (sem, q.cnt[k])
            inc = 16
        else:
            sem = q.sems[0]
            q.cnt[0] += 1
            ev = (sem, q.cnt[0])
            inc = 1
        wl = []
        for k2, (s_, v) in waits.items():
            if q.known.get(k2, 0) >= v:
                continue
            q.known[k2] = v
            wl.append((s_, v))
        q.ops.append((fn, wl, sem, inc))
        for b in reads:
            k3 = id(sem)
            if k3 not in b.r or b.r[k3][1] < ev[1]:
                b.r[k3] = ev
        for b in writes:
            b.w = ev
            b.r = {}
        return ev

    def barrier(self):
        evs = []
        for q in self.q.values():
            if q.name == "pool":
                continue
            for k, s_ in enumerate(q.sems):
                if q.cnt[k] > 0:
                    evs.append((s_, q.cnt[k]))
        for q in self.q.values():
            if q.name == "pool":
                continue
            for (s_, v) in evs:
                if q.known.get(id(s_), 0) < v:
                    q.known[id(s_)] = v
                    q.ops.append((None, [(s_, v)], None, 0))
        for b in self.allbufs:
            b.w = None
            b.r = {}

    def emit(self, qn, e):
        q = self.q[qn]
        for fn, wl, sem, inc in q.ops:
            for s_, v in wl:
                e.wait_ge(s_, v)
            if fn is not None:
                fn(e).then_inc(sem, inc)
        if qn == "sp":
            for k, s_ in enumerate(q.sems):
                if q.cnt[k] > 0:
                    e.wait_ge(s_, q.cnt[k])


class Arena:
    def __init__(self, ap_u8, nbytes):
        self.ap, self.n = ap_u8, nbytes
        self.base = 0
        self.off = 0

    def persist(self):
        self.base = self.off

    def reset(self):
        self.off = self.base

    def alloc(self, shape, dt):
        esz = 4 if dt == F32 else 2
        n = int(np.prod(shape[1:]))
        nb = (n * esz + 63) // 64 * 64
        assert self.off + nb <= self.n, f"SBUF arena overflow {self.off}+{nb}>{self.n}"
        a = self.ap[0:shape[0], self.off:self.off + nb].bitcast(dt)[:, 0:n]
        self.off += nb
        if len(shape) == 3:
            a = a.rearrange("p (a b) -> p a b", b=shape[2])
        elif len(shape) == 4:
            a = a.rearrange("p (a b c) -> p a b c", b=shape[2], c=shape[3])
        return a


def dap(t, off, dims):
    return bass.AP(tensor=t.tensor, offset=off, ap=[[int(s), int(n)] for s, n in dims])


def build():
    from contextlib import ExitStack
    nc = bass.Bass("TRN2", target_bir_lowering=False)
    es = ExitStack()
    es.enter_context(nc.allow_low_precision("bf16 matmul operands, fp32 accumulate"))
    es.enter_context(nc.allow_non_contiguous_dma(reason="small strided param loads"))

    def din(name, shape, dt=F32):
        return nc.dram_tensor(name, list(shape), dt, kind="ExternalInput").ap()

    def dscr(name, shape, dt):
        kind = "ExternalOutput" if name in DBG["dump"] else "Internal"
        return nc.dram_tensor(name, list(shape), dt, kind=kind).ap()

    xs = din("xs", [NSLOT, D])
    wsrc = {
        "w_in": din("w_in", [2 * D * 4096 // 2048, 2048]),
        "w_out": din("w_out", [2 * D, 2048]),
        "pool_w": din("pool_w", [2 * 4 * 256 * 256 // 2048, 2048]),
        "ffn_g": din("ffn_g", [DFF, 2048]), "ffn_u": din("ffn_u", [DFF, 2048]), "ffn_d": din("ffn_d", [DFF, 2048]),
        "moe_g": din("moe_g", [NE * DFF, 2048]), "moe_u": din("moe_u", [NE * DFF, 2048]),
        "moe_d": din("moe_d", [NE * DFF, 2048]),
    }
    rpb = din("rpb", [2, 16 * 15 * 31])
    mbT = din("mbT", [2, 16, 16])
    ln_mix = din("ln_mix", [2, D]); ln_ffn = din("ln_ffn", [2, D])
    g_attn = din("g_attn", [2, 1024]); g_pool = din("g_pool", [2, 1024]); pscale = din("pscale", [2, 1024])
    g_final = din("g_final", [1, D])
    routerT = din("routerT", [NE, D])
    t_rowmask = din("rowmask", [128, NMASK]); t_valid = din("valid", [1, NSLOT]); t_invcnt = din("invcnt", [4, NSLOT])
    t_colmask = din("colmask", [128, 64]); t_jj = din("jj", [128, 64]); t_ident = din("ident", [128, 128])
    ys = nc.dram_tensor("ys", [2048, D], F32, kind="ExternalOutput").ap()
    yp = nc.dram_tensor("yp", [1024, D], F32, kind="ExternalOutput").ap()

    wb = {k: dscr(k + "_b", list(v.shape), BF16) for k, v in wsrc.items()}
    H = dscr("H", [NSLOT, D], F32)
    HNT = dscr("HNT", [D, NSLOT], BF16)
    QKT = dscr("QKT", [D, NSLOT], BF16)
    VA = dscr("VA", [NSLOT, 1040], BF16)
    UT = dscr("UT", [1024, NSLOT], F32)
    PLT = dscr("PLT", [1024, NSLOT], BF16)
    OA = dscr("OA", [NSLOT, 1024], F32)
    OT = dscr("OT", [D, NSLOT], BF16)
    ACTT = dscr("ACTT", [DFF, NSLOT], BF16)
    RPAD = dscr("RPAD", [1, 64 + 7440 + 128], F32)

    ARENA_BYTES = 188 * 1024
    arena_t = es.enter_context(nc.sbuf_tensor("arena", [128, ARENA_BYTES], mybir.dt.uint8))
    A = Arena(arena_t[:, :], ARENA_BYTES)
    psum = [es.enter_context(nc.psum_tensor(f"ps{i}", [128, 512], F32)) for i in range(8)]
    P = Prog(nc, es)
    PSB = [P.buf() for _ in range(8)]

    class T:
        def __init__(self, shape, dt):
            self.ap = A.alloc(shape, dt)
            self.b = P.buf()

    def dma(out, in_, reads, writes, qn="sp"):
        return P.op(qn, lambda e: e.dma_start(out=out, in_=in_), reads, writes)

    ident_f = T([128, 128], F32); ident = T([128, 128], BF16)
    jj_f = T([128, 64], F32); jj = T([128, 64], BF16)
    colmask = T([128, 64], F32)
    rowmask = T([128, NMASK], F32)
    COMB = T([128, 24, 8], F32)
    zero_c = T([128, 1], F32)
    dma(ident_f.ap, t_ident, [], [ident_f.b]); dma(jj_f.ap, t_jj, [], [jj_f.b])
    dma(colmask.ap, t_colmask, [], [colmask.b]); dma(rowmask.ap, t_rowmask, [], [rowmask.b])
    P.op("dve", lambda e: e.tensor_copy(out=ident.ap, in_=ident_f.ap), [ident_f.b], [ident.b])
    P.op("dve", lambda e: e.tensor_copy(out=jj.ap, in_=jj_f.ap), [jj_f.b], [jj.b])
    P.op("dve", lambda e: e.memset(zero_c.ap, 0.0), [], [zero_c.b])
    eps_c = T([128, 1], F32)
    P.op("dve", lambda e: e.memset(eps_c.ap, EPS), [], [eps_c.b])
    A.persist()

    WBUFS = {}

    def cast_rows(name, r0, r1):
        CH = 2048
        lst = WBUFS.setdefault((name, r0, r1), [])
        for a in range(r0, r1, CH):
            b_ = min(a + CH, r1)
            bb = P.buf(persistent=True)
            lst.append(bb)
            dma(wb[name][a:b_, :], wsrc[name][a:b_, :], [], [bb], qn="pool")

    def wdep(name, r0, r1):
        return WBUFS[(name, r0, r1)]

    WIN_R = D * 4096 // 2048
    cast_rows("w_in", 0, WIN_R); cast_rows("pool_w", 0, 128); cast_rows("w_out", 0, D)
    cast_rows("ffn_g", 0, DFF); cast_rows("ffn_u", 0, DFF); cast_rows("ffn_d", 0, DFF)
    cast_rows("w_in", WIN_R, 2 * WIN_R); cast_rows("pool_w", 128, 256); cast_rows("w_out", D, 2 * D)
    for e_ in range(NE):
        for nm in ("moe_g", "moe_u", "moe_d"):
            cast_rows(nm, e_ * DFF, (e_ + 1) * DFF)

    bank_rr = [0]

    def next_bank():
        i = bank_rr[0] % 8
        bank_rr[0] += 1
        return i

    def stop_here(tag):
        return DBG["stop"] == tag

    def phase_nt(rows, src, src_name, gain_ap, router=False):
        A.reset()
        gt = T([128, D], F32)
        dma(gt.ap, gain_ap.to_broadcast([128, D]), [], [gt.b])
        hb = [T([128, D], F32) for _ in range(2)]
        hnb = [T([128, D], BF16) for _ in range(2)]
        xt = [T([128, 16, 128], BF16) for _ in range(2)]
        junk = T([128, D], BF16)
        ss = [T([128, 1], F32) for _ in range(2)]
        rs = [T([128, 1], F32) for _ in range(2)]
        if router:
            rt = T([128, NE, D], F32)
            dma(rt.ap, dap(routerT, 0, [[0, 128], [D, NE], [1, D]]), [], [rt.b])
            hnf = T([128, D], F32)
            junk2 = T([128, D], F32)
            lg = T([128, 8], F32); m8 = T([128, 8], F32); nv1 = T([128, 1], F32)
            ex = T([128, 8], F32); mk = T([128, 8], F32); num = T([128, 8], F32); den = T([128, 1], F32)
        tiles = [(rows[i], rows[i + 1]) for i in range(0, len(rows), 2)]
        def ld(ti):
            ra, rb = tiles[ti]
            h_ = hb[ti % 2]
            dma(h_.ap[0:64, :], src[ra * 64:(ra + 1) * 64, :], [P.D(src_name, ra)], [h_.b])
            dma(h_.ap[64:128, :], src[rb * 64:(rb + 1) * 64, :], [P.D(src_name, rb)], [h_.b])

        ld(0)
        for ti, (ra, rb) in enumerate(tiles):
            p = ti % 2
            h_, hn_, xt_, ss_, rs_ = hb[p], hnb[p], xt[p], ss[p], rs[p]
            if ti + 1 < len(tiles):
                ld(ti + 1)
            P.op("act", lambda e, h_=h_, ss_=ss_: e.activation(out=junk.ap, in_=h_.ap, func=AF.Square, accum_out=ss_.ap),
                 [h_.b], [junk.b, ss_.b])
            P.op("act", lambda e, ss_=ss_, rs_=rs_: e.activation(out=rs_.ap, in_=ss_.ap, func=AF.Sqrt, bias=eps_c.ap,
                                                                 scale=1.0 / D), [ss_.b, eps_c.b], [rs_.b])
            P.op("dve", lambda e, rs_=rs_: e.reciprocal(out=rs_.ap, in_=rs_.ap), [rs_.b], [rs_.b])
            if not router:
                P.op("dve", lambda e, h_=h_, rs_=rs_, hn_=hn_: e.scalar_tensor_tensor(
                    out=hn_.ap, in0=h_.ap, scalar=rs_.ap, in1=gt.ap, op0=ALU.mult, op1=ALU.mult),
                    [h_.b, rs_.b, gt.b], [hn_.b])
            else:
                P.op("dve", lambda e, h_=h_, rs_=rs_: e.scalar_tensor_tensor(
                    out=hnf.ap, in0=h_.ap, scalar=rs_.ap, in1=gt.ap, op0=ALU.mult, op1=ALU.mult),
                    [h_.b, rs_.b, gt.b], [hnf.b])
                P.op("act", lambda e, hn_=hn_: e.activation(out=hn_.ap, in_=hnf.ap, func=AF.Copy), [hnf.b], [hn_.b])
                for ex_ in range(NE):
                    P.op("dve", lambda e, ex_=ex_: e.scalar_tensor_tensor(
                        out=junk2.ap, in0=hnf.ap, scalar=1.0, in1=rt.ap[:, ex_, :], op0=ALU.mult, op1=ALU.mult,
                        accum_out=lg.ap[:, ex_:ex_ + 1]), [hnf.b, rt.b], [junk2.b, lg.b])
                P.op("dve", lambda e: e.max(out=m8.ap, in_=lg.ap), [lg.b], [m8.b])
                P.op("dve", lambda e: e.tensor_scalar(out=nv1.ap, in0=m8.ap[:, 0:1], scalar1=-1.0, scalar2=None, op0=ALU.mult),
                     [m8.b], [nv1.b])
                P.op("act", lambda e: e.activation(out=ex.ap, in_=lg.ap, func=AF.Exp, bias=nv1.ap, scale=1.0),
                     [lg.b, nv1.b], [ex.b])
                P.op("dve", lambda e: e.tensor_scalar(out=mk.ap, in0=lg.ap, scalar1=m8.ap[:, 1:2], scalar2=None, op0=ALU.is_ge),
                     [lg.b, m8.b], [mk.b])
                P.op("dve", lambda e: e.scalar_tensor_tensor(out=num.ap, in0=ex.ap, scalar=1.0, in1=mk.ap, op0=ALU.mult,
                                                             op1=ALU.mult, accum_out=den.ap), [ex.b, mk.b], [num.b, den.b])
                P.op("dve", lambda e: e.reciprocal(out=den.ap, in_=den.ap), [den.b], [den.b])
                P.op("dve", lambda e, ti=ti: e.tensor_scalar(out=COMB.ap[:, ti, :], in0=num.ap, scalar1=den.ap, scalar2=None,
                                                             op0=ALU.mult), [num.b, den.b], [COMB.b])
            bks = [next_bank(), next_bank()]
            for kc in range(16):
                bk = bks[kc // 8]
                P.op("pe", lambda e, hn_=hn_, kc=kc, bk=bk: e.transpose(
                    out=psum[bk][:, :].bitcast(BF16)[:, (kc % 8) * 128:(kc % 8 + 1) * 128],
                    in_=hn_.ap[:, kc * 128:(kc + 1) * 128], identity=ident.ap), [hn_.b, ident.b], [PSB[bk]])
            P.op("act", lambda e, xt_=xt_, bk=bks[0]: e.activation(
                out=xt_.ap[:, 0:8, :], in_=psum[bk][:, :].bitcast(BF16).rearrange("p (a b) -> p a b", b=128), func=AF.Copy),
                [PSB[bks[0]]], [xt_.b])
            P.op("dve", lambda e, xt_=xt_, bk=bks[1]: e.tensor_copy(
                out=xt_.ap[:, 8:16, :], in_=psum[bk][:, :].bitcast(BF16).rearrange("p (a b) -> p a b", b=128)),
                [PSB[bks[1]]], [xt_.b])
            for j, r in enumerate((ra, rb)):
                dma(dap(HNT, r * 64, [[NSLOT, 128], [128 * NSLOT, 16], [1, 64]]), xt_.ap[:, :, j * 64:(j + 1) * 64],
                    [xt_.b], [P.D("HNT", r)])
        P.barrier()

    def load_xg(xg, xt_ap, f0, KC, grows):
        for (r0, n, j) in runs_of(grows):
            dma(xg.ap[:, :, j * 64:(j + n) * 64],
                dap(xt_ap, f0 * NSLOT + r0 * 64, [[NSLOT, 128], [128 * NSLOT, KC], [1, 64 * n]]),
                P.Ds(xt_ap.tensor.name, range(r0, r0 + n)), [xg.b])

    def lin_b(groups, xt_ap, KC, wspecs, nfeat, epi, FB=512):
        A.reset()
        TMAX = max(len(g) for g in groups) * 64
        nxg = 2 if KC * TMAX * 2 * 2 <= 70 * 1024 else 1
        xg = [T([128, KC, TMAX], BF16) for _ in range(nxg)]
        wbk = [[T([128, KC, FB], BF16) for _ in range(2)] for _ in wspecs]
        st = epi("alloc", None)
        tasks = [(gi, fb) for gi in range(len(groups)) for fb in range(nfeat // FB)]

        def loadx(ti):
            gi, fb = tasks[ti]
            if fb == 0:
                load_xg(xg[gi % nxg], xt_ap, 0, KC, groups[gi])

        def loads(ti):
            gi, fb = tasks[ti]
            for si, (wname, dep, base, ldw, col0) in enumerate(wspecs):
                w_ = wbk[si][ti % 2]
                dma(w_.ap, dap(wb[wname], base + col0 + fb * FB, [[ldw, 128], [128 * ldw, KC], [1, FB]]),
                    wdep(wname, *dep), [w_.b])

        loadx(0)
        loads(0)
        for ti, (gi, fb) in enumerate(tasks):
            if ti + 1 < len(tasks):
                if nxg == 2:
                    loadx(ti + 1)
                loads(ti + 1)
            grows = groups[gi]
            xg_ = xg[gi % nxg]
            Tn = len(grows) * 64
            ws = [wbk[si][ti % 2] for si in range(len(wspecs))]
            for ftl in range(FB // 128):
                ft = fb * (FB // 128) + ftl
                for h0 in range(0, Tn, 512):
                    n = min(512, Tn - h0)
                    bks = []
                    for w_ in ws:
                        bk = next_bank()
                        bks.append(bk)
                        for kc in range(KC):
                            P.op("pe", lambda e, bk=bk, w_=w_, kc=kc, ftl=ftl, xg_=xg_, h0=h0, n=n: e.matmul(
                                psum[bk][:, 0:n], w_.ap[:, kc, ftl * 128:(ftl + 1) * 128], xg_.ap[:, kc, h0:h0 + n],
                                start=(kc == 0), stop=(kc == KC - 1)), [w_.b, xg_.b], [PSB[bk]])
                    epi("run", (st, ft, bks, n, grows[h0 // 64:(h0 + n) // 64]))
            if ti + 1 < len(tasks) and nxg == 1:
                loadx(ti + 1)
        P.barrier()

    def store_fm(dst_ap, dst_name, f0, stg, rows_half):
        for (r0, n, j) in runs_of(rows_half):
            dma(dap(dst_ap, f0 * NSLOT + r0 * 64, [[NSLOT, 128], [1, 64 * n]]), stg.ap[:, j * 64:(j + n) * 64],
                [stg.b], P.Ds(dst_name, range(r0, r0 + n)))

    def lin_a(groups, xt_ap, KC, wname, dep, base, ldw, col0, ncols, NB, epi):
        A.reset()
        TMAX = max(len(g) for g in groups) * 64
        nxg = 2 if KC * TMAX * 2 * 2 <= 70 * 1024 else 1
        xg = [T([128, KC, TMAX], BF16) for _ in range(nxg)]
        wbk = [T([128, KC, NB], BF16) for _ in range(2)]
        st = epi("alloc", None)
        tasks = [(gi, nb) for gi in range(len(groups)) for nb in range(ncols // NB)]

        def loadx(ti):
            gi, nb = tasks[ti]
            if nb == 0:
                load_xg(xg[gi % nxg], xt_ap, 0, KC, groups[gi])

        def loads(ti):
            gi, nb = tasks[ti]
            w_ = wbk[ti % 2]
            dma(w_.ap, dap(wb[wname], base + col0 + nb * NB, [[ldw, 128], [128 * ldw, KC], [1, NB]]),
                wdep(wname, *dep), [w_.b])

        loadx(0)
        loads(0)
        for ti, (gi, nb) in enumerate(tasks):
            if ti + 1 < len(tasks):
                if nxg == 2:
                    loadx(ti + 1)
                loads(ti + 1)
            grows = groups[gi]
            xg_ = xg[gi % nxg]
            w_ = wbk[ti % 2]
            for t in range(len(grows) // 2):
                bk = next_bank()
                for kc in range(KC):
                    P.op("pe", lambda e, bk=bk, w_=w_, kc=kc, xg_=xg_, t=t: e.matmul(
                        psum[bk][:, 0:NB], xg_.ap[:, kc, t * 128:(t + 1) * 128], w_.ap[:, kc, :],
                        start=(kc == 0), stop=(kc == KC - 1)), [w_.b, xg_.b], [PSB[bk]])
                epi("run", (st, nb, bk, (grows[2 * t], grows[2 * t + 1]), gi * 8 + t))
            if ti + 1 < len(tasks) and nxg == 1:
                loadx(ti + 1)
        P.barrier()

    def epi_qk(mode, a):
        if mode == "alloc":
            return [T([128, 512], BF16) for _ in range(3)], [0]
        (stgs, cnt), ft, bks, n, rows_half = a
        stg = stgs[cnt[0] % 3]
        cnt[0] += 1
        sc = 0.125 if ft < 8 else 1.0
        if cnt[0] % 2:
            P.op("act", lambda e: e.activation(out=stg.ap[:, 0:n], in_=psum[bks[0]][:, 0:n], func=AF.Copy, scale=sc),
                 [PSB[bks[0]]], [stg.b])
        else:
            P.op("dve", lambda e: e.tensor_scalar(out=stg.ap[:, 0:n], in0=psum[bks[0]][:, 0:n], scalar1=sc, scalar2=None,
                                                  op0=ALU.mult), [PSB[bks[0]]], [stg.b])
        store_fm(QKT, "QKT", ft * 128, stg, rows_half)

    def epi_u(mode, a):
        if mode == "alloc":
            return [T([128, 512], F32) for _ in range(3)], [0]
        (stgs, cnt), ft, bks, n, rows_half = a
        stg = stgs[cnt[0] % 3]
        cnt[0] += 1
        if cnt[0] % 2:
            P.op("act", lambda e: e.activation(out=stg.ap[:, 0:n], in_=psum[bks[0]][:, 0:n], func=AF.Copy),
                 [PSB[bks[0]]], [stg.b])
        else:
            P.op("dve", lambda e: e.tensor_copy(out=stg.ap[:, 0:n], in_=psum[bks[0]][:, 0:n]), [PSB[bks[0]]], [stg.b])
        store_fm(UT, "UT", ft * 128, stg, rows_half)

    def epi_gu(mode, a):
        if mode == "alloc":
            return [T([128, 512], BF16) for _ in range(3)], [T([128, 512], F32) for _ in range(2)], [0]
        (stgs, tmps, cnt), ft, bks, n, rows_half = a
        stg = stgs[cnt[0] % 3]
        tmp = tmps[cnt[0] % 2]
        cnt[0] += 1
        P.op("act", lambda e: e.activation(out=tmp.ap[:, 0:n], in_=psum[bks[0]][:, 0:n], func=AF.Silu),
             [PSB[bks[0]]], [tmp.b])
        P.op("dve", lambda e: e.tensor_tensor(out=stg.ap[:, 0:n], in0=psum[bks[1]][:, 0:n], in1=tmp.ap[:, 0:n], op=ALU.mult),
             [PSB[bks[1]], tmp.b], [stg.b])
        store_fm(ACTT, "ACTT", ft * 128, stg, rows_half)

    def epi_v(mode, a):
        if mode == "alloc":
            vs = [T([128, 8, 65], BF16) for _ in range(3)]
            for v_ in vs:
                P.op("dve", lambda e, v_=v_: e.memset(v_.ap, 1.0), [], [v_.b])
            return vs, [0]
        (vs, cnt), nb, bk, (ra, rb), tix = a
        v_ = vs[cnt[0] % 3]
        cnt[0] += 1
        src = psum[bk][:, :].rearrange("p (a b) -> p a b", b=64)
        if cnt[0] % 2:
            P.op("act", lambda e: e.activation(out=v_.ap[:, :, 0:64], in_=src, func=AF.Copy), [PSB[bk]], [v_.b])
        else:
            P.op("dve", lambda e: e.tensor_copy(out=v_.ap[:, :, 0:64], in_=src), [PSB[bk]], [v_.b])
        for j, r in enumerate((ra, rb)):
            dma(VA[r * 64:(r + 1) * 64, nb * 520:(nb + 1) * 520],
                v_.ap[j * 64:(j + 1) * 64, :, :].rearrange("p a b -> p (a b)"), [v_.b], [P.D("VA", r)])

    def make_epi_res(src, src_name, NB, comb_e=None, tile_base=0):
        def epi(mode, a):
            if mode == "alloc":
                return [T([128, NB], F32) for _ in range(3)], [T([128, NB], F32) for _ in range(3)], [0]
            (hr, os_, cnt), nb, bk, (ra, rb), tix = a
            h_ = hr[cnt[0] % 3]
            o_ = os_[cnt[0] % 3]
            cnt[0] += 1
            for j, r in enumerate((ra, rb)):
                dma(h_.ap[j * 64:(j + 1) * 64, :], src[r * 64:(r + 1) * 64, nb * NB:(nb + 1) * NB],
                    [P.D(src_name, (r, nb) if src_name == "H" else r)], [h_.b])
            if comb_e is None:
                P.op("dve", lambda e: e.tensor_tensor(out=o_.ap, in0=psum[bk][:, 0:NB], in1=h_.ap, op=ALU.add),
                     [PSB[bk], h_.b], [o_.b])
            else:
                P.op("dve", lambda e: e.scalar_tensor_tensor(out=o_.ap, in0=psum[bk][:, 0:NB],
                                                             scalar=COMB.ap[:, tix, comb_e:comb_e + 1], in1=h_.ap,
                                                             op0=ALU.mult, op1=ALU.add), [PSB[bk], h_.b, COMB.b], [o_.b])
            for j, r in enumerate((ra, rb)):
                dma(H[r * 64:(r + 1) * 64, nb * NB:(nb + 1) * NB], o_.ap[j * 64:(j + 1) * 64, :], [o_.b],
                    [P.D("H", (r, nb))])
        return epi

    def Hrow_bufs(r, nblk):
        return [P.D("H", (r, k)) for k in range(nblk)]

    def phase_att(layer):
        A.reset()
        Tb = T([128, 16, 14, 64], BF16)
        MB = T([128, 16, 64], BF16)
        P.op("dve", lambda e: e.memset(MB.ap, 0.0), [], [MB.b])
        zt = T([1, 128], F32)
        P.op("dve", lambda e: e.memset(zt.ap, 0.0), [], [zt.b])
        dma(RPAD[0:1, 0:64], zt.ap[0:1, 0:64], [zt.b], [P.D("RPAD")])
        dma(RPAD[0:1, 64 + 7440:64 + 7440 + 128], zt.ap[0:1, 0:128], [zt.b], [P.D("RPAD")])
        dma(RPAD[0:1, 64:64 + 7440], rpb[layer:layer + 1, :], [], [P.D("RPAD")])
        mbt = T([16, 16], F32)
        dma(mbt.ap, mbT[layer, :, :], [], [mbt.b])
        P.op("dve", lambda e: e.tensor_copy(out=MB.ap[0:16, :, :], in_=mbt.ap.unsqueeze(2).to_broadcast([16, 16, 64])), [mbt.b], [MB.b])
        mark = A.off
        hk = [T([128, 4, 14, 64], F32) for _ in range(2)]
        bd = [T([128, 4, 14, 128], BF16) for _ in range(2)]
        for b_ in bd:
            P.op("dve", lambda e, b_=b_: e.memset(b_.ap, 0.0), [], [b_.b])
        for hg in range(4):
            hk_, bd_ = hk[hg % 2], bd[hg % 2]
            for kh in range(2):
                for hl in range(4):
                    dma(hk_.ap[kh * 64:(kh + 1) * 64, hl, :, :],
                        dap(RPAD, 64 + (hg * 4 + hl) * 465 + kh * 31 - 48, [[1, 64], [31, 14], [1, 64]]),
                        [P.D("RPAD")], [hk_.b])
                P.op("dve", lambda e, kh=kh, hk_=hk_, bd_=bd_: e.tensor_copy(
                    out=bd_.ap[kh * 64:(kh + 1) * 64, :, :, kh * 64:(kh + 1) * 64], in_=hk_.ap[kh * 64:(kh + 1) * 64, :, :, :]),
                    [hk_.b], [bd_.b])
            for hl in range(4):
                for e0 in (0, 7):
                    bk = next_bank()
                    for ei in range(7):
                        P.op("pe", lambda e, bk=bk, hl=hl, ee=e0 + ei, ei=ei, bd_=bd_: e.matmul(
                            psum[bk][:, ei * 64:(ei + 1) * 64], bd_.ap[:, hl, ee, :], jj.ap, start=True, stop=True),
                            [bd_.b, jj.b], [PSB[bk]])
                    P.op("dve", lambda e, bk=bk, h=hg * 4 + hl, e0=e0: e.tensor_tensor(
                        out=Tb.ap[:, h, e0:e0 + 7, :], in0=psum[bk][:, 0:448].rearrange("p (a b) -> p a b", b=64),
                        in1=colmask.ap.unsqueeze(1).to_broadcast([128, 7, 64]), op=ALU.add),
                        [PSB[bk], colmask.b], [Tb.b])
        P.barrier()
        A.off = mark
        NKMAX = 12 * 64
        qt = [T([128, 8, 64], BF16) for _ in range(2)]
        qa = [[T([128, 8, 64], BF16) for _ in range(2)] for _ in range(2)]
        ktw = [T([128, 8, NKMAX], BF16) for _ in range(2)]
        vw = [T([128, 6, 1040], BF16) for _ in range(2)]
        ktm = {r: T([128, 8, 128], BF16) for r in (0, ROW_PM)}
        vm = {r: T([128, 1040], BF16) for r in (0, ROW_PM)}
        pt = [T([128, 512], BF16) for _ in range(8)]
        rec = [T([64, 8], F32) for _ in range(2)]
        oa = [T([64, 16, 64], F32) for _ in range(2)]
        for p_ in pt:
            P.op("dve", lambda e, p_=p_: e.memset(p_.ap, 0.0), [], [p_.b])
        for i_ in range(2):
            for par in range(2):
                q_ = qa[par][i_]
                P.op("dve", lambda e, q_=q_: e.memset(q_.ap, 0.0), [], [q_.b])
        for r in (0, ROW_PM):
            P.op("dve", lambda e, r=r: e.memset(ktm[r].ap, 0.0), [], [ktm[r].b])
            P.op("dve", lambda e, r=r: e.memset(vm[r].ap, 0.0), [], [vm[r].b])
            dma(ktm[r].ap[:, :, 0:16], dap(QKT, 1024 * NSLOT + r * 64 + 48, [[NSLOT, 128], [128 * NSLOT, 8], [1, 16]]),
                [P.D("QKT", r)], [ktm[r].b])
            dma(vm[r].ap[0:16, :], VA[r * 64 + 48:r * 64 + 64, :], [P.D("VA", r)], [vm[r].b])
        sb_rr = [0]
        pt_rr = [0]
        ob_rr = [0]
        SBANKS = (0, 1, 2, 3)
        OBANKS = ((4, 5), (6, 7))
        plan_l = [x for x in PLAN if x[0] == layer]

        def ld(it):
            (ly, row, st, nb, mrow, mcol) = plan_l[it]
            p = it % 2
            qt_, kt_, vw_ = qt[p], ktw[p], vw[p]
            dma(qt_.ap, dap(QKT, row * 64, [[NSLOT, 128], [128 * NSLOT, 8], [1, 64]]), [P.D("QKT", row)], [qt_.b])
            if nb > 0:
                dma(kt_.ap[:, :, 0:nb * 128], dap(QKT, 1024 * NSLOT + st * 64, [[NSLOT, 128], [128 * NSLOT, 8], [1, nb * 128]]),
                    P.Ds("QKT", range(st, st + 2 * nb)), [kt_.b])
                dma(vw_.ap[:, 0:nb, :], dap(VA, st * 64 * 1040, [[1040, 128], [128 * 1040, nb], [1, 1040]]),
                    P.Ds("VA", range(st, st + 2 * nb)), [vw_.b])

        ld(0)
        for it, (ly, row, st, nb, mrow, mcol) in enumerate(plan_l):
            p = it % 2
            qt_, kt_, vw_, oa_ = qt[p], ktw[p], vw[p], oa[p]
            if it + 1 < len(plan_l):
                ld(it + 1)
            for par in range(2):
                q_ = qa[par][p]
                P.op("dve", lambda e, q_=q_, qt_=qt_, par=par: e.tensor_copy(
                    out=q_.ap[par * 64:(par + 1) * 64, :, :], in_=qt_.ap[par * 64:(par + 1) * 64, :, :]), [qt_.b], [q_.b])
            for par in range(2):
                q_ = qa[par][p]
                pts = []
                for b in range(nb + 1):
                    ismeta = (b == nb)
                    bk = SBANKS[sb_rr[0] % 4]
                    sb_rr[0] += 1
                    p_ = pt[pt_rr[0] % 8]
                    pt_rr[0] += 1
                    pts.append(p_)
                    if ismeta:
                        P.op("pe", lambda e, bk=bk, par=par: e.matmul(
                            psum[bk][0:16, :], ident.ap[:, 0:16], MB.ap[:, par::2, :], start=True, stop=False),
                            [ident.b, MB.b], [PSB[bk]])
                        for hp in range(8):
                            P.op("pe", lambda e, bk=bk, hp=hp, q_=q_, mrow=mrow: e.matmul(
                                psum[bk][0:16, hp * 64:(hp + 1) * 64], ktm[mrow].ap[:, hp, 0:16], q_.ap[:, hp, :],
                                start=False, stop=(hp == 7)), [ktm[mrow].b, q_.b], [PSB[bk]])
                        P.op("act", lambda e, bk=bk, p_=p_: e.activation(
                            out=p_.ap[0:16, :], in_=psum[bk][0:16, :], func=AF.Exp, bias=zero_c.ap[0:16, :], scale=1.0),
                            [PSB[bk], zero_c.b], [p_.b])
                    else:
                        e_idx = (st + 2 * b) - row + 7
                        P.op("pe", lambda e, bk=bk, par=par, e_idx=e_idx: e.matmul(
                            psum[bk][:, :], ident.ap, Tb.ap[:, par::2, e_idx, :], start=True, stop=False),
                            [ident.b, Tb.b], [PSB[bk]])
                        for hp in range(8):
                            P.op("pe", lambda e, bk=bk, hp=hp, q_=q_, kt_=kt_, b=b: e.matmul(
                                psum[bk][:, hp * 64:(hp + 1) * 64], kt_.ap[:, hp, b * 128:(b + 1) * 128], q_.ap[:, hp, :],
                                start=False, stop=(hp == 7)), [kt_.b, q_.b], [PSB[bk]])
                        P.op("act", lambda e, bk=bk, p_=p_, c=mcol + b: e.activation(
                            out=p_.ap, in_=psum[bk][:, :], func=AF.Exp, bias=rowmask.ap[:, c:c + 1], scale=1.0),
                            [PSB[bk], rowmask.b], [p_.b])
                ob = OBANKS[ob_rr[0] % 2]
                ob_rr[0] += 1
                for hp in range(8):
                    h = 2 * hp + par
                    obk = ob[hp // 4]
                    for b in range(nb + 1):
                        ismeta = (b == nb)
                        p_ = pts[b]
                        if ismeta:
                            P.op("pe", lambda e, obk=obk, hp=hp, h=h, p_=p_, b=b, mrow=mrow: e.matmul(
                                psum[obk][0:64, (hp % 4) * 65:(hp % 4 + 1) * 65], p_.ap[:, hp * 64:(hp + 1) * 64],
                                vm[mrow].ap[:, h * 65:(h + 1) * 65], start=(b == 0), stop=True),
                                [p_.b, vm[mrow].b], [PSB[obk]])
                        else:
                            P.op("pe", lambda e, obk=obk, hp=hp, h=h, p_=p_, b=b, vw_=vw_: e.matmul(
                                psum[obk][0:64, (hp % 4) * 65:(hp % 4 + 1) * 65], p_.ap[:, hp * 64:(hp + 1) * 64],
                                vw_.ap[:, b, h * 65:(h + 1) * 65], start=(b == 0), stop=False),
                                [p_.b, vw_.b], [PSB[obk]])
                rc = rec[par]
                for half in range(2):
                    obk = ob[half]
                    ov = psum[obk][0:64, 0:260].rearrange("p (a b) -> p a b", b=65)
                    P.op("dve", lambda e, ov=ov, rc=rc, half=half: e.reciprocal(
                        out=rc.ap[:, half * 4:(half + 1) * 4].unsqueeze(2), in_=ov[:, :, 64:65]), [PSB[obk]], [rc.b])
                    h0 = par + 8 * half
                    P.op("dve", lambda e, ov=ov, rc=rc, half=half, oa_=oa_, h0=h0: e.tensor_tensor(
                        out=oa_.ap[:, h0:h0 + 7:2, :], in0=ov[:, :, 0:64],
                        in1=rc.ap[:, half * 4:(half + 1) * 4].unsqueeze(2).to_broadcast([64, 4, 64]), op=ALU.mult),
                        [PSB[obk], rc.b], [oa_.b])
            dma(OA[row * 64:(row + 1) * 64, :], oa_.ap.rearrange("p a b -> p (a b)"), [oa_.b], [P.D("OA", row)])
        P.barrier()

    def phase_pool():
        A.reset()
        W_ = NSLOT + 32
        vt = T([128, NSLOT], F32)
        dma(vt.ap, t_valid.to_broadcast([128, NSLOT]), [], [vt.b])
        ic = T([128, NSLOT], F32)
        ut = [T([128, W_], F32) for _ in range(2)]
        sa = T([128, W_], F32)
        sb = T([128, W_], F32)
        plt = [T([128, NSLOT], BF16) for _ in range(2)]
        for t_ in ut + [sa, sb]:
            P.op("dve", lambda e, t_=t_: e.memset(t_.ap, 0.0), [], [t_.b])
        allrows = range(NR)
        for c in range(8):
            g = c // 2
            u_ = ut[c % 2]
            pl_ = plt[c % 2]
            if c % 2 == 0:
                dma(ic.ap, t_invcnt[g:g + 1, :].to_broadcast([128, NSLOT]), [], [ic.b])
            dma(u_.ap[:, 16:16 + NSLOT], UT[c * 128:(c + 1) * 128, :], P.Ds("UT", allrows), [u_.b])
            P.op("dve", lambda e, u_=u_: e.tensor_tensor(out=sa.ap[:, 16:16 + NSLOT], in0=u_.ap[:, 16:16 + NSLOT], in1=vt.ap,
                                                         op=ALU.mult), [u_.b, vt.b], [sa.b])
            lo, hi = 8, W_ - 8
            P.op("dve", lambda e: e.tensor_tensor(out=sb.ap[:, lo:hi], in0=sa.ap[:, lo - 1:hi - 1], in1=sa.ap[:, lo:hi], op=ALU.add),
                 [sa.b], [sb.b])
            cur, oth = sb, sa
            for k in range(g):
                sh = 1 << k
                P.op("dve", lambda e, cur=cur, oth=oth, sh=sh: e.tensor_tensor(
                    out=oth.ap[:, lo:hi], in0=cur.ap[:, lo - sh:hi - sh], in1=cur.ap[:, lo + sh:hi + sh], op=ALU.add),
                    [cur.b], [oth.b])
                cur, oth = oth, cur
            P.op("dve", lambda e, cur=cur, oth=oth: e.tensor_tensor(out=oth.ap[:, 16:16 + NSLOT], in0=cur.ap[:, 16:16 + NSLOT],
                                                                    in1=ic.ap, op=ALU.mult), [cur.b, ic.b], [oth.b])
            P.op("dve", lambda e, oth=oth, u_=u_, pl_=pl_: e.tensor_tensor(
                out=pl_.ap, in0=oth.ap[:, 16:16 + NSLOT], in1=u_.ap[:, 16:16 + NSLOT], op=ALU.subtract),
                [oth.b, u_.b], [pl_.b])
            dma(PLT[c * 128:(c + 1) * 128, :], pl_.ap, [pl_.b], P.Ds("PLT", allrows))
        P.barrier()

    def phase_mo(layer, rows):
        A.reset()
        ga = T([128, 1024], F32); gp = T([128, 1024], F32); psc = T([128, 1024], F32)
        dma(ga.ap, g_attn[layer:layer + 1, :].to_broadcast([128, 1024]), [], [ga.b])
        dma(gp.ap, g_pool[layer:layer + 1, :].to_broadcast([128, 1024]), [], [gp.b])
        dma(psc.ap, pscale[layer:layer + 1, :].to_broadcast([128, 1024]), [], [psc.b])
        pw = T([128, 4, 2, 256], BF16)
        dma(pw.ap, dap(wb["pool_w"], layer * 4 * 65536, [[256, 128], [65536, 4], [128 * 256, 2], [1, 256]]),
            wdep("pool_w", layer * 128, layer * 128 + 128), [pw.b])
        oat = [T([128, 1024], F32) for _ in range(2)]
        plt = [T([128, 8, 128], BF16) for _ in range(2)]
        mx = [T([128, 1024], F32) for _ in range(2)]
        on = [T([128, D], BF16) for _ in range(2)]
        ot = [T([128, 16, 128], BF16) for _ in range(2)]
        junk = T([128, 1024], BF16)
        ss = [T([128, 2], F32) for _ in range(2)]
        rs = [T([128, 2], F32) for _ in range(2)]
        tiles = [(rows[i], rows[i + 1]) for i in range(0, len(rows), 2)]
        def ld(ti):
            oa_, pl_ = oat[ti % 2], plt[ti % 2]
            for j, r in enumerate(tiles[ti]):
                dma(oa_.ap[j * 64:(j + 1) * 64, :], OA[r * 64:(r + 1) * 64, :], [P.D("OA", r)], [oa_.b])
                dma(pl_.ap[:, :, j * 64:(j + 1) * 64], dap(PLT, r * 64, [[NSLOT, 128], [128 * NSLOT, 8], [1, 64]]),
                    [P.D("PLT", r)], [pl_.b])

        ld(0)
        for ti, (ra, rb) in enumerate(tiles):
            p = ti % 2
            oa_, pl_, mx_, on_, ot_, ss_, rs_ = oat[p], plt[p], mx[p], on[p], ot[p], ss[p], rs[p]
            if ti + 1 < len(tiles):
                ld(ti + 1)
            bk2 = [next_bank(), next_bank()]
            for g in range(4):
                bk = bk2[g // 2]
                for cc in range(2):
                    P.op("pe", lambda e, bk=bk, g=g, cc=cc, pl_=pl_: e.matmul(
                        psum[bk][:, (g % 2) * 256:(g % 2 + 1) * 256], pl_.ap[:, 2 * g + cc, :], pw.ap[:, g, cc, :],
                        start=(cc == 0), stop=(cc == 1)), [pl_.b, pw.b], [PSB[bk]])
            for hf in range(2):
                P.op("dve", lambda e, hf=hf, mx_=mx_, bk=bk2[hf]: e.tensor_tensor(
                    out=mx_.ap[:, hf * 512:(hf + 1) * 512], in0=psum[bk][:, :], in1=psc.ap[:, hf * 512:(hf + 1) * 512], op=ALU.mult),
                    [PSB[bk2[hf]], psc.b], [mx_.b])
            P.op("act", lambda e, oa_=oa_, ss_=ss_: e.activation(out=junk.ap, in_=oa_.ap, func=AF.Square, accum_out=ss_.ap[:, 0:1]),
                 [oa_.b], [junk.b, ss_.b])
            P.op("act", lambda e, mx_=mx_, ss_=ss_: e.activation(out=junk.ap, in_=mx_.ap, func=AF.Square, accum_out=ss_.ap[:, 1:2]),
                 [mx_.b], [junk.b, ss_.b])
            P.op("act", lambda e, ss_=ss_, rs_=rs_: e.activation(out=rs_.ap, in_=ss_.ap, func=AF.Sqrt, bias=eps_c.ap,
                                                                 scale=1.0 / 1024), [ss_.b, eps_c.b], [rs_.b])
            P.op("dve", lambda e, rs_=rs_: e.reciprocal(out=rs_.ap, in_=rs_.ap), [rs_.b], [rs_.b])
            P.op("dve", lambda e, oa_=oa_, rs_=rs_, on_=on_: e.scalar_tensor_tensor(
                out=on_.ap[:, 0:1024], in0=oa_.ap, scalar=rs_.ap[:, 0:1], in1=ga.ap, op0=ALU.mult, op1=ALU.mult),
                [oa_.b, rs_.b, ga.b], [on_.b])
            P.op("dve", lambda e, mx_=mx_, rs_=rs_, on_=on_: e.scalar_tensor_tensor(
                out=on_.ap[:, 1024:2048], in0=mx_.ap, scalar=rs_.ap[:, 1:2], in1=gp.ap, op0=ALU.mult, op1=ALU.mult),
                [mx_.b, rs_.b, gp.b], [on_.b])
            bks = [next_bank(), next_bank()]
            for kc in range(16):
                bk = bks[kc // 8]
                P.op("pe", lambda e, on_=on_, kc=kc, bk=bk: e.transpose(
                    out=psum[bk][:, :].bitcast(BF16)[:, (kc % 8) * 128:(kc % 8 + 1) * 128],
                    in_=on_.ap[:, kc * 128:(kc + 1) * 128], identity=ident.ap), [on_.b, ident.b], [PSB[bk]])
            P.op("act", lambda e, ot_=ot_, bk=bks[0]: e.activation(
                out=ot_.ap[:, 0:8, :], in_=psum[bk][:, :].bitcast(BF16).rearrange("p (a b) -> p a b", b=128), func=AF.Copy),
                [PSB[bks[0]]], [ot_.b])
            P.op("dve", lambda e, ot_=ot_, bk=bks[1]: e.tensor_copy(
                out=ot_.ap[:, 8:16, :], in_=psum[bk][:, :].bitcast(BF16).rearrange("p (a b) -> p a b", b=128)),
                [PSB[bks[1]]], [ot_.b])
            for j, r in enumerate((ra, rb)):
                dma(dap(OT, r * 64, [[NSLOT, 128], [128 * NSLOT, 16], [1, 64]]), ot_.ap[:, :, j * 64:(j + 1) * 64],
                    [ot_.b], [P.D("OT", r)])
        P.barrier()

    def phase_final():
        A.reset()
        gt = T([128, D], F32)
        dma(gt.ap, g_final.to_broadcast([128, D]), [], [gt.b])
        hb = [T([128, D], F32) for _ in range(2)]
        ob = [T([128, D], F32) for _ in range(2)]
        junk = T([128, D], BF16)
        ss = [T([128, 1], F32) for _ in range(2)]
        rs = [T([128, 1], F32) for _ in range(2)]
        tiles = [(L1_OUT[i], L1_OUT[i + 1]) for i in range(0, len(L1_OUT), 2)]
        def ld(ti):
            h_ = hb[ti % 2]
            for j, r in enumerate(tiles[ti]):
                dma(h_.ap[j * 64:(j + 1) * 64, :], H[r * 64:(r + 1) * 64, :], [P.D("H", r)], [h_.b])

        ld(0)
        for ti, (ra, rb) in enumerate(tiles):
            p = ti % 2
            h_, o_, ss_, rs_ = hb[p], ob[p], ss[p], rs[p]
            if ti + 1 < len(tiles):
                ld(ti + 1)
            P.op("act", lambda e, h_=h_, ss_=ss_: e.activation(out=junk.ap, in_=h_.ap, func=AF.Square, accum_out=ss_.ap),
                 [h_.b], [junk.b, ss_.b])
            P.op("act", lambda e, ss_=ss_, rs_=rs_: e.activation(out=rs_.ap, in_=ss_.ap, func=AF.Sqrt, bias=eps_c.ap,
                                                                 scale=1.0 / D), [ss_.b, eps_c.b], [rs_.b])
            P.op("dve", lambda e, rs_=rs_: e.reciprocal(out=rs_.ap, in_=rs_.ap), [rs_.b], [rs_.b])
            P.op("dve", lambda e, h_=h_, rs_=rs_, o_=o_: e.scalar_tensor_tensor(
                out=o_.ap, in0=h_.ap, scalar=rs_.ap, in1=gt.ap, op0=ALU.mult, op1=ALU.mult), [h_.b, rs_.b, gt.b], [o_.b])
            for j, r in enumerate((ra, rb)):
                if r <= 32:
                    dst = ys[(r - 1) * 64:r * 64, :]
                else:
                    dst = yp[(r - 43) * 64:(r - 42) * 64, :]
                dma(dst, o_.ap[j * 64:(j + 1) * 64, :], [o_.b], [])
        P.barrier()

    def mixer(layer, kv_rows, out_rows, src, src_name):
        WB = layer * D * 4096
        dep_in = (layer * WIN_R, (layer + 1) * WIN_R)
        phase_nt(kv_rows, src, src_name, ln_mix[layer:layer + 1, :])
        if stop_here(f"nt{layer}"): return True
        gk = groups_of(kv_rows)
        lin_b(gk, HNT, 16, [("w_in", dep_in, WB, 4096, 0)], 2048, epi_qk)
        lin_a(gk, HNT, 16, "w_in", dep_in, WB, 4096, 2048, 1024, 512, epi_v)
        lin_b(gk, HNT, 16, [("w_in", dep_in, WB, 4096, 3072)], 1024, epi_u)
        if stop_here(f"qkvu{layer}"): return True
        phase_att(layer)
        if stop_here(f"att{layer}"): return True
        phase_pool()
        if stop_here(f"pool{layer}"): return True
        orows = out_rows if len(out_rows) % 2 == 0 else out_rows + [65]
        phase_mo(layer, orows)
        if stop_here(f"mo{layer}"): return True
        lin_a(groups_of(orows), OT, 16, "w_out", (layer * D, (layer + 1) * D), layer * D * D, D, 0, D, 512,
              make_epi_res(src, src_name, 512))
        if stop_here(f"wout{layer}"): return True
        return False

    def run_all():
        if mixer(0, L0_ALL, L0_OUT, xs, "xs"): return
        phase_nt(L0_OUT, H, "H4", ln_ffn[0:1, :])
        g0 = groups_of(L0_OUT)
        lin_b(g0, HNT, 16, [("ffn_g", (0, DFF), 0, DFF, 0), ("ffn_u", (0, DFF), 0, DFF, 0)], DFF, epi_gu)
        lin_a(g0, ACTT, 44, "ffn_d", (0, DFF), 0, D, 0, D, 256, make_epi_res(H, "H", 256))
        if stop_here("ffn0"): return
        if mixer(1, L1_KV, L1_OUT, H, "H8"): return
        phase_nt(L1_OUT, H, "H4", ln_ffn[1:2, :], router=True)
        g1 = groups_of(L1_OUT)
        for ex_ in range(NE):
            dep = (ex_ * DFF, (ex_ + 1) * DFF)
            lin_b(g1, HNT, 16, [("moe_g", dep, ex_ * D * DFF, DFF, 0), ("moe_u", dep, ex_ * D * DFF, DFF, 0)], DFF, epi_gu)
            lin_a(g1, ACTT, 44, "moe_d", dep, ex_ * DFF * D, D, 0, D, 256, make_epi_res(H, "H", 256, comb_e=ex_))
        phase_final()

    run_all()
    P.barrier()

    with nc.Block() as block:
        @block.sync
        def _(e):
            P.emit("sp", e)

        @block.gpsimd
        def _(e):
            P.emit("pool", e)

        @block.scalar
        def _(e):
            P.emit("act", e)

        @block.vector
        def _(e):
            P.emit("dve", e)

        @block.tensor
        def _(e):
            P.emit("pe", e)
    es.close()
    return nc


def make_in_maps(inp):
    f = np.float32
    xp = np.asarray(inp["x_prompt"], f)[0]
    xsm = np.asarray(inp["x_sample"], f)
    meta = np.asarray(inp["meta_tokens"], f)
    shared = {
        "w_in": np.ascontiguousarray(np.asarray(inp["w_in"], f)).reshape(-1, 2048),
        "w_out": np.ascontiguousarray(np.asarray(inp["w_out"], f)).reshape(-1, 2048),
        "pool_w": np.ascontiguousarray(np.asarray(inp["pool_w"], f)).reshape(-1, 2048),
        "ffn_g": np.ascontiguousarray(np.asarray(inp["ffn_w_gate"], f)).reshape(-1, 2048),
        "ffn_u": np.ascontiguousarray(np.asarray(inp["ffn_w_up"], f)).reshape(-1, 2048),
        "ffn_d": np.ascontiguousarray(np.asarray(inp["ffn_w_down"], f)).reshape(-1, 2048),
        "moe_g": np.ascontiguousarray(np.asarray(inp["moe_w_gate"], f)).reshape(-1, 2048),
        "moe_u": np.ascontiguousarray(np.asarray(inp["moe_w_up"], f)).reshape(-1, 2048),
        "moe_d": np.ascontiguousarray(np.asarray(inp["moe_w_down"], f)).reshape(-1, 2048),
        "rpb": np.ascontiguousarray(np.asarray(inp["rpb"], f)).reshape(2, -1),
        "mbT": np.ascontiguousarray(np.transpose(np.asarray(inp["meta_bias"], f), (0, 2, 1))),
        "ln_mix": np.asarray(inp["ln_mix"], f), "ln_ffn": np.asarray(inp["ln_ffn"], f),
        "g_attn": np.asarray(inp["g_attn_out"], f), "g_pool": np.asarray(inp["g_pool_out"], f),
        "pscale": np.asarray(inp["pool_scale"], f),
        "g_final": np.asarray(inp["g_final"], f).reshape(1, D),
        "routerT": np.ascontiguousarray(np.asarray(inp["router"], f)[0].T),
    }
    maps = []
    for c in range(8):
        x = np.zeros((NSLOT, D), f)
        x[48:64] = meta
        x[64:64 + 2048] = xsm[c]
        x[ROW_PM * 64 + 48:ROW_PM * 64 + 64] = meta
        x[34 * 64:35 * 64] = xp[0:64]
        for i in range(30):
            r = 16 * c - 8 + i
            b0 = (ROW_P0 + i) * 64
            if 0 <= r < 128:
                x[b0:b0 + 64] = xp[r * 64:(r + 1) * 64]
            elif r == -1:
                x[b0 + 48:b0 + 64] = meta
        m = dict(shared)
        m["xs"] = x
        m.update(host_tables(c))
        maps.append(m)
    return maps


_NC = None


def kernel(**inputs):
    global _NC
    if _NC is None:
        _NC = build()
    maps = make_in_maps(inputs)
    res = run_bass_kernel_spmd(_NC, maps, core_ids=list(range(8)))
    ysamp = np.stack([np.asarray(res.results[c]["ys"], np.float32) for c in range(8)], axis=0)
    yprm = np.concatenate([np.asarray(res.results[c]["yp"], np.float32) for c in range(8)], axis=0)[None]
    return (yprm, ysamp)
```

```python
import numpy as np
import concourse.bass as bass
import concourse.mybir as mybir
from concourse.bass_utils import run_bass_kernel_spmd

F32, BF16 = mybir.dt.float32, mybir.dt.bfloat16
AF = mybir.ActivationFunctionType
ALU = mybir.AluOpType

D = 2048
DFF = 5632
NE = 8
NR = 66
NSLOT = NR * 64
NEG = -30000.0
EPS = 1e-6
ROW_PM = 33
ROW_P0 = 35
DBG = {"stop": None, "dump": []}

L0_ALL = list(range(66))
L0_OUT = list(range(0, 34)) + list(range(39, 62)) + [65]
L1_KV = L0_OUT
L1_OUT = list(range(1, 33)) + list(range(43, 59))


def groups_of(rows, g=16):
    return [rows[i:i + g] for i in range(0, len(rows), g)]


def runs_of(rows):
    out = []
    for j, r in enumerate(rows):
        if out and out[-1][0] + out[-1][1] == r:
            out[-1][1] += 1
        else:
            out.append([r, 1, j])
    return [tuple(x) for x in out]


def att_plan():
    plan = []
    col = 2
    for layer in range(2):
        rows = []
        if layer == 0:
            rows += [0]
        rows += list(range(1, 33))
        if layer == 0:
            rows += [ROW_PM]
        prange = range(4, 27) if layer == 0 else range(8, 24)
        rows += [ROW_P0 + i for i in prange]
        for row in rows:
            if row == 0 or row == ROW_PM:
                plan.append((layer, row, 0, 0, row, col))
                continue
            if row <= 32:
                g = row - 1
                r0 = min(max(g - 4, 0), 24)
                plan.append((layer, row, 1 + r0, 4, 0, col))
                col += 4
                continue
            i = row - ROW_P0
            st, nb = i - 4, 4
            if i in (8, 9):
                nb = 6
            elif i in (10, 11):
                nb = 5
            elif i in (21, 22):
                st, nb = 16, 5
            elif i == 23:
                st, nb = 16, 6
            plan.append((layer, row, ROW_P0 + st, nb, ROW_PM, col))
            col += nb
    return plan, col


PLAN, NMASK = att_plan()


def host_tables(c):
    rowmask = np.zeros((128, NMASK), np.float32)
    rowmask[16:, 1] = NEG
    for (layer, row, st, nb, mrow, col) in PLAN:
        if row < ROW_P0:
            continue
        i = row - ROW_P0
        r = 16 * c - 8 + i
        for b in range(nb):
            for kh in range(2):
                j = st - ROW_P0 + 2 * b + kh
                rk = 16 * c - 8 + j
                if r < 0 or r > 127:
                    ok = not (c == 0 and r == -1)
                else:
                    r0 = min(max(r - 4, 0), 120)
                    ok = (r0 <= rk <= r0 + 7)
                if not ok:
                    rowmask[kh * 64:(kh + 1) * 64, col + b] = NEG
    valid = np.zeros((NSLOT,), np.float32)
    pos = np.zeros((NSLOT,), np.int64)
    slen = np.ones((NSLOT,), np.int64)
    sl = np.arange(64)
    valid[48:64] = 1; pos[48:64] = np.arange(16); slen[0:64] = 2064
    for k in range(1, 33):
        valid[k * 64:(k + 1) * 64] = 1
        pos[k * 64:(k + 1) * 64] = 16 + (k - 1) * 64 + sl
        slen[k * 64:(k + 1) * 64] = 2064
    LP = 16 + 8192
    b0 = ROW_PM * 64
    valid[b0 + 48:b0 + 64] = 1; pos[b0 + 48:b0 + 64] = np.arange(16); slen[b0:b0 + 64] = LP
    b0 = 34 * 64
    valid[b0:b0 + 64] = 1; pos[b0:b0 + 64] = 16 + sl; slen[b0:b0 + 64] = LP
    for i in range(30):
        r = 16 * c - 8 + i
        b0 = (ROW_P0 + i) * 64
        slen[b0:b0 + 64] = LP
        if 0 <= r < 128:
            valid[b0:b0 + 64] = 1
            pos[b0:b0 + 64] = 16 + r * 64 + sl
        elif r == -1:
            valid[b0 + 48:b0 + 64] = 1
            pos[b0 + 48:b0 + 64] = np.arange(16)
    invcnt = np.zeros((4, NSLOT), np.float32)
    for g, w in enumerate((2, 4, 8, 16)):
        lo = np.maximum(pos - w // 2, 0)
        hi = np.minimum(pos + w // 2, slen)
        cnt = np.where(valid > 0, hi - lo, w)
        invcnt[g] = 1.0 / cnt.astype(np.float32)
    colmask = np.zeros((128, 64), np.float32)
    for qc in range(64):
        cs = min(max(qc - 8, 0), 48)
        for kc in range(64):
            if not (cs <= kc < cs + 16):
                colmask[kc, qc] = NEG
                colmask[64 + kc, qc] = NEG
    jj = np.zeros((128, 64), np.float32)
    for q in range(64):
        jj[q, 63 - q] = 1.0
        jj[64 + q, 63 - q] = 1.0
    return dict(rowmask=rowmask, valid=valid.reshape(1, NSLOT), invcnt=invcnt, colmask=colmask, jj=jj,
                ident=np.eye(128, dtype=np.float32))


class Buf:
    __slots__ = ("w", "r")

    def __init__(self):
        self.w = None
        self.r = {}


class Q:
    def __init__(self, name, csem, dsems):
        self.name, self.csem, self.dsems = name, csem, dsems
        self.ccnt = 0
        self.dcnt = [0] * len(dsems)
        self.i = 0
        self.ops = []
        self.known = {}


class Prog:
    def __init__(self, nc, es):
        self.nc = nc
        self.q = {}
        for name, ndma, comp in (("sp", 8, False), ("pool", 8, False), ("act", 6, True), ("dve", 0, True), ("pe", 0, True)):
            dsems = [es.enter_context(nc.semaphore(f"d_{name}{k}")) for k in range(ndma)]
            csem = es.enter_context(nc.semaphore(f"c_{name}")) if comp else None
            self.q[name] = Q(name, csem, dsems)
        self.dbufs = {}
        self.allbufs = []

    def buf(self, persistent=False):
        b = Buf()
        if not persistent:
            self.allbufs.append(b)
        return b

    def D(self, name, key=0):
        k = (name, key)
        b = self.dbufs.get(k)
        if b is None:
            b = self.dbufs[k] = self.buf()
        return b

    def Ds(self, name, keys):
        return [self.D(name, k) for k in keys]

    def op(self, qn, fn, reads=(), writes=(), dma=None):
        q = self.q[qn]
        if dma is None:
            dma = q.csem is None
        waits = {}

        def need(ev):
            if ev is None:
                return
            sem, val = ev
            if qn == "pe" and sem is q.csem:
                return
            k = id(sem)
            if k not in waits or waits[k][1] < val:
                waits[k] = (sem, val)

        for b in reads:
            need(b.w)
        for b in writes:
            need(b.w)
            for ev in b.r.values():
                need(ev)
        if dma:
            k = q.i % len(q.dsems)
            q.i += 1
            sem = q.dsems[k]
            if q.dcnt[k] > 0:
                need((sem, q.dcnt[k]))
            q.dcnt[k] += 16
            ev = (sem, q.dcnt[k])
            inc = 16
        else:
            sem = q.csem
            q.ccnt += 1
            ev = (sem, q.ccnt)
            inc = 1
        wl = []
        for k2, (s_, v) in waits.items():
            if q.known.get(k2, 0) >= v:
                continue
            q.known[k2] = v
            wl.append((s_, v))
        q.ops.append((fn, wl, sem, inc))
        for b in reads:
            k3 = id(sem)
            if k3 not in b.r or b.r[k3][1] < ev[1]:
                b.r[k3] = ev
        for b in writes:
            b.w = ev
            b.r = {}
        return ev

    def barrier(self):
        evs = []
        for q in self.q.values():
            if q.name == "pool":
                continue
            if q.csem is not None and q.ccnt > 0:
                evs.append((q.csem, q.ccnt))
            for k, s_ in enumerate(q.dsems):
                if q.dcnt[k] > 0:
                    evs.append((s_, q.dcnt[k]))
        for q in self.q.values():
            if q.name == "pool":
                continue
            for (s_, v) in evs:
                if q.known.get(id(s_), 0) < v:
                    q.known[id(s_)] = v
                    q.ops.append((None, [(s_, v)], None, 0))
        for b in self.allbufs:
            b.w = None
            b.r = {}

    def emit(self, qn, e):
        q = self.q[qn]
        for fn, wl, sem, inc in q.ops:
            for s_, v in wl:
                e.wait_ge(s_, v)
            if fn is not None:
                fn(e).then_inc(sem, inc)
        if qn == "sp":
            for k, s_ in enumerate(q.dsems):
                if q.dcnt[k] > 0:
                    e.wait_ge(s_, q.dcnt[k])


class Arena:
    def __init__(self, ap_u8, nbytes):
        self.ap, self.n = ap_u8, nbytes
        self.base = 0
        self.off = 0

    def persist(self):
        self.base = self.off

    def reset(self):
        self.off = self.base

    def alloc(self, shape, dt):
        esz = 4 if dt == F32 else 2
        n = int(np.prod(shape[1:]))
        nb = (n * esz + 63) // 64 * 64
        assert self.off + nb <= self.n, f"SBUF arena overflow {self.off}+{nb}>{self.n}"
        a = self.ap[0:shape[0], self.off:self.off + nb].bitcast(dt)[:, 0:n]
        self.off += nb
        if len(shape) == 3:
            a = a.rearrange("p (a b) -> p a b", b=shape[2])
        elif len(shape) == 4:
            a = a.rearrange("p (a b c) -> p a b c", b=shape[2], c=shape[3])
        return a


def dap(t, off, dims):
    return bass.AP(tensor=t.tensor, offset=off, ap=[[int(s), int(n)] for s, n in dims])


def build():
    from contextlib import ExitStack
    nc = bass.Bass("TRN2", target_bir_lowering=False)
    es = ExitStack()
    es.enter_context(nc.allow_low_precision("bf16 matmul operands, fp32 accumulate"))
    es.enter_context(nc.allow_non_contiguous_dma(reason="small strided param loads"))

    def din(name, shape, dt=F32):
        return nc.dram_tensor(name, list(shape), dt, kind="ExternalInput").ap()

    def dscr(name, shape, dt):
        kind = "ExternalOutput" if name in DBG["dump"] else "Internal"
        return nc.dram_tensor(name, list(shape), dt, kind=kind).ap()

    xs = din("xs", [NSLOT, D])
    wsrc = {
        "w_in": din("w_in", [2 * D * 4096 // 2048, 2048]),
        "w_out": din("w_out", [2 * D, 2048]),
        "pool_w": din("pool_w", [2 * 4 * 256 * 256 // 2048, 2048]),
        "ffn_g": din("ffn_g", [DFF, 2048]), "ffn_u": din("ffn_u", [DFF, 2048]), "ffn_d": din("ffn_d", [DFF, 2048]),
        "moe_g": din("moe_g", [NE * DFF, 2048]), "moe_u": din("moe_u", [NE * DFF, 2048]),
        "moe_d": din("moe_d", [NE * DFF, 2048]),
    }
    rpb = din("rpb", [2, 16 * 15 * 31])
    mbT = din("mbT", [2, 16, 16])
    ln_mix = din("ln_mix", [2, D]); ln_ffn = din("ln_ffn", [2, D])
    g_attn = din("g_attn", [2, 1024]); g_pool = din("g_pool", [2, 1024]); pscale = din("pscale", [2, 1024])
    g_final = din("g_final", [1, D])
    routerT = din("routerT", [NE, D])
    t_rowmask = din("rowmask", [128, NMASK]); t_valid = din("valid", [1, NSLOT]); t_invcnt = din("invcnt", [4, NSLOT])
    t_colmask = din("colmask", [128, 64]); t_jj = din("jj", [128, 64]); t_ident = din("ident", [128, 128])
    ys = nc.dram_tensor("ys", [2048, D], F32, kind="ExternalOutput").ap()
    yp = nc.dram_tensor("yp", [1024, D], F32, kind="ExternalOutput").ap()

    wb = {k: dscr(k + "_b", list(v.shape), BF16) for k, v in wsrc.items()}
    H = dscr("H", [NSLOT, D], F32)
    HNT = dscr("HNT", [D, NSLOT], BF16)
    QKT = dscr("QKT", [D, NSLOT], BF16)
    VA = dscr("VA", [NSLOT, 1040], BF16)
    UT = dscr("UT", [1024, NSLOT], F32)
    PLT = dscr("PLT", [1024, NSLOT], BF16)
    OA = dscr("OA", [NSLOT, 1024], F32)
    OT = dscr("OT", [D, NSLOT], BF16)
    ACTT = dscr("ACTT", [DFF, NSLOT], BF16)
    RPAD = dscr("RPAD", [1, 64 + 7440 + 128], F32)

    ARENA_BYTES = 188 * 1024
    arena_t = es.enter_context(nc.sbuf_tensor("arena", [128, ARENA_BYTES], mybir.dt.uint8))
    A = Arena(arena_t[:, :], ARENA_BYTES)
    psum = [es.enter_context(nc.psum_tensor(f"ps{i}", [128, 512], F32)) for i in range(8)]
    P = Prog(nc, es)
    PSB = [P.buf() for _ in range(8)]

    class T:
        def __init__(self, shape, dt):
            self.ap = A.alloc(shape, dt)
            self.b = P.buf()

    def dma(out, in_, reads, writes, qn="sp"):
        return P.op(qn, lambda e: e.dma_start(out=out, in_=in_), reads, writes, dma=True)

    ident_f = T([128, 128], F32); ident = T([128, 128], BF16)
    jj_f = T([128, 64], F32); jj = T([128, 64], BF16)
    colmask = T([128, 64], F32)
    rowmask = T([128, NMASK], F32)
    COMB = T([128, 24, 8], F32)
    zero_c = T([128, 1], F32)
    dma(ident_f.ap, t_ident, [], [ident_f.b]); dma(jj_f.ap, t_jj, [], [jj_f.b])
    dma(colmask.ap, t_colmask, [], [colmask.b]); dma(rowmask.ap, t_rowmask, [], [rowmask.b])
    P.op("dve", lambda e: e.tensor_copy(out=ident.ap, in_=ident_f.ap), [ident_f.b], [ident.b])
    P.op("dve", lambda e: e.tensor_copy(out=jj.ap, in_=jj_f.ap), [jj_f.b], [jj.b])
    P.op("dve", lambda e: e.memset(zero_c.ap, 0.0), [], [zero_c.b])
    eps_c = T([128, 1], F32)
    P.op("dve", lambda e: e.memset(eps_c.ap, EPS), [], [eps_c.b])
    A.persist()

    WBUFS = {}

    def cast_rows(name, r0, r1):
        CH = 2048
        lst = WBUFS.setdefault((name, r0, r1), [])
        for a in range(r0, r1, CH):
            b_ = min(a + CH, r1)
            bb = P.buf(persistent=True)
            lst.append(bb)
            dma(wb[name][a:b_, :], wsrc[name][a:b_, :], [], [bb], qn="pool")

    def wdep(name, r0, r1):
        return WBUFS[(name, r0, r1)]

    WIN_R = D * 4096 // 2048
    cast_rows("w_in", 0, WIN_R); cast_rows("pool_w", 0, 128); cast_rows("w_out", 0, D)
    cast_rows("ffn_g", 0, DFF); cast_rows("ffn_u", 0, DFF); cast_rows("ffn_d", 0, DFF)
    cast_rows("w_in", WIN_R, 2 * WIN_R); cast_rows("pool_w", 128, 256); cast_rows("w_out", D, 2 * D)
    for e_ in range(NE):
        for nm in ("moe_g", "moe_u", "moe_d"):
            cast_rows(nm, e_ * DFF, (e_ + 1) * DFF)

    bank_rr = [0]

    def next_bank():
        i = bank_rr[0] % 8
        bank_rr[0] += 1
        return i

    def stop_here(tag):
        return DBG["stop"] == tag

    def phase_nt(rows, src, src_name, gain_ap, router=False):
        A.reset()
        gt = T([128, D], F32)
        dma(gt.ap, gain_ap.to_broadcast([128, D]), [], [gt.b])
        hb = [T([128, D], F32) for _ in range(2)]
        hnb = [T([128, D], BF16) for _ in range(2)]
        xt = [T([128, 16, 128], BF16) for _ in range(2)]
        junk = T([128, D], BF16)
        ss = [T([128, 1], F32) for _ in range(2)]
        rs = [T([128, 1], F32) for _ in range(2)]
        if router:
            rt = T([128, NE, D], F32)
            dma(rt.ap, dap(routerT, 0, [[0, 128], [D, NE], [1, D]]), [], [rt.b])
            hnf = T([128, D], F32)
            junk2 = T([128, D], F32)
            lg = T([128, 8], F32); m8 = T([128, 8], F32); nv1 = T([128, 1], F32)
            ex = T([128, 8], F32); mk = T([128, 8], F32); num = T([128, 8], F32); den = T([128, 1], F32)
        tiles = [(rows[i], rows[i + 1]) for i in range(0, len(rows), 2)]
        def ld(ti):
            ra, rb = tiles[ti]
            h_ = hb[ti % 2]
            dma(h_.ap[0:64, :], src[ra * 64:(ra + 1) * 64, :], [P.D(src_name, ra)], [h_.b])
            dma(h_.ap[64:128, :], src[rb * 64:(rb + 1) * 64, :], [P.D(src_name, rb)], [h_.b])

        ld(0)
        for ti, (ra, rb) in enumerate(tiles):
            p = ti % 2
            h_, hn_, xt_, ss_, rs_ = hb[p], hnb[p], xt[p], ss[p], rs[p]
            if ti + 1 < len(tiles):
                ld(ti + 1)
            P.op("act", lambda e, h_=h_, ss_=ss_: e.activation(out=junk.ap, in_=h_.ap, func=AF.Square, accum_out=ss_.ap),
                 [h_.b], [junk.b, ss_.b])
            P.op("act", lambda e, ss_=ss_, rs_=rs_: e.activation(out=rs_.ap, in_=ss_.ap, func=AF.Sqrt, bias=eps_c.ap,
                                                                 scale=1.0 / D), [ss_.b, eps_c.b], [rs_.b])
            P.op("dve", lambda e, rs_=rs_: e.reciprocal(out=rs_.ap, in_=rs_.ap), [rs_.b], [rs_.b])
            if not router:
                P.op("dve", lambda e, h_=h_, rs_=rs_, hn_=hn_: e.scalar_tensor_tensor(
                    out=hn_.ap, in0=h_.ap, scalar=rs_.ap, in1=gt.ap, op0=ALU.mult, op1=ALU.mult),
                    [h_.b, rs_.b, gt.b], [hn_.b])
            else:
                P.op("dve", lambda e, h_=h_, rs_=rs_: e.scalar_tensor_tensor(
                    out=hnf.ap, in0=h_.ap, scalar=rs_.ap, in1=gt.ap, op0=ALU.mult, op1=ALU.mult),
                    [h_.b, rs_.b, gt.b], [hnf.b])
                P.op("act", lambda e, hn_=hn_: e.activation(out=hn_.ap, in_=hnf.ap, func=AF.Copy), [hnf.b], [hn_.b])
                for ex_ in range(NE):
                    P.op("dve", lambda e, ex_=ex_: e.scalar_tensor_tensor(
                        out=junk2.ap, in0=hnf.ap, scalar=1.0, in1=rt.ap[:, ex_, :], op0=ALU.mult, op1=ALU.mult,
                        accum_out=lg.ap[:, ex_:ex_ + 1]), [hnf.b, rt.b], [junk2.b, lg.b])
                P.op("dve", lambda e: e.max(out=m8.ap, in_=lg.ap), [lg.b], [m8.b])
                P.op("dve", lambda e: e.tensor_scalar(out=nv1.ap, in0=m8.ap[:, 0:1], scalar1=-1.0, scalar2=None, op0=ALU.mult),
                     [m8.b], [nv1.b])
                P.op("act", lambda e: e.activation(out=ex.ap, in_=lg.ap, func=AF.Exp, bias=nv1.ap, scale=1.0),
                     [lg.b, nv1.b], [ex.b])
                P.op("dve", lambda e: e.tensor_scalar(out=mk.ap, in0=lg.ap, scalar1=m8.ap[:, 1:2], scalar2=None, op0=ALU.is_ge),
                     [lg.b, m8.b], [mk.b])
                P.op("dve", lambda e: e.scalar_tensor_tensor(out=num.ap, in0=ex.ap, scalar=1.0, in1=mk.ap, op0=ALU.mult,
                                                             op1=ALU.mult, accum_out=den.ap), [ex.b, mk.b], [num.b, den.b])
                P.op("dve", lambda e: e.reciprocal(out=den.ap, in_=den.ap), [den.b], [den.b])
                P.op("dve", lambda e, ti=ti: e.tensor_scalar(out=COMB.ap[:, ti, :], in0=num.ap, scalar1=den.ap, scalar2=None,
                                                             op0=ALU.mult), [num.b, den.b], [COMB.b])
            bks = [next_bank(), next_bank()]
            for kc in range(16):
                bk = bks[kc // 8]
                P.op("pe", lambda e, hn_=hn_, kc=kc, bk=bk: e.transpose(
                    out=psum[bk][:, :].bitcast(BF16)[:, (kc % 8) * 128:(kc % 8 + 1) * 128],
                    in_=hn_.ap[:, kc * 128:(kc + 1) * 128], identity=ident.ap), [hn_.b, ident.b], [PSB[bk]])
            P.op("act", lambda e, xt_=xt_, bk=bks[0]: e.activation(
                out=xt_.ap[:, 0:8, :], in_=psum[bk][:, :].bitcast(BF16).rearrange("p (a b) -> p a b", b=128), func=AF.Copy),
                [PSB[bks[0]]], [xt_.b])
            P.op("dve", lambda e, xt_=xt_, bk=bks[1]: e.tensor_copy(
                out=xt_.ap[:, 8:16, :], in_=psum[bk][:, :].bitcast(BF16).rearrange("p (a b) -> p a b", b=128)),
                [PSB[bks[1]]], [xt_.b])
            for j, r in enumerate((ra, rb)):
                dma(dap(HNT, r * 64, [[NSLOT, 128], [128 * NSLOT, 16], [1, 64]]), xt_.ap[:, :, j * 64:(j + 1) * 64],
                    [xt_.b], [P.D("HNT", r)])
        P.barrier()

    def load_xg(xg, xt_ap, f0, KC, grows):
        for (r0, n, j) in runs_of(grows):
            dma(xg.ap[:, :, j * 64:(j + n) * 64],
                dap(xt_ap, f0 * NSLOT + r0 * 64, [[NSLOT, 128], [128 * NSLOT, KC], [1, 64 * n]]),
                P.Ds(xt_ap.tensor.name, range(r0, r0 + n)), [xg.b])

    def lin_b(groups, xt_ap, KC, wspecs, nfeat, epi, FB=512):
        A.reset()
        TMAX = max(len(g) for g in groups) * 64
        nxg = 2 if KC * TMAX * 2 * 2 <= 70 * 1024 else 1
        xg = [T([128, KC, TMAX], BF16) for _ in range(nxg)]
        wbk = [[T([128, KC, FB], BF16) for _ in range(2)] for _ in wspecs]
        st = epi("alloc", None)
        tasks = [(gi, fb) for gi in range(len(groups)) for fb in range(nfeat // FB)]

        def loadx(ti):
            gi, fb = tasks[ti]
            if fb == 0:
                load_xg(xg[gi % nxg], xt_ap, 0, KC, groups[gi])

        def loads(ti):
            gi, fb = tasks[ti]
            for si, (wname, dep, base, ldw, col0) in enumerate(wspecs):
                w_ = wbk[si][ti % 2]
                dma(w_.ap, dap(wb[wname], base + col0 + fb * FB, [[ldw, 128], [128 * ldw, KC], [1, FB]]),
                    wdep(wname, *dep), [w_.b], qn="act")

        loadx(0)
        loads(0)
        for ti, (gi, fb) in enumerate(tasks):
            if ti + 1 < len(tasks):
                if nxg == 2:
                    loadx(ti + 1)
                loads(ti + 1)
            grows = groups[gi]
            xg_ = xg[gi % nxg]
            Tn = len(grows) * 64
            ws = [wbk[si][ti % 2] for si in range(len(wspecs))]
            for ftl in range(FB // 128):
                ft = fb * (FB // 128) + ftl
                for h0 in range(0, Tn, 512):
                    n = min(512, Tn - h0)
                    bks = []
                    for w_ in ws:
                        bk = next_bank()
                        bks.append(bk)
                        for kc in range(KC):
                            P.op("pe", lambda e, bk=bk, w_=w_, kc=kc, ftl=ftl, xg_=xg_, h0=h0, n=n: e.matmul(
                                psum[bk][:, 0:n], w_.ap[:, kc, ftl * 128:(ftl + 1) * 128], xg_.ap[:, kc, h0:h0 + n],
                                start=(kc == 0), stop=(kc == KC - 1)), [w_.b, xg_.b], [PSB[bk]])
                    epi("run", (st, ft, bks, n, grows[h0 // 64:(h0 + n) // 64]))
            if ti + 1 < len(tasks) and nxg == 1:
                loadx(ti + 1)
        P.barrier()

    def store_fm(dst_ap, dst_name, f0, stg, rows_half):
        for (r0, n, j) in runs_of(rows_half):
            dma(dap(dst_ap, f0 * NSLOT + r0 * 64, [[NSLOT, 128], [1, 64 * n]]), stg.ap[:, j * 64:(j + n) * 64],
                [stg.b], P.Ds(dst_name, range(r0, r0 + n)))

    def lin_a(groups, xt_ap, KC, wname, dep, base, ldw, col0, ncols, NB, epi, nxg=2):
        A.reset()
        TMAX = max(len(g) for g in groups) * 64
        xg = [T([128, KC, TMAX], BF16) for _ in range(nxg)]
        wbk = [T([128, KC, NB], BF16) for _ in range(2)]
        st = epi("alloc", None)
        tasks = [(gi, nb) for gi in range(len(groups)) for nb in range(ncols // NB)]
        tbase = [sum(len(g) for g in groups[:gi]) // 2 for gi in range(len(groups))]
        seq = [(ti, t) for ti, (gi, nb) in enumerate(tasks) for t in range(len(groups[gi]) // 2)]

        def loadx(ti):
            gi, nb = tasks[ti]
            if nb == 0:
                load_xg(xg[gi % nxg], xt_ap, 0, KC, groups[gi])

        def loads(ti):
            gi, nb = tasks[ti]
            w_ = wbk[ti % 2]
            dma(w_.ap, dap(wb[wname], base + col0 + nb * NB, [[ldw, 128], [128 * ldw, KC], [1, NB]]),
                wdep(wname, *dep), [w_.b], qn="act")

        def eload(k):
            if k < len(seq):
                ti, t = seq[k]
                gi, nb = tasks[ti]
                grows = groups[gi]
                epi("load", (st, nb, (grows[2 * t], grows[2 * t + 1]), k))

        loadx(0)
        loads(0)
        eload(0)
        eload(1)
        k = 0
        for ti, (gi, nb) in enumerate(tasks):
            if ti + 1 < len(tasks):
                if nxg == 2:
                    loadx(ti + 1)
                loads(ti + 1)
            grows = groups[gi]
            xg_ = xg[gi % nxg]
            w_ = wbk[ti % 2]
            for t in range(len(grows) // 2):
                eload(k + 2)
                bk = next_bank()
                for kc in range(KC):
                    P.op("pe", lambda e, bk=bk, w_=w_, kc=kc, xg_=xg_, t=t: e.matmul(
                        psum[bk][:, 0:NB], xg_.ap[:, kc, t * 128:(t + 1) * 128], w_.ap[:, kc, :],
                        start=(kc == 0), stop=(kc == KC - 1)), [w_.b, xg_.b], [PSB[bk]])
                epi("run", (st, nb, bk, (grows[2 * t], grows[2 * t + 1]), tbase[gi] + t, k))
                k += 1
            if ti + 1 < len(tasks) and nxg == 1:
                loadx(ti + 1)
        P.barrier()

    def epi_qk(mode, a):
        if mode == "alloc":
            return [T([128, 512], BF16) for _ in range(3)], [0]
        (stgs, cnt), ft, bks, n, rows_half = a
        stg = stgs[cnt[0] % 3]
        cnt[0] += 1
        sc = 0.125 if ft < 8 else 1.0
        if cnt[0] % 2:
            P.op("act", lambda e: e.activation(out=stg.ap[:, 0:n], in_=psum[bks[0]][:, 0:n], func=AF.Copy, scale=sc),
                 [PSB[bks[0]]], [stg.b])
        else:
            P.op("dve", lambda e: e.tensor_scalar(out=stg.ap[:, 0:n], in0=psum[bks[0]][:, 0:n], scalar1=sc, scalar2=None,
                                                  op0=ALU.mult), [PSB[bks[0]]], [stg.b])
        store_fm(QKT, "QKT", ft * 128, stg, rows_half)

    def epi_u(mode, a):
        if mode == "alloc":
            return [T([128, 512], F32) for _ in range(3)], [0]
        (stgs, cnt), ft, bks, n, rows_half = a
        stg = stgs[cnt[0] % 3]
        cnt[0] += 1
        if cnt[0] % 2:
            P.op("act", lambda e: e.activation(out=stg.ap[:, 0:n], in_=psum[bks[0]][:, 0:n], func=AF.Copy),
                 [PSB[bks[0]]], [stg.b])
        else:
            P.op("dve", lambda e: e.tensor_copy(out=stg.ap[:, 0:n], in_=psum[bks[0]][:, 0:n]), [PSB[bks[0]]], [stg.b])
        store_fm(UT, "UT", ft * 128, stg, rows_half)

    def epi_gu(mode, a):
        if mode == "alloc":
            return [T([128, 512], BF16) for _ in range(3)], [T([128, 512], F32) for _ in range(2)], [0]
        (stgs, tmps, cnt), ft, bks, n, rows_half = a
        stg = stgs[cnt[0] % 3]
        tmp = tmps[cnt[0] % 2]
        cnt[0] += 1
        P.op("act", lambda e: e.activation(out=tmp.ap[:, 0:n], in_=psum[bks[0]][:, 0:n], func=AF.Silu),
             [PSB[bks[0]]], [tmp.b])
        P.op("dve", lambda e: e.tensor_tensor(out=stg.ap[:, 0:n], in0=psum[bks[1]][:, 0:n], in1=tmp.ap[:, 0:n], op=ALU.mult),
             [PSB[bks[1]], tmp.b], [stg.b])
        store_fm(ACTT, "ACTT", ft * 128, stg, rows_half)

    def epi_v(mode, a):
        if mode == "alloc":
            vs = [T([128, 8, 65], BF16) for _ in range(3)]
            for v_ in vs:
                P.op("dve", lambda e, v_=v_: e.memset(v_.ap, 1.0), [], [v_.b])
            return vs, [0]
        if mode == "load":
            return
        (vs, cnt), nb, bk, (ra, rb), tix, kk = a
        v_ = vs[cnt[0] % 3]
        cnt[0] += 1
        src = psum[bk][:, :].rearrange("p (a b) -> p a b", b=64)
        if cnt[0] % 2:
            P.op("act", lambda e: e.activation(out=v_.ap[:, :, 0:64], in_=src, func=AF.Copy), [PSB[bk]], [v_.b])
        else:
            P.op("dve", lambda e: e.tensor_copy(out=v_.ap[:, :, 0:64], in_=src), [PSB[bk]], [v_.b])
        for j, r in enumerate((ra, rb)):
            dma(VA[r * 64:(r + 1) * 64, nb * 520:(nb + 1) * 520],
                v_.ap[j * 64:(j + 1) * 64, :, :].rearrange("p a b -> p (a b)"), [v_.b], [P.D("VA", r)])

    def make_epi_res(src, src_name, NB, comb_e=None, tile_base=0):
        def epi(mode, a):
            if mode == "alloc":
                return [T([128, NB], F32) for _ in range(4)], [T([128, NB], F32) for _ in range(3)]
            if mode == "load":
                (hr, os_), nb, (ra, rb), kk = a
                h_ = hr[kk % 4]
                for j, r in enumerate((ra, rb)):
                    dma(h_.ap[j * 64:(j + 1) * 64, :], src[r * 64:(r + 1) * 64, nb * NB:(nb + 1) * NB],
                        [P.D(src_name, (r, nb) if src_name == "H" else r)], [h_.b])
                return
            (hr, os_), nb, bk, (ra, rb), tix, kk = a
            h_ = hr[kk % 4]
            o_ = os_[kk % 3]
            if comb_e is None:
                P.op("dve", lambda e: e.tensor_tensor(out=o_.ap, in0=psum[bk][:, 0:NB], in1=h_.ap, op=ALU.add),
                     [PSB[bk], h_.b], [o_.b])
            else:
                P.op("dve", lambda e: e.scalar_tensor_tensor(out=o_.ap, in0=psum[bk][:, 0:NB],
                                                             scalar=COMB.ap[:, tix, comb_e:comb_e + 1], in1=h_.ap,
                                                             op0=ALU.mult, op1=ALU.add), [PSB[bk], h_.b, COMB.b], [o_.b])
            for j, r in enumerate((ra, rb)):
                dma(H[r * 64:(r + 1) * 64, nb * NB:(nb + 1) * NB], o_.ap[j * 64:(j + 1) * 64, :], [o_.b],
                    [P.D("H", (r, nb))])
        return epi

    def Hrow_bufs(r, nblk):
        return [P.D("H", (r, k)) for k in range(nblk)]

    def phase_att(layer):
        A.reset()
        Tb = T([128, 16, 14, 64], BF16)
        MB = T([128, 16, 64], BF16)
        P.op("dve", lambda e: e.memset(MB.ap, 0.0), [], [MB.b])
        zt = T([1, 128], F32)
        P.op("dve", lambda e: e.memset(zt.ap, 0.0), [], [zt.b])
        dma(RPAD[0:1, 0:64], zt.ap[0:1, 0:64], [zt.b], [P.D("RPAD")])
        dma(RPAD[0:1, 64 + 7440:64 + 7440 + 128], zt.ap[0:1, 0:128], [zt.b], [P.D("RPAD")])
        dma(RPAD[0:1, 64:64 + 7440], rpb[layer:layer + 1, :], [], [P.D("RPAD")])
        mbt = T([16, 16], F32)
        dma(mbt.ap, mbT[layer, :, :], [], [mbt.b])
        P.op("dve", lambda e: e.tensor_copy(out=MB.ap[0:16, :, :], in_=mbt.ap.unsqueeze(2).to_broadcast([16, 16, 64])), [mbt.b], [MB.b])
        mark = A.off
        hk = [T([128, 4, 14, 64], F32) for _ in range(2)]
        bd = [T([128, 4, 14, 128], BF16) for _ in range(2)]
        for b_ in bd:
            P.op("dve", lambda e, b_=b_: e.memset(b_.ap, 0.0), [], [b_.b])
        for hg in range(4):
            hk_, bd_ = hk[hg % 2], bd[hg % 2]
            for kh in range(2):
                for hl in range(4):
                    dma(hk_.ap[kh * 64:(kh + 1) * 64, hl, :, :],
                        dap(RPAD, 64 + (hg * 4 + hl) * 465 + kh * 31 - 48, [[1, 64], [31, 14], [1, 64]]),
                        [P.D("RPAD")], [hk_.b])
                P.op("dve", lambda e, kh=kh, hk_=hk_, bd_=bd_: e.tensor_copy(
                    out=bd_.ap[kh * 64:(kh + 1) * 64, :, :, kh * 64:(kh + 1) * 64], in_=hk_.ap[kh * 64:(kh + 1) * 64, :, :, :]),
                    [hk_.b], [bd_.b])
            for hl in range(4):
                for e0 in (0, 7):
                    bk = next_bank()
                    for ei in range(7):
                        P.op("pe", lambda e, bk=bk, hl=hl, ee=e0 + ei, ei=ei, bd_=bd_: e.matmul(
                            psum[bk][:, ei * 64:(ei + 1) * 64], bd_.ap[:, hl, ee, :], jj.ap, start=True, stop=True),
                            [bd_.b, jj.b], [PSB[bk]])
                    P.op("dve", lambda e, bk=bk, h=hg * 4 + hl, e0=e0: e.tensor_tensor(
                        out=Tb.ap[:, h, e0:e0 + 7, :], in0=psum[bk][:, 0:448].rearrange("p (a b) -> p a b", b=64),
                        in1=colmask.ap.unsqueeze(1).to_broadcast([128, 7, 64]), op=ALU.add),
                        [PSB[bk], colmask.b], [Tb.b])
        P.barrier()
        A.off = mark
        NKMAX = 12 * 64
        qt = [T([128, 8, 64], BF16) for _ in range(2)]
        qa = [[T([128, 8, 64], BF16) for _ in range(2)] for _ in range(2)]
        ktw = [T([128, 8, NKMAX], BF16) for _ in range(2)]
        vw = [T([128, 6, 1040], BF16) for _ in range(2)]
        ktm = {r: T([128, 8, 128], BF16) for r in (0, ROW_PM)}
        vm = {r: T([128, 1040], BF16) for r in (0, ROW_PM)}
        pt = [T([128, 512], BF16) for _ in range(8)]
        rec = [T([64, 8], F32) for _ in range(2)]
        oa = [T([64, 16, 64], F32) for _ in range(2)]
        for p_ in pt:
            P.op("dve", lambda e, p_=p_: e.memset(p_.ap, 0.0), [], [p_.b])
        for i_ in range(2):
            for par in range(2):
                q_ = qa[par][i_]
                P.op("dve", lambda e, q_=q_: e.memset(q_.ap, 0.0), [], [q_.b])
        for r in (0, ROW_PM):
            P.op("dve", lambda e, r=r: e.memset(ktm[r].ap, 0.0), [], [ktm[r].b])
            P.op("dve", lambda e, r=r: e.memset(vm[r].ap, 0.0), [], [vm[r].b])
            dma(ktm[r].ap[:, :, 0:16], dap(QKT, 1024 * NSLOT + r * 64 + 48, [[NSLOT, 128], [128 * NSLOT, 8], [1, 16]]),
                [P.D("QKT", r)], [ktm[r].b])
            dma(vm[r].ap[0:16, :], VA[r * 64 + 48:r * 64 + 64, :], [P.D("VA", r)], [vm[r].b])
        sb_rr = [0]
        pt_rr = [0]
        ob_rr = [0]
        SBANKS = (0, 1, 2, 3)
        OBANKS = ((4, 5), (6, 7))
        plan_l = [x for x in PLAN if x[0] == layer]

        def ld(it):
            (ly, row, st, nb, mrow, mcol) = plan_l[it]
            p = it % 2
            qt_, kt_, vw_ = qt[p], ktw[p], vw[p]
            dma(qt_.ap, dap(QKT, row * 64, [[NSLOT, 128], [128 * NSLOT, 8], [1, 64]]), [P.D("QKT", row)], [qt_.b])
            if nb > 0:
                dma(kt_.ap[:, :, 0:nb * 128], dap(QKT, 1024 * NSLOT + st * 64, [[NSLOT, 128], [128 * NSLOT, 8], [1, nb * 128]]),
                    P.Ds("QKT", range(st, st + 2 * nb)), [kt_.b])
                dma(vw_.ap[:, 0:nb, :], dap(VA, st * 64 * 1040, [[1040, 128], [128 * 1040, nb], [1, 1040]]),
                    P.Ds("VA", range(st, st + 2 * nb)), [vw_.b])

        ld(0)
        for it, (ly, row, st, nb, mrow, mcol) in enumerate(plan_l):
            p = it % 2
            qt_, kt_, vw_, oa_ = qt[p], ktw[p], vw[p], oa[p]
            if it + 1 < len(plan_l):
                ld(it + 1)
            for par in range(2):
                q_ = qa[par][p]
                P.op("dve", lambda e, q_=q_, qt_=qt_, par=par: e.tensor_copy(
                    out=q_.ap[par * 64:(par + 1) * 64, :, :], in_=qt_.ap[par * 64:(par + 1) * 64, :, :]), [qt_.b], [q_.b])
            for par in range(2):
                q_ = qa[par][p]
                pts = []
                for b in range(nb + 1):
                    ismeta = (b == nb)
                    bk = SBANKS[sb_rr[0] % 4]
                    sb_rr[0] += 1
                    p_ = pt[pt_rr[0] % 8]
                    pt_rr[0] += 1
                    pts.append(p_)
                    if ismeta:
                        P.op("pe", lambda e, bk=bk, par=par: e.matmul(
                            psum[bk][0:16, :], ident.ap[:, 0:16], MB.ap[:, par::2, :], start=True, stop=False),
                            [ident.b, MB.b], [PSB[bk]])
                        for hp in range(8):
                            P.op("pe", lambda e, bk=bk, hp=hp, q_=q_, mrow=mrow: e.matmul(
                                psum[bk][0:16, hp * 64:(hp + 1) * 64], ktm[mrow].ap[:, hp, 0:16], q_.ap[:, hp, :],
                                start=False, stop=(hp == 7)), [ktm[mrow].b, q_.b], [PSB[bk]])
                        P.op("act", lambda e, bk=bk, p_=p_: e.activation(
                            out=p_.ap[0:16, :], in_=psum[bk][0:16, :], func=AF.Exp, bias=zero_c.ap[0:16, :], scale=1.0),
                            [PSB[bk], zero_c.b], [p_.b])
                    else:
                        e_idx = (st + 2 * b) - row + 7
                        P.op("pe", lambda e, bk=bk, par=par, e_idx=e_idx: e.matmul(
                            psum[bk][:, :], ident.ap, Tb.ap[:, par::2, e_idx, :], start=True, stop=False),
                            [ident.b, Tb.b], [PSB[bk]])
                        for hp in range(8):
                            P.op("pe", lambda e, bk=bk, hp=hp, q_=q_, kt_=kt_, b=b: e.matmul(
                                psum[bk][:, hp * 64:(hp + 1) * 64], kt_.ap[:, hp, b * 128:(b + 1) * 128], q_.ap[:, hp, :],
                                start=False, stop=(hp == 7)), [kt_.b, q_.b], [PSB[bk]])
                        P.op("act", lambda e, bk=bk, p_=p_, c=mcol + b: e.activation(
                            out=p_.ap, in_=psum[bk][:, :], func=AF.Exp, bias=rowmask.ap[:, c:c + 1], scale=1.0),
                            [PSB[bk], rowmask.b], [p_.b])
                ob = OBANKS[ob_rr[0] % 2]
                ob_rr[0] += 1
                for hp in range(8):
                    h = 2 * hp + par
                    obk = ob[hp // 4]
                    for b in range(nb + 1):
                        ismeta = (b == nb)
                        p_ = pts[b]
                        if ismeta:
                            P.op("pe", lambda e, obk=obk, hp=hp, h=h, p_=p_, b=b, mrow=mrow: e.matmul(
                                psum[obk][0:64, (hp % 4) * 65:(hp % 4 + 1) * 65], p_.ap[:, hp * 64:(hp + 1) * 64],
                                vm[mrow].ap[:, h * 65:(h + 1) * 65], start=(b == 0), stop=True),
                                [p_.b, vm[mrow].b], [PSB[obk]])
                        else:
                            P.op("pe", lambda e, obk=obk, hp=hp, h=h, p_=p_, b=b, vw_=vw_: e.matmul(
                                psum[obk][0:64, (hp % 4) * 65:(hp % 4 + 1) * 65], p_.ap[:, hp * 64:(hp + 1) * 64],
                                vw_.ap[:, b, h * 65:(h + 1) * 65], start=(b == 0), stop=False),
                                [p_.b, vw_.b], [PSB[obk]])
                rc = rec[par]
                for half in range(2):
                    obk = ob[half]
                    ov = psum[obk][0:64, 0:260].rearrange("p (a b) -> p a b", b=65)
                    P.op("dve", lambda e, ov=ov, rc=rc, half=half: e.reciprocal(
                        out=rc.ap[:, half * 4:(half + 1) * 4].unsqueeze(2), in_=ov[:, :, 64:65]), [PSB[obk]], [rc.b])
                    h0 = par + 8 * half
                    P.op("dve", lambda e, ov=ov, rc=rc, half=half, oa_=oa_, h0=h0: e.tensor_tensor(
                        out=oa_.ap[:, h0:h0 + 7:2, :], in0=ov[:, :, 0:64],
                        in1=rc.ap[:, half * 4:(half + 1) * 4].unsqueeze(2).to_broadcast([64, 4, 64]), op=ALU.mult),
                        [PSB[obk], rc.b], [oa_.b])
            dma(OA[row * 64:(row + 1) * 64, :], oa_.ap.rearrange("p a b -> p (a b)"), [oa_.b], [P.D("OA", row)])
        P.barrier()

    def phase_pool():
        A.reset()
        W_ = NSLOT + 32
        vt = T([128, NSLOT], F32)
        dma(vt.ap, t_valid.to_broadcast([128, NSLOT]), [], [vt.b])
        ic = T([128, NSLOT], F32)
        ut = [T([128, W_], F32) for _ in range(2)]
        sa = T([128, W_], F32)
        sb = T([128, W_], F32)
        plt = [T([128, NSLOT], BF16) for _ in range(2)]
        for t_ in ut + [sa, sb]:
            P.op("dve", lambda e, t_=t_: e.memset(t_.ap, 0.0), [], [t_.b])
        allrows = range(NR)
        for c in range(8):
            g = c // 2
            u_ = ut[c % 2]
            pl_ = plt[c % 2]
            if c % 2 == 0:
                dma(ic.ap, t_invcnt[g:g + 1, :].to_broadcast([128, NSLOT]), [], [ic.b])
            dma(u_.ap[:, 16:16 + NSLOT], UT[c * 128:(c + 1) * 128, :], P.Ds("UT", allrows), [u_.b])
            P.op("dve", lambda e, u_=u_: e.tensor_tensor(out=sa.ap[:, 16:16 + NSLOT], in0=u_.ap[:, 16:16 + NSLOT], in1=vt.ap,
                                                         op=ALU.mult), [u_.b, vt.b], [sa.b])
            lo, hi = 8, W_ - 8
            P.op("dve", lambda e: e.tensor_tensor(out=sb.ap[:, lo:hi], in0=sa.ap[:, lo - 1:hi - 1], in1=sa.ap[:, lo:hi], op=ALU.add),
                 [sa.b], [sb.b])
            cur, oth = sb, sa
            for k in range(g):
                sh = 1 << k
                P.op("dve", lambda e, cur=cur, oth=oth, sh=sh: e.tensor_tensor(
                    out=oth.ap[:, lo:hi], in0=cur.ap[:, lo - sh:hi - sh], in1=cur.ap[:, lo + sh:hi + sh], op=ALU.add),
                    [cur.b], [oth.b])
                cur, oth = oth, cur
            P.op("dve", lambda e, cur=cur, oth=oth: e.tensor_tensor(out=oth.ap[:, 16:16 + NSLOT], in0=cur.ap[:, 16:16 + NSLOT],
                                                                    in1=ic.ap, op=ALU.mult), [cur.b, ic.b], [oth.b])
            P.op("dve", lambda e, oth=oth, u_=u_, pl_=pl_: e.tensor_tensor(
                out=pl_.ap, in0=oth.ap[:, 16:16 + NSLOT], in1=u_.ap[:, 16:16 + NSLOT], op=ALU.subtract),
                [oth.b, u_.b], [pl_.b])
            dma(PLT[c * 128:(c + 1) * 128, :], pl_.ap, [pl_.b], P.Ds("PLT", allrows))
        P.barrier()

    def phase_mo(layer, rows):
        A.reset()
        ga = T([128, 1024], F32); gp = T([128, 1024], F32); psc = T([128, 1024], F32)
        dma(ga.ap, g_attn[layer:layer + 1, :].to_broadcast([128, 1024]), [], [ga.b])
        dma(gp.ap, g_pool[layer:layer + 1, :].to_broadcast([128, 1024]), [], [gp.b])
        dma(psc.ap, pscale[layer:layer + 1, :].to_broadcast([128, 1024]), [], [psc.b])
        pw = T([128, 4, 2, 256], BF16)
        dma(pw.ap, dap(wb["pool_w"], layer * 4 * 65536, [[256, 128], [65536, 4], [128 * 256, 2], [1, 256]]),
            wdep("pool_w", layer * 128, layer * 128 + 128), [pw.b])
        oat = [T([128, 1024], F32) for _ in range(2)]
        plt = [T([128, 8, 128], BF16) for _ in range(2)]
        mx = [T([128, 1024], F32) for _ in range(2)]
        on = [T([128, D], BF16) for _ in range(2)]
        ot = [T([128, 16, 128], BF16) for _ in range(2)]
        junk = T([128, 1024], BF16)
        ss = [T([128, 2], F32) for _ in range(2)]
        rs = [T([128, 2], F32) for _ in range(2)]
        tiles = [(rows[i], rows[i + 1]) for i in range(0, len(rows), 2)]
        def ld(ti):
            oa_, pl_ = oat[ti % 2], plt[ti % 2]
            for j, r in enumerate(tiles[ti]):
                dma(oa_.ap[j * 64:(j + 1) * 64, :], OA[r * 64:(r + 1) * 64, :], [P.D("OA", r)], [oa_.b])
                dma(pl_.ap[:, :, j * 64:(j + 1) * 64], dap(PLT, r * 64, [[NSLOT, 128], [128 * NSLOT, 8], [1, 64]]),
                    [P.D("PLT", r)], [pl_.b])

        ld(0)
        for ti, (ra, rb) in enumerate(tiles):
            p = ti % 2
            oa_, pl_, mx_, on_, ot_, ss_, rs_ = oat[p], plt[p], mx[p], on[p], ot[p], ss[p], rs[p]
            if ti + 1 < len(tiles):
                ld(ti + 1)
            bk2 = [next_bank(), next_bank()]
            for g in range(4):
                bk = bk2[g // 2]
                for cc in range(2):
                    P.op("pe", lambda e, bk=bk, g=g, cc=cc, pl_=pl_: e.matmul(
                        psum[bk][:, (g % 2) * 256:(g % 2 + 1) * 256], pl_.ap[:, 2 * g + cc, :], pw.ap[:, g, cc, :],
                        start=(cc == 0), stop=(cc == 1)), [pl_.b, pw.b], [PSB[bk]])
            for hf in range(2):
                P.op("dve", lambda e, hf=hf, mx_=mx_, bk=bk2[hf]: e.tensor_tensor(
                    out=mx_.ap[:, hf * 512:(hf + 1) * 512], in0=psum[bk][:, :], in1=psc.ap[:, hf * 512:(hf + 1) * 512], op=ALU.mult),
                    [PSB[bk2[hf]], psc.b], [mx_.b])
            P.op("act", lambda e, oa_=oa_, ss_=ss_: e.activation(out=junk.ap, in_=oa_.ap, func=AF.Square, accum_out=ss_.ap[:, 0:1]),
                 [oa_.b], [junk.b, ss_.b])
            P.op("act", lambda e, mx_=mx_, ss_=ss_: e.activation(out=junk.ap, in_=mx_.ap, func=AF.Square, accum_out=ss_.ap[:, 1:2]),
                 [mx_.b], [junk.b, ss_.b])
            P.op("act", lambda e, ss_=ss_, rs_=rs_: e.activation(out=rs_.ap, in_=ss_.ap, func=AF.Sqrt, bias=eps_c.ap,
                                                                 scale=1.0 / 1024), [ss_.b, eps_c.b], [rs_.b])
            P.op("dve", lambda e, rs_=rs_: e.reciprocal(out=rs_.ap, in_=rs_.ap), [rs_.b], [rs_.b])
            P.op("dve", lambda e, oa_=oa_, rs_=rs_, on_=on_: e.scalar_tensor_tensor(
                out=on_.ap[:, 0:1024], in0=oa_.ap, scalar=rs_.ap[:, 0:1], in1=ga.ap, op0=ALU.mult, op1=ALU.mult),
                [oa_.b, rs_.b, ga.b], [on_.b])
            P.op("dve", lambda e, mx_=mx_, rs_=rs_, on_=on_: e.scalar_tensor_tensor(
                out=on_.ap[:, 1024:2048], in0=mx_.ap, scalar=rs_.ap[:, 1:2], in1=gp.ap, op0=ALU.mult, op1=ALU.mult),
                [mx_.b, rs_.b, gp.b], [on_.b])
            bks = [next_bank(), next_bank()]
            for kc in range(16):
                bk = bks[kc // 8]
                P.op("pe", lambda e, on_=on_, kc=kc, bk=bk: e.transpose(
                    out=psum[bk][:, :].bitcast(BF16)[:, (kc % 8) * 128:(kc % 8 + 1) * 128],
                    in_=on_.ap[:, kc * 128:(kc + 1) * 128], identity=ident.ap), [on_.b, ident.b], [PSB[bk]])
            P.op("act", lambda e, ot_=ot_, bk=bks[0]: e.activation(
                out=ot_.ap[:, 0:8, :], in_=psum[bk][:, :].bitcast(BF16).rearrange("p (a b) -> p a b", b=128), func=AF.Copy),
                [PSB[bks[0]]], [ot_.b])
            P.op("dve", lambda e, ot_=ot_, bk=bks[1]: e.tensor_copy(
                out=ot_.ap[:, 8:16, :], in_=psum[bk][:, :].bitcast(BF16).rearrange("p (a b) -> p a b", b=128)),
                [PSB[bks[1]]], [ot_.b])
            for j, r in enumerate((ra, rb)):
                dma(dap(OT, r * 64, [[NSLOT, 128], [128 * NSLOT, 16], [1, 64]]), ot_.ap[:, :, j * 64:(j + 1) * 64],
                    [ot_.b], [P.D("OT", r)])
        P.barrier()

    def phase_final():
        A.reset()
        gt = T([128, D], F32)
        dma(gt.ap, g_final.to_broadcast([128, D]), [], [gt.b])
        hb = [T([128, D], F32) for _ in range(2)]
        ob = [T([128, D], F32) for _ in range(2)]
        junk = T([128, D], BF16)
        ss = [T([128, 1], F32) for _ in range(2)]
        rs = [T([128, 1], F32) for _ in range(2)]
        tiles = [(L1_OUT[i], L1_OUT[i + 1]) for i in range(0, len(L1_OUT), 2)]
        def ld(ti):
            h_ = hb[ti % 2]
            for j, r in enumerate(tiles[ti]):
                dma(h_.ap[j * 64:(j + 1) * 64, :], H[r * 64:(r + 1) * 64, :], [P.D("H", r)], [h_.b])

        ld(0)
        for ti, (ra, rb) in enumerate(tiles):
            p = ti % 2
            h_, o_, ss_, rs_ = hb[p], ob[p], ss[p], rs[p]
            if ti + 1 < len(tiles):
                ld(ti + 1)
            P.op("act", lambda e, h_=h_, ss_=ss_: e.activation(out=junk.ap, in_=h_.ap, func=AF.Square, accum_out=ss_.ap),
                 [h_.b], [junk.b, ss_.b])
            P.op("act", lambda e, ss_=ss_, rs_=rs_: e.activation(out=rs_.ap, in_=ss_.ap, func=AF.Sqrt, bias=eps_c.ap,
                                                                 scale=1.0 / D), [ss_.b, eps_c.b], [rs_.b])
            P.op("dve", lambda e, rs_=rs_: e.reciprocal(out=rs_.ap, in_=rs_.ap), [rs_.b], [rs_.b])
            P.op("dve", lambda e, h_=h_, rs_=rs_, o_=o_: e.scalar_tensor_tensor(
                out=o_.ap, in0=h_.ap, scalar=rs_.ap, in1=gt.ap, op0=ALU.mult, op1=ALU.mult), [h_.b, rs_.b, gt.b], [o_.b])
            for j, r in enumerate((ra, rb)):
                if r <= 32:
                    dst = ys[(r - 1) * 64:r * 64, :]
                else:
                    dst = yp[(r - 43) * 64:(r - 42) * 64, :]
                dma(dst, o_.ap[j * 64:(j + 1) * 64, :], [o_.b], [])
        P.barrier()

    def mixer(layer, kv_rows, out_rows, src, src_name):
        WB = layer * D * 4096
        dep_in = (layer * WIN_R, (layer + 1) * WIN_R)
        phase_nt(kv_rows, src, src_name, ln_mix[layer:layer + 1, :])
        if stop_here(f"nt{layer}"): return True
        gk = groups_of(kv_rows)
        lin_b(gk, HNT, 16, [("w_in", dep_in, WB, 4096, 0)], 2048, epi_qk)
        lin_a(gk, HNT, 16, "w_in", dep_in, WB, 4096, 2048, 1024, 512, epi_v)
        lin_b(gk, HNT, 16, [("w_in", dep_in, WB, 4096, 3072)], 1024, epi_u)
        if stop_here(f"qkvu{layer}"): return True
        phase_att(layer)
        if stop_here(f"att{layer}"): return True
        phase_pool()
        if stop_here(f"pool{layer}"): return True
        orows = out_rows if len(out_rows) % 2 == 0 else out_rows + [65]
        phase_mo(layer, orows)
        if stop_here(f"mo{layer}"): return True
        lin_a(groups_of(orows), OT, 16, "w_out", (layer * D, (layer + 1) * D), layer * D * D, D, 0, D, 512,
              make_epi_res(src, src_name, 512))
        if stop_here(f"wout{layer}"): return True
        return False

    def run_all():
        if mixer(0, L0_ALL, L0_OUT, xs, "xs"): return
        phase_nt(L0_OUT, H, "H4", ln_ffn[0:1, :])
        g0 = groups_of(L0_OUT)
        lin_b(g0, HNT, 16, [("ffn_g", (0, DFF), 0, DFF, 0), ("ffn_u", (0, DFF), 0, DFF, 0)], DFF, epi_gu)
        lin_a(groups_of(L0_OUT, 10), ACTT, 44, "ffn_d", (0, DFF), 0, D, 0, D, 256, make_epi_res(H, "H", 256), nxg=2)
        if stop_here("ffn0"): return
        if mixer(1, L1_KV, L1_OUT, H, "H8"): return
        phase_nt(L1_OUT, H, "H4", ln_ffn[1:2, :], router=True)
        g1 = groups_of(L1_OUT)
        for ex_ in range(NE):
            dep = (ex_ * DFF, (ex_ + 1) * DFF)
            lin_b(g1, HNT, 16, [("moe_g", dep, ex_ * D * DFF, DFF, 0), ("moe_u", dep, ex_ * D * DFF, DFF, 0)], DFF, epi_gu)
            lin_a(groups_of(L1_OUT, 10), ACTT, 44, "moe_d", dep, ex_ * DFF * D, D, 0, D, 256,
                  make_epi_res(H, "H", 256, comb_e=ex_), nxg=2)
        phase_final()

    run_all()
    P.barrier()

    with nc.Block() as block:
        @block.sync
        def _(e):
            P.emit("sp", e)

        @block.gpsimd
        def _(e):
            P.emit("pool", e)

        @block.scalar
        def _(e):
            P.emit("act", e)

        @block.vector
        def _(e):
            P.emit("dve", e)

        @block.tensor
        def _(e):
            P.emit("pe", e)
    es.close()
    return nc


def make_in_maps(inp):
    f = np.float32
    xp = np.asarray(inp["x_prompt"], f)[0]
    xsm = np.asarray(inp["x_sample"], f)
    meta = np.asarray(inp["meta_tokens"], f)
    shared = {
        "w_in": np.ascontiguousarray(np.asarray(inp["w_in"], f)).reshape(-1, 2048),
        "w_out": np.ascontiguousarray(np.asarray(inp["w_out"], f)).reshape(-1, 2048),
        "pool_w": np.ascontiguousarray(np.asarray(inp["pool_w"], f)).reshape(-1, 2048),
        "ffn_g": np.ascontiguousarray(np.asarray(inp["ffn_w_gate"], f)).reshape(-1, 2048),
        "ffn_u": np.ascontiguousarray(np.asarray(inp["ffn_w_up"], f)).reshape(-1, 2048),
        "ffn_d": np.ascontiguousarray(np.asarray(inp["ffn_w_down"], f)).reshape(-1, 2048),
        "moe_g": np.ascontiguousarray(np.asarray(inp["moe_w_gate"], f)).reshape(-1, 2048),
        "moe_u": np.ascontiguousarray(np.asarray(inp["moe_w_up"], f)).reshape(-1, 2048),
        "moe_d": np.ascontiguousarray(np.asarray(inp["moe_w_down"], f)).reshape(-1, 2048),
        "rpb": np.ascontiguousarray(np.asarray(inp["rpb"], f)).reshape(2, -1),
        "mbT": np.ascontiguousarray(np.transpose(np.asarray(inp["meta_bias"], f), (0, 2, 1))),
        "ln_mix": np.asarray(inp["ln_mix"], f), "ln_ffn": np.asarray(inp["ln_ffn"], f),
        "g_attn": np.asarray(inp["g_attn_out"], f), "g_pool": np.asarray(inp["g_pool_out"], f),
        "pscale": np.asarray(inp["pool_scale"], f),
        "g_final": np.asarray(inp["g_final"], f).reshape(1, D),
        "routerT": np.ascontiguousarray(np.asarray(inp["router"], f)[0].T),
    }
    maps = []
    for c in range(8):
        x = np.zeros((NSLOT, D), f)
        x[48:64] = meta
        x[64:64 + 2048] = xsm[c]
        x[ROW_PM * 64 + 48:ROW_PM * 64 + 64] = meta
        x[34 * 64:35 * 64] = xp[0:64]
        for i in range(30):
            r = 16 * c - 8 + i
            b0 = (ROW_P0 + i) * 64
            if 0 <= r < 128:
                x[b0:b0 + 64] = xp[r * 64:(r + 1) * 64]
            elif r == -1:
                x[b0 + 48:b0 + 64] = meta
        m = dict(shared)
        m["xs"] = x
        m.update(host_tables(c))
        maps.append(m)
    return maps


_NC = None


def kernel(**inputs):
    global _NC
    if _NC is None:
        _NC = build()
    maps = make_in_maps(inputs)
    res = run_bass_kernel_spmd(_NC, maps, core_ids=list(range(8)))
    ysamp = np.stack([np.asarray(res.results[c]["ys"], np.float32) for c in range(8)], axis=0)
    yprm = np.concatenate([np.asarray(res.results[c]["yp"], np.float32) for c in range(8)], axis=0)[None]
    return (yprm, ysamp)
```

```python
import numpy as np
import concourse.bass as bass
import concourse.mybir as mybir
from concourse.bass_utils import run_bass_kernel_spmd

F32, BF16 = mybir.dt.float32, mybir.dt.bfloat16
AF = mybir.ActivationFunctionType
ALU = mybir.AluOpType

D = 2048
DFF = 5632
NE = 8
NR = 66
NSLOT = NR * 64
NEG = -30000.0
EPS = 1e-6
ROW_PM = 33
ROW_P0 = 35
DBG = {"stop": None, "dump": []}

L0_ALL = list(range(66))
L0_OUT = list(range(0, 34)) + list(range(39, 62)) + [65]
L1_KV = L0_OUT
L1_OUT = list(range(1, 33)) + list(range(43, 59))


def groups_of(rows, g=16):
    return [rows[i:i + g] for i in range(0, len(rows), g)]


def runs_of(rows):
    out = []
    for j, r in enumerate(rows):
        if out and out[-1][0] + out[-1][1] == r:
            out[-1][1] += 1
        else:
            out.append([r, 1, j])
    return [tuple(x) for x in out]


def att_plan():
    plan = []
    col = 2
    for layer in range(2):
        rows = []
        if layer == 0:
            rows += [0]
        rows += list(range(1, 33))
        if layer == 0:
            rows += [ROW_PM]
        prange = range(4, 27) if layer == 0 else range(8, 24)
        rows += [ROW_P0 + i for i in prange]
        for row in rows:
            if row == 0 or row == ROW_PM:
                plan.append((layer, row, 0, 0, row, col))
                continue
            if row <= 32:
                g = row - 1
                r0 = min(max(g - 4, 0), 24)
                plan.append((layer, row, 1 + r0, 4, 0, col))
                col += 4
                continue
            i = row - ROW_P0
            st, nb = i - 4, 4
            if i in (8, 9):
                nb = 6
            elif i in (10, 11):
                nb = 5
            elif i in (21, 22):
                st, nb = 16, 5
            elif i == 23:
                st, nb = 16, 6
            plan.append((layer, row, ROW_P0 + st, nb, ROW_PM, col))
            col += nb
    return plan, col


PLAN, NMASK = att_plan()


def host_tables(c):
    rowmask = np.zeros((128, NMASK), np.float32)
    rowmask[16:, 1] = NEG
    for (layer, row, st, nb, mrow, col) in PLAN:
        if row < ROW_P0:
            continue
        i = row - ROW_P0
        r = 16 * c - 8 + i
        for b in range(nb):
            for kh in range(2):
                j = st - ROW_P0 + 2 * b + kh
                rk = 16 * c - 8 + j
                if r < 0 or r > 127:
                    ok = not (c == 0 and r == -1)
                else:
                    r0 = min(max(r - 4, 0), 120)
                    ok = (r0 <= rk <= r0 + 7)
                if not ok:
                    rowmask[kh * 64:(kh + 1) * 64, col + b] = NEG
    valid = np.zeros((NSLOT,), np.float32)
    pos = np.zeros((NSLOT,), np.int64)
    slen = np.ones((NSLOT,), np.int64)
    sl = np.arange(64)
    valid[48:64] = 1; pos[48:64] = np.arange(16); slen[0:64] = 2064
    for k in range(1, 33):
        valid[k * 64:(k + 1) * 64] = 1
        pos[k * 64:(k + 1) * 64] = 16 + (k - 1) * 64 + sl
        slen[k * 64:(k + 1) * 64] = 2064
    LP = 16 + 8192
    b0 = ROW_PM * 64
    valid[b0 + 48:b0 + 64] = 1; pos[b0 + 48:b0 + 64] = np.arange(16); slen[b0:b0 + 64] = LP
    b0 = 34 * 64
    valid[b0:b0 + 64] = 1; pos[b0:b0 + 64] = 16 + sl; slen[b0:b0 + 64] = LP
    for i in range(30):
        r = 16 * c - 8 + i
        b0 = (ROW_P0 + i) * 64
        slen[b0:b0 + 64] = LP
        if 0 <= r < 128:
            valid[b0:b0 + 64] = 1
            pos[b0:b0 + 64] = 16 + r * 64 + sl
        elif r == -1:
            valid[b0 + 48:b0 + 64] = 1
            pos[b0 + 48:b0 + 64] = np.arange(16)
    invcnt = np.zeros((4, NSLOT), np.float32)
    for g, w in enumerate((2, 4, 8, 16)):
        lo = np.maximum(pos - w // 2, 0)
        hi = np.minimum(pos + w // 2, slen)
        cnt = np.where(valid > 0, hi - lo, w)
        invcnt[g] = 1.0 / cnt.astype(np.float32)
    colmask = np.zeros((128, 64), np.float32)
    for qc in range(64):
        cs = min(max(qc - 8, 0), 48)
        for kc in range(64):
            if not (cs <= kc < cs + 16):
                colmask[kc, qc] = NEG
                colmask[64 + kc, qc] = NEG
    jj = np.zeros((128, 64), np.float32)
    for q in range(64):
        jj[q, 63 - q] = 1.0
        jj[64 + q, 63 - q] = 1.0
    return dict(rowmask=rowmask, valid=valid.reshape(1, NSLOT), invcnt=invcnt, colmask=colmask, jj=jj,
                ident=np.eye(128, dtype=np.float32))


class Buf:
    __slots__ = ("w", "r")

    def __init__(self):
        self.w = None
        self.r = {}


class Q:
    def __init__(self, name, csem, dsems):
        self.name, self.csem, self.dsems = name, csem, dsems
        self.ccnt = 0
        self.dcnt = [0] * len(dsems)
        self.i = 0
        self.ops = []
        self.known = {}


class Prog:
    def __init__(self, nc, es):
        self.nc = nc
        self.q = {}
        for name, ndma, comp in (("sp", 8, False), ("pool", 2, False), ("act", 6, True), ("dve", 0, True), ("pe", 0, True)):
            dsems = [es.enter_context(nc.semaphore(f"d_{name}{k}")) for k in range(ndma)]
            csem = es.enter_context(nc.semaphore(f"c_{name}")) if comp else None
            self.q[name] = Q(name, csem, dsems)
        self.dbufs = {}
        self.allbufs = []

    def buf(self, persistent=False):
        b = Buf()
        if not persistent:
            self.allbufs.append(b)
        return b

    def D(self, name, key=0):
        k = (name, key)
        b = self.dbufs.get(k)
        if b is None:
            b = self.dbufs[k] = self.buf()
        return b

    def Ds(self, name, keys):
        return [self.D(name, k) for k in keys]

    def op(self, qn, fn, reads=(), writes=(), dma=None):
        q = self.q[qn]
        if dma is None:
            dma = q.csem is None
        waits = {}

        def need(ev):
            if ev is None:
                return
            sem, val = ev
            if qn == "pe" and sem is q.csem:
                return
            k = id(sem)
            if k not in waits or waits[k][1] < val:
                waits[k] = (sem, val)

        for b in reads:
            need(b.w)
        for b in writes:
            need(b.w)
            for ev in b.r.values():
                need(ev)
        if dma:
            k = q.i % len(q.dsems)
            q.i += 1
            sem = q.dsems[k]
            if q.dcnt[k] > 0:
                need((sem, q.dcnt[k]))
            q.dcnt[k] += 16
            ev = (sem, q.dcnt[k])
            inc = 16
        else:
            sem = q.csem
            q.ccnt += 1
            ev = (sem, q.ccnt)
            inc = 1
        wl = []
        for k2, (s_, v) in waits.items():
            if q.known.get(k2, 0) >= v:
                continue
            q.known[k2] = v
            wl.append((s_, v))
        q.ops.append((fn, wl, sem, inc))
        for b in reads:
            k3 = id(sem)
            if k3 not in b.r or b.r[k3][1] < ev[1]:
                b.r[k3] = ev
        for b in writes:
            b.w = ev
            b.r = {}
        return ev

    def barrier(self):
        evs = []
        for q in self.q.values():
            if q.name == "pool":
                continue
            if q.csem is not None and q.ccnt > 0:
                evs.append((q.csem, q.ccnt))
            for k, s_ in enumerate(q.dsems):
                if q.dcnt[k] > 0:
                    evs.append((s_, q.dcnt[k]))
        for q in self.q.values():
            if q.name == "pool":
                continue
            for (s_, v) in evs:
                if q.known.get(id(s_), 0) < v:
                    q.known[id(s_)] = v
                    q.ops.append((None, [(s_, v)], None, 0))
        for b in self.allbufs:
            b.w = None
            b.r = {}

    def emit(self, qn, e):
        q = self.q[qn]
        for fn, wl, sem, inc in q.ops:
            for s_, v in wl:
                e.wait_ge(s_, v)
            if fn is not None:
                fn(e).then_inc(sem, inc)
        if qn == "sp":
            for k, s_ in enumerate(q.dsems):
                if q.dcnt[k] > 0:
                    e.wait_ge(s_, q.dcnt[k])


class Arena:
    def __init__(self, ap_u8, nbytes):
        self.ap, self.n = ap_u8, nbytes
        self.base = 0
        self.off = 0

    def persist(self):
        self.base = self.off

    def reset(self):
        self.off = self.base

    def alloc(self, shape, dt):
        esz = 4 if dt == F32 else 2
        n = int(np.prod(shape[1:]))
        nb = (n * esz + 63) // 64 * 64
        assert self.off + nb <= self.n, f"SBUF arena overflow {self.off}+{nb}>{self.n}"
        a = self.ap[0:shape[0], self.off:self.off + nb].bitcast(dt)[:, 0:n]
        self.off += nb
        if len(shape) == 3:
            a = a.rearrange("p (a b) -> p a b", b=shape[2])
        elif len(shape) == 4:
            a = a.rearrange("p (a b c) -> p a b c", b=shape[2], c=shape[3])
        return a


def dap(t, off, dims):
    return bass.AP(tensor=t.tensor, offset=off, ap=[[int(s), int(n)] for s, n in dims])


def build():
    from contextlib import ExitStack
    nc = bass.Bass("TRN2", target_bir_lowering=False)
    es = ExitStack()
    es.enter_context(nc.allow_low_precision("bf16 matmul operands, fp32 accumulate"))
    es.enter_context(nc.allow_non_contiguous_dma(reason="small strided param loads"))

    def din(name, shape, dt=F32):
        return nc.dram_tensor(name, list(shape), dt, kind="ExternalInput").ap()

    def dscr(name, shape, dt):
        kind = "ExternalOutput" if name in DBG["dump"] else "Internal"
        return nc.dram_tensor(name, list(shape), dt, kind=kind).ap()

    xs = din("xs", [NSLOT, D])
    wsrc = {
        "w_in": din("w_in", [2 * D * 4096 // 2048, 2048]),
        "w_out": din("w_out", [2 * D, 2048]),
        "pool_w": din("pool_w", [2 * 4 * 256 * 256 // 2048, 2048]),
        "ffn_g": din("ffn_g", [DFF, 2048]), "ffn_u": din("ffn_u", [DFF, 2048]), "ffn_d": din("ffn_d", [DFF, 2048]),
        "moe_g": din("moe_g", [NE * DFF, 2048]), "moe_u": din("moe_u", [NE * DFF, 2048]),
        "moe_d": din("moe_d", [NE * DFF, 2048]),
    }
    rpb = din("rpb", [2, 16 * 15 * 31])
    mbT = din("mbT", [2, 16, 16])
    ln_mix = din("ln_mix", [2, D]); ln_ffn = din("ln_ffn", [2, D])
    g_attn = din("g_attn", [2, 1024]); g_pool = din("g_pool", [2, 1024]); pscale = din("pscale", [2, 1024])
    g_final = din("g_final", [1, D])
    routerT = din("routerT", [NE, D])
    t_rowmask = din("rowmask", [128, NMASK]); t_valid = din("valid", [1, NSLOT]); t_invcnt = din("invcnt", [4, NSLOT])
    t_colmask = din("colmask", [128, 64]); t_jj = din("jj", [128, 64]); t_ident = din("ident", [128, 128])
    ys = nc.dram_tensor("ys", [2048, D], F32, kind="ExternalOutput").ap()
    yp = nc.dram_tensor("yp", [1024, D], F32, kind="ExternalOutput").ap()

    wb = {k: dscr(k + "_b", list(v.shape), BF16) for k, v in wsrc.items()}
    H = dscr("H", [NSLOT, D], F32)
    HNT = dscr("HNT", [D, NSLOT], BF16)
    QKT = dscr("QKT", [D, NSLOT], BF16)
    VA = dscr("VA", [NSLOT, 1040], BF16)
    UT = dscr("UT", [1024, NSLOT], F32)
    PLT = dscr("PLT", [1024, NSLOT], BF16)
    OA = dscr("OA", [NSLOT, 1024], F32)
    OT = dscr("OT", [D, NSLOT], BF16)
    ACTT = dscr("ACTT", [DFF, NSLOT], BF16)
    RPAD = dscr("RPAD", [1, 64 + 7440 + 128], F32)

    ARENA_BYTES = 188 * 1024
    arena_t = es.enter_context(nc.sbuf_tensor("arena", [128, ARENA_BYTES], mybir.dt.uint8))
    A = Arena(arena_t[:, :], ARENA_BYTES)
    psum = [es.enter_context(nc.psum_tensor(f"ps{i}", [128, 512], F32)) for i in range(8)]
    P = Prog(nc, es)
    PSB = [P.buf() for _ in range(8)]

    class T:
        def __init__(self, shape, dt):
            self.ap = A.alloc(shape, dt)
            self.b = P.buf()

    def dma(out, in_, reads, writes, qn="sp"):
        return P.op(qn, lambda e: e.dma_start(out=out, in_=in_), reads, writes, dma=True)

    ident_f = T([128, 128], F32); ident = T([128, 128], BF16)
    jj_f = T([128, 64], F32); jj = T([128, 64], BF16)
    colmask = T([128, 64], F32)
    rowmask = T([128, NMASK], F32)
    COMB = T([128, 24, 8], F32)
    zero_c = T([128, 1], F32)
    dma(ident_f.ap, t_ident, [], [ident_f.b]); dma(jj_f.ap, t_jj, [], [jj_f.b])
    dma(colmask.ap, t_colmask, [], [colmask.b]); dma(rowmask.ap, t_rowmask, [], [rowmask.b])
    P.op("dve", lambda e: e.tensor_copy(out=ident.ap, in_=ident_f.ap), [ident_f.b], [ident.b])
    P.op("dve", lambda e: e.tensor_copy(out=jj.ap, in_=jj_f.ap), [jj_f.b], [jj.b])
    P.op("dve", lambda e: e.memset(zero_c.ap, 0.0), [], [zero_c.b])
    eps_c = T([128, 1], F32)
    P.op("dve", lambda e: e.memset(eps_c.ap, EPS), [], [eps_c.b])
    A.persist()

    WBUFS = {}

    def cast_rows(name, r0, r1):
        CH = 512
        lst = WBUFS.setdefault((name, r0, r1), [])
        for a in range(r0, r1, CH):
            b_ = min(a + CH, r1)
            bb = P.buf(persistent=True)
            lst.append(bb)
            dma(wb[name][a:b_, :], wsrc[name][a:b_, :], [], [bb], qn="pool")

    def wdep(name, r0, r1):
        return WBUFS[(name, r0, r1)]

    WIN_R = D * 4096 // 2048
    cast_rows("w_in", 0, WIN_R); cast_rows("pool_w", 0, 128); cast_rows("w_out", 0, D)
    cast_rows("ffn_g", 0, DFF); cast_rows("ffn_u", 0, DFF); cast_rows("ffn_d", 0, DFF)
    cast_rows("w_in", WIN_R, 2 * WIN_R); cast_rows("pool_w", 128, 256); cast_rows("w_out", D, 2 * D)
    for e_ in range(NE):
        for nm in ("moe_g", "moe_u", "moe_d"):
            cast_rows(nm, e_ * DFF, (e_ + 1) * DFF)

    bank_rr = [0]

    def next_bank():
        i = bank_rr[0] % 8
        bank_rr[0] += 1
        return i

    def stop_here(tag):
        return DBG["stop"] == tag

    def phase_nt(rows, src, src_name, gain_ap, router=False):
        A.reset()
        gt = T([128, D], F32)
        dma(gt.ap, gain_ap.to_broadcast([128, D]), [], [gt.b])
        hb = [T([128, D], F32) for _ in range(2)]
        hnb = [T([128, D], BF16) for _ in range(2)]
        xt = [T([128, 16, 128], BF16) for _ in range(2)]
        junk = T([128, D], BF16)
        ss = [T([128, 1], F32) for _ in range(2)]
        rs = [T([128, 1], F32) for _ in range(2)]
        if router:
            rt = T([128, NE, D], F32)
            dma(rt.ap, dap(routerT, 0, [[0, 128], [D, NE], [1, D]]), [], [rt.b])
            hnf = T([128, D], F32)
            junk2 = T([128, D], F32)
            lg = T([128, 8], F32); m8 = T([128, 8], F32); nv1 = T([128, 1], F32)
            ex = T([128, 8], F32); mk = T([128, 8], F32); num = T([128, 8], F32); den = T([128, 1], F32)
        tiles = [(rows[i], rows[i + 1]) for i in range(0, len(rows), 2)]
        def ld(ti):
            ra, rb = tiles[ti]
            h_ = hb[ti % 2]
            dma(h_.ap[0:64, :], src[ra * 64:(ra + 1) * 64, :], [P.D(src_name, ra)], [h_.b])
            dma(h_.ap[64:128, :], src[rb * 64:(rb + 1) * 64, :], [P.D(src_name, rb)], [h_.b])

        ld(0)
        for ti, (ra, rb) in enumerate(tiles):
            p = ti % 2
            h_, hn_, xt_, ss_, rs_ = hb[p], hnb[p], xt[p], ss[p], rs[p]
            if ti + 1 < len(tiles):
                ld(ti + 1)
            P.op("act", lambda e, h_=h_, ss_=ss_: e.activation(out=junk.ap, in_=h_.ap, func=AF.Square, accum_out=ss_.ap),
                 [h_.b], [junk.b, ss_.b])
            P.op("act", lambda e, ss_=ss_, rs_=rs_: e.activation(out=rs_.ap, in_=ss_.ap, func=AF.Sqrt, bias=eps_c.ap,
                                                                 scale=1.0 / D), [ss_.b, eps_c.b], [rs_.b])
            P.op("dve", lambda e, rs_=rs_: e.reciprocal(out=rs_.ap, in_=rs_.ap), [rs_.b], [rs_.b])
            if not router:
                P.op("dve", lambda e, h_=h_, rs_=rs_, hn_=hn_: e.scalar_tensor_tensor(
                    out=hn_.ap, in0=h_.ap, scalar=rs_.ap, in1=gt.ap, op0=ALU.mult, op1=ALU.mult),
                    [h_.b, rs_.b, gt.b], [hn_.b])
            else:
                P.op("dve", lambda e, h_=h_, rs_=rs_: e.scalar_tensor_tensor(
                    out=hnf.ap, in0=h_.ap, scalar=rs_.ap, in1=gt.ap, op0=ALU.mult, op1=ALU.mult),
                    [h_.b, rs_.b, gt.b], [hnf.b])
                P.op("act", lambda e, hn_=hn_: e.activation(out=hn_.ap, in_=hnf.ap, func=AF.Copy), [hnf.b], [hn_.b])
                for ex_ in range(NE):
                    P.op("dve", lambda e, ex_=ex_: e.scalar_tensor_tensor(
                        out=junk2.ap, in0=hnf.ap, scalar=1.0, in1=rt.ap[:, ex_, :], op0=ALU.mult, op1=ALU.mult,
                        accum_out=lg.ap[:, ex_:ex_ + 1]), [hnf.b, rt.b], [junk2.b, lg.b])
                P.op("dve", lambda e: e.max(out=m8.ap, in_=lg.ap), [lg.b], [m8.b])
                P.op("dve", lambda e: e.tensor_scalar(out=nv1.ap, in0=m8.ap[:, 0:1], scalar1=-1.0, scalar2=None, op0=ALU.mult),
                     [m8.b], [nv1.b])
                P.op("act", lambda e: e.activation(out=ex.ap, in_=lg.ap, func=AF.Exp, bias=nv1.ap, scale=1.0),
                     [lg.b, nv1.b], [ex.b])
                P.op("dve", lambda e: e.tensor_scalar(out=mk.ap, in0=lg.ap, scalar1=m8.ap[:, 1:2], scalar2=None, op0=ALU.is_ge),
                     [lg.b, m8.b], [mk.b])
                P.op("dve", lambda e: e.scalar_tensor_tensor(out=num.ap, in0=ex.ap, scalar=1.0, in1=mk.ap, op0=ALU.mult,
                                                             op1=ALU.mult, accum_out=den.ap), [ex.b, mk.b], [num.b, den.b])
                P.op("dve", lambda e: e.reciprocal(out=den.ap, in_=den.ap), [den.b], [den.b])
                P.op("dve", lambda e, ti=ti: e.tensor_scalar(out=COMB.ap[:, ti, :], in0=num.ap, scalar1=den.ap, scalar2=None,
                                                             op0=ALU.mult), [num.b, den.b], [COMB.b])
            bks = [next_bank(), next_bank()]
            for kc in range(16):
                bk = bks[kc // 8]
                P.op("pe", lambda e, hn_=hn_, kc=kc, bk=bk: e.transpose(
                    out=psum[bk][:, :].bitcast(BF16)[:, (kc % 8) * 128:(kc % 8 + 1) * 128],
                    in_=hn_.ap[:, kc * 128:(kc + 1) * 128], identity=ident.ap), [hn_.b, ident.b], [PSB[bk]])
            P.op("act", lambda e, xt_=xt_, bk=bks[0]: e.activation(
                out=xt_.ap[:, 0:8, :], in_=psum[bk][:, :].bitcast(BF16).rearrange("p (a b) -> p a b", b=128), func=AF.Copy),
                [PSB[bks[0]]], [xt_.b])
            P.op("dve", lambda e, xt_=xt_, bk=bks[1]: e.tensor_copy(
                out=xt_.ap[:, 8:16, :], in_=psum[bk][:, :].bitcast(BF16).rearrange("p (a b) -> p a b", b=128)),
                [PSB[bks[1]]], [xt_.b])
            for j, r in enumerate((ra, rb)):
                dma(dap(HNT, r * 64, [[NSLOT, 128], [128 * NSLOT, 16], [1, 64]]), xt_.ap[:, :, j * 64:(j + 1) * 64],
                    [xt_.b], [P.D("HNT", r)])
        P.barrier()

    def load_xg(xg, xt_ap, f0, KC, grows):
        for (r0, n, j) in runs_of(grows):
            dma(xg.ap[:, :, j * 64:(j + n) * 64],
                dap(xt_ap, f0 * NSLOT + r0 * 64, [[NSLOT, 128], [128 * NSLOT, KC], [1, 64 * n]]),
                P.Ds(xt_ap.tensor.name, range(r0, r0 + n)), [xg.b])

    def lin_b(groups, xt_ap, KC, wspecs, nfeat, epi, FB=512):
        A.reset()
        TMAX = max(len(g) for g in groups) * 64
        nxg = 2 if KC * TMAX * 2 * 2 <= 70 * 1024 else 1
        xg = [T([128, KC, TMAX], BF16) for _ in range(nxg)]
        wbk = [[T([128, KC, FB], BF16) for _ in range(2)] for _ in wspecs]
        st = epi("alloc", None)
        tasks = [(gi, fb) for gi in range(len(groups)) for fb in range(nfeat // FB)]

        def loadx(ti):
            gi, fb = tasks[ti]
            if fb == 0:
                load_xg(xg[gi % nxg], xt_ap, 0, KC, groups[gi])

        def loads(ti):
            gi, fb = tasks[ti]
            for si, (wname, dep, base, ldw, col0) in enumerate(wspecs):
                w_ = wbk[si][ti % 2]
                dma(w_.ap, dap(wb[wname], base + col0 + fb * FB, [[ldw, 128], [128 * ldw, KC], [1, FB]]),
                    wdep(wname, *dep), [w_.b], qn="act")

        loadx(0)
        loads(0)
        for ti, (gi, fb) in enumerate(tasks):
            if ti + 1 < len(tasks):
                if nxg == 2:
                    loadx(ti + 1)
                loads(ti + 1)
            grows = groups[gi]
            xg_ = xg[gi % nxg]
            Tn = len(grows) * 64
            ws = [wbk[si][ti % 2] for si in range(len(wspecs))]
            for ftl in range(FB // 128):
                ft = fb * (FB // 128) + ftl
                for h0 in range(0, Tn, 512):
                    n = min(512, Tn - h0)
                    bks = []
                    for w_ in ws:
                        bk = next_bank()
                        bks.append(bk)
                        for kc in range(KC):
                            P.op("pe", lambda e, bk=bk, w_=w_, kc=kc, ftl=ftl, xg_=xg_, h0=h0, n=n: e.matmul(
                                psum[bk][:, 0:n], w_.ap[:, kc, ftl * 128:(ftl + 1) * 128], xg_.ap[:, kc, h0:h0 + n],
                                start=(kc == 0), stop=(kc == KC - 1)), [w_.b, xg_.b], [PSB[bk]])
                    epi("run", (st, ft, bks, n, grows[h0 // 64:(h0 + n) // 64]))
            if ti + 1 < len(tasks) and nxg == 1:
                loadx(ti + 1)
        P.barrier()

    def store_fm(dst_ap, dst_name, f0, stg, rows_half):
        for (r0, n, j) in runs_of(rows_half):
            dma(dap(dst_ap, f0 * NSLOT + r0 * 64, [[NSLOT, 128], [1, 64 * n]]), stg.ap[:, j * 64:(j + n) * 64],
                [stg.b], P.Ds(dst_name, range(r0, r0 + n)))

    def lin_a(groups, xt_ap, KC, wname, dep, base, ldw, col0, ncols, NB, epi, nxg=2):
        A.reset()
        TMAX = max(len(g) for g in groups) * 64
        xg = [T([128, KC, TMAX], BF16) for _ in range(nxg)]
        wbk = [T([128, KC, NB], BF16) for _ in range(2)]
        st = epi("alloc", None)
        tasks = [(gi, nb) for gi in range(len(groups)) for nb in range(ncols // NB)]
        tbase = [sum(len(g) for g in groups[:gi]) // 2 for gi in range(len(groups))]
        seq = [(ti, t) for ti, (gi, nb) in enumerate(tasks) for t in range(len(groups[gi]) // 2)]

        def loadx(ti):
            gi, nb = tasks[ti]
            if nb == 0:
                load_xg(xg[gi % nxg], xt_ap, 0, KC, groups[gi])

        def loads(ti):
            gi, nb = tasks[ti]
            w_ = wbk[ti % 2]
            dma(w_.ap, dap(wb[wname], base + col0 + nb * NB, [[ldw, 128], [128 * ldw, KC], [1, NB]]),
                wdep(wname, *dep), [w_.b], qn="act")

        def eload(k):
            if k < len(seq):
                ti, t = seq[k]
                gi, nb = tasks[ti]
                grows = groups[gi]
                epi("load", (st, nb, (grows[2 * t], grows[2 * t + 1]), k))

        loadx(0)
        loads(0)
        eload(0)
        eload(1)
        k = 0
        for ti, (gi, nb) in enumerate(tasks):
            if ti + 1 < len(tasks):
                if nxg == 2:
                    loadx(ti + 1)
                loads(ti + 1)
            grows = groups[gi]
            xg_ = xg[gi % nxg]
            w_ = wbk[ti % 2]
            for t in range(len(grows) // 2):
                eload(k + 2)
                bk = next_bank()
                for kc in range(KC):
                    P.op("pe", lambda e, bk=bk, w_=w_, kc=kc, xg_=xg_, t=t: e.matmul(
                        psum[bk][:, 0:NB], xg_.ap[:, kc, t * 128:(t + 1) * 128], w_.ap[:, kc, :],
                        start=(kc == 0), stop=(kc == KC - 1)), [w_.b, xg_.b], [PSB[bk]])
                epi("run", (st, nb, bk, (grows[2 * t], grows[2 * t + 1]), tbase[gi] + t, k))
                k += 1
            if ti + 1 < len(tasks) and nxg == 1:
                loadx(ti + 1)
        P.barrier()

    def epi_qk(mode, a):
        if mode == "alloc":
            return [T([128, 512], BF16) for _ in range(3)], [0]
        (stgs, cnt), ft, bks, n, rows_half = a
        stg = stgs[cnt[0] % 3]
        cnt[0] += 1
        sc = 0.125 if ft < 8 else 1.0
        if cnt[0] % 2:
            P.op("act", lambda e: e.activation(out=stg.ap[:, 0:n], in_=psum[bks[0]][:, 0:n], func=AF.Copy, scale=sc),
                 [PSB[bks[0]]], [stg.b])
        else:
            P.op("dve", lambda e: e.tensor_scalar(out=stg.ap[:, 0:n], in0=psum[bks[0]][:, 0:n], scalar1=sc, scalar2=None,
                                                  op0=ALU.mult), [PSB[bks[0]]], [stg.b])
        store_fm(QKT, "QKT", ft * 128, stg, rows_half)

    def epi_u(mode, a):
        if mode == "alloc":
            return [T([128, 512], F32) for _ in range(3)], [0]
        (stgs, cnt), ft, bks, n, rows_half = a
        stg = stgs[cnt[0] % 3]
        cnt[0] += 1
        if cnt[0] % 2:
            P.op("act", lambda e: e.activation(out=stg.ap[:, 0:n], in_=psum[bks[0]][:, 0:n], func=AF.Copy),
                 [PSB[bks[0]]], [stg.b])
        else:
            P.op("dve", lambda e: e.tensor_copy(out=stg.ap[:, 0:n], in_=psum[bks[0]][:, 0:n]), [PSB[bks[0]]], [stg.b])
        store_fm(UT, "UT", ft * 128, stg, rows_half)

    def epi_gu(mode, a):
        if mode == "alloc":
            return [T([128, 512], BF16) for _ in range(3)], [T([128, 512], F32) for _ in range(2)], [0]
        (stgs, tmps, cnt), ft, bks, n, rows_half = a
        stg = stgs[cnt[0] % 3]
        tmp = tmps[cnt[0] % 2]
        cnt[0] += 1
        P.op("act", lambda e: e.activation(out=tmp.ap[:, 0:n], in_=psum[bks[0]][:, 0:n], func=AF.Silu),
             [PSB[bks[0]]], [tmp.b])
        P.op("dve", lambda e: e.tensor_tensor(out=stg.ap[:, 0:n], in0=psum[bks[1]][:, 0:n], in1=tmp.ap[:, 0:n], op=ALU.mult),
             [PSB[bks[1]], tmp.b], [stg.b])
        store_fm(ACTT, "ACTT", ft * 128, stg, rows_half)

    def epi_v(mode, a):
        if mode == "alloc":
            vs = [T([128, 8, 65], BF16) for _ in range(3)]
            for v_ in vs:
                P.op("dve", lambda e, v_=v_: e.memset(v_.ap, 1.0), [], [v_.b])
            return vs, [0]
        if mode == "load":
            return
        (vs, cnt), nb, bk, (ra, rb), tix, kk = a
        v_ = vs[cnt[0] % 3]
        cnt[0] += 1
        src = psum[bk][:, :].rearrange("p (a b) -> p a b", b=64)
        if cnt[0] % 2:
            P.op("act", lambda e: e.activation(out=v_.ap[:, :, 0:64], in_=src, func=AF.Copy), [PSB[bk]], [v_.b])
        else:
            P.op("dve", lambda e: e.tensor_copy(out=v_.ap[:, :, 0:64], in_=src), [PSB[bk]], [v_.b])
        for j, r in enumerate((ra, rb)):
            dma(VA[r * 64:(r + 1) * 64, nb * 520:(nb + 1) * 520],
                v_.ap[j * 64:(j + 1) * 64, :, :].rearrange("p a b -> p (a b)"), [v_.b], [P.D("VA", r)])

    def make_epi_res(src, src_name, NB, comb_e=None, tile_base=0):
        def epi(mode, a):
            if mode == "alloc":
                return [T([128, NB], F32) for _ in range(4)], [T([128, NB], F32) for _ in range(3)]
            if mode == "load":
                (hr, os_), nb, (ra, rb), kk = a
                h_ = hr[kk % 4]
                for j, r in enumerate((ra, rb)):
                    dma(h_.ap[j * 64:(j + 1) * 64, :], src[r * 64:(r + 1) * 64, nb * NB:(nb + 1) * NB],
                        [P.D(src_name, (r, nb) if src_name == "H" else r)], [h_.b])
                return
            (hr, os_), nb, bk, (ra, rb), tix, kk = a
            h_ = hr[kk % 4]
            o_ = os_[kk % 3]
            if comb_e is None:
                P.op("dve", lambda e: e.tensor_tensor(out=o_.ap, in0=psum[bk][:, 0:NB], in1=h_.ap, op=ALU.add),
                     [PSB[bk], h_.b], [o_.b])
            else:
                P.op("dve", lambda e: e.scalar_tensor_tensor(out=o_.ap, in0=psum[bk][:, 0:NB],
                                                             scalar=COMB.ap[:, tix, comb_e:comb_e + 1], in1=h_.ap,
                                                             op0=ALU.mult, op1=ALU.add), [PSB[bk], h_.b, COMB.b], [o_.b])
            for j, r in enumerate((ra, rb)):
                dma(H[r * 64:(r + 1) * 64, nb * NB:(nb + 1) * NB], o_.ap[j * 64:(j + 1) * 64, :], [o_.b],
                    [P.D("H", (r, nb))])
        return epi

    def Hrow_bufs(r, nblk):
        return [P.D("H", (r, k)) for k in range(nblk)]

    def phase_att(layer):
        A.reset()
        Tb = T([128, 16, 14, 64], BF16)
        MB = T([128, 16, 64], BF16)
        P.op("dve", lambda e: e.memset(MB.ap, 0.0), [], [MB.b])
        zt = T([1, 128], F32)
        P.op("dve", lambda e: e.memset(zt.ap, 0.0), [], [zt.b])
        dma(RPAD[0:1, 0:64], zt.ap[0:1, 0:64], [zt.b], [P.D("RPAD")])
        dma(RPAD[0:1, 64 + 7440:64 + 7440 + 128], zt.ap[0:1, 0:128], [zt.b], [P.D("RPAD")])
        dma(RPAD[0:1, 64:64 + 7440], rpb[layer:layer + 1, :], [], [P.D("RPAD")])
        mbt = T([16, 16], F32)
        dma(mbt.ap, mbT[layer, :, :], [], [mbt.b])
        P.op("dve", lambda e: e.tensor_copy(out=MB.ap[0:16, :, :], in_=mbt.ap.unsqueeze(2).to_broadcast([16, 16, 64])), [mbt.b], [MB.b])
        mark = A.off
        hk = [T([128, 4, 14, 64], F32) for _ in range(2)]
        bd = [T([128, 4, 14, 128], BF16) for _ in range(2)]
        for b_ in bd:
            P.op("dve", lambda e, b_=b_: e.memset(b_.ap, 0.0), [], [b_.b])
        for hg in range(4):
            hk_, bd_ = hk[hg % 2], bd[hg % 2]
            for kh in range(2):
                for hl in range(4):
                    dma(hk_.ap[kh * 64:(kh + 1) * 64, hl, :, :],
                        dap(RPAD, 64 + (hg * 4 + hl) * 465 + kh * 31 - 48, [[1, 64], [31, 14], [1, 64]]),
                        [P.D("RPAD")], [hk_.b])
                P.op("dve", lambda e, kh=kh, hk_=hk_, bd_=bd_: e.tensor_copy(
                    out=bd_.ap[kh * 64:(kh + 1) * 64, :, :, kh * 64:(kh + 1) * 64], in_=hk_.ap[kh * 64:(kh + 1) * 64, :, :, :]),
                    [hk_.b], [bd_.b])
            for hl in range(4):
                for e0 in (0, 7):
                    bk = next_bank()
                    for ei in range(7):
                        P.op("pe", lambda e, bk=bk, hl=hl, ee=e0 + ei, ei=ei, bd_=bd_: e.matmul(
                            psum[bk][:, ei * 64:(ei + 1) * 64], bd_.ap[:, hl, ee, :], jj.ap, start=True, stop=True),
                            [bd_.b, jj.b], [PSB[bk]])
                    P.op("dve", lambda e, bk=bk, h=hg * 4 + hl, e0=e0: e.tensor_tensor(
                        out=Tb.ap[:, h, e0:e0 + 7, :], in0=psum[bk][:, 0:448].rearrange("p (a b) -> p a b", b=64),
                        in1=colmask.ap.unsqueeze(1).to_broadcast([128, 7, 64]), op=ALU.add),
                        [PSB[bk], colmask.b], [Tb.b])
        P.barrier()
        A.off = mark
        NKMAX = 12 * 64
        qt = [T([128, 8, 64], BF16) for _ in range(2)]
        qa = [[T([128, 8, 64], BF16) for _ in range(2)] for _ in range(2)]
        ktw = [T([128, 8, NKMAX], BF16) for _ in range(2)]
        vw = [T([128, 6, 1040], BF16) for _ in range(2)]
        ktm = {r: T([128, 8, 128], BF16) for r in (0, ROW_PM)}
        vm = {r: T([128, 1040], BF16) for r in (0, ROW_PM)}
        pt = [T([128, 512], BF16) for _ in range(8)]
        rec = [T([64, 8], F32) for _ in range(2)]
        oa = [T([64, 16, 64], F32) for _ in range(2)]
        for p_ in pt:
            P.op("dve", lambda e, p_=p_: e.memset(p_.ap, 0.0), [], [p_.b])
        for i_ in range(2):
            for par in range(2):
                q_ = qa[par][i_]
                P.op("dve", lambda e, q_=q_: e.memset(q_.ap, 0.0), [], [q_.b])
        for r in (0, ROW_PM):
            P.op("dve", lambda e, r=r: e.memset(ktm[r].ap, 0.0), [], [ktm[r].b])
            P.op("dve", lambda e, r=r: e.memset(vm[r].ap, 0.0), [], [vm[r].b])
            dma(ktm[r].ap[:, :, 0:16], dap(QKT, 1024 * NSLOT + r * 64 + 48, [[NSLOT, 128], [128 * NSLOT, 8], [1, 16]]),
                [P.D("QKT", r)], [ktm[r].b])
            dma(vm[r].ap[0:16, :], VA[r * 64 + 48:r * 64 + 64, :], [P.D("VA", r)], [vm[r].b])
        sb_rr = [0]
        pt_rr = [0]
        ob_rr = [0]
        SBANKS = (0, 1, 2, 3)
        OBANKS = ((4, 5), (6, 7))
        plan_l = [x for x in PLAN if x[0] == layer]

        def ld(it):
            (ly, row, st, nb, mrow, mcol) = plan_l[it]
            p = it % 2
            qt_, kt_, vw_ = qt[p], ktw[p], vw[p]
            dma(qt_.ap, dap(QKT, row * 64, [[NSLOT, 128], [128 * NSLOT, 8], [1, 64]]), [P.D("QKT", row)], [qt_.b])
            if nb > 0:
                dma(kt_.ap[:, :, 0:nb * 128], dap(QKT, 1024 * NSLOT + st * 64, [[NSLOT, 128], [128 * NSLOT, 8], [1, nb * 128]]),
                    P.Ds("QKT", range(st, st + 2 * nb)), [kt_.b])
                dma(vw_.ap[:, 0:nb, :], dap(VA, st * 64 * 1040, [[1040, 128], [128 * 1040, nb], [1, 1040]]),
                    P.Ds("VA", range(st, st + 2 * nb)), [vw_.b])

        ld(0)
        for it, (ly, row, st, nb, mrow, mcol) in enumerate(plan_l):
            p = it % 2
            qt_, kt_, vw_, oa_ = qt[p], ktw[p], vw[p], oa[p]
            if it + 1 < len(plan_l):
                ld(it + 1)
            for par in range(2):
                q_ = qa[par][p]
                P.op("dve", lambda e, q_=q_, qt_=qt_, par=par: e.tensor_copy(
                    out=q_.ap[par * 64:(par + 1) * 64, :, :], in_=qt_.ap[par * 64:(par + 1) * 64, :, :]), [qt_.b], [q_.b])
            for par in range(2):
                q_ = qa[par][p]
                pts = []
                for b in range(nb + 1):
                    ismeta = (b == nb)
                    bk = SBANKS[sb_rr[0] % 4]
                    sb_rr[0] += 1
                    p_ = pt[pt_rr[0] % 8]
                    pt_rr[0] += 1
                    pts.append(p_)
                    if ismeta:
                        P.op("pe", lambda e, bk=bk, par=par: e.matmul(
                            psum[bk][0:16, :], ident.ap[:, 0:16], MB.ap[:, par::2, :], start=True, stop=False),
                            [ident.b, MB.b], [PSB[bk]])
                        for hp in range(8):
                            P.op("pe", lambda e, bk=bk, hp=hp, q_=q_, mrow=mrow: e.matmul(
                                psum[bk][0:16, hp * 64:(hp + 1) * 64], ktm[mrow].ap[:, hp, 0:16], q_.ap[:, hp, :],
                                start=False, stop=(hp == 7)), [ktm[mrow].b, q_.b], [PSB[bk]])
                        P.op("act", lambda e, bk=bk, p_=p_: e.activation(
                            out=p_.ap[0:16, :], in_=psum[bk][0:16, :], func=AF.Exp, bias=zero_c.ap[0:16, :], scale=1.0),
                            [PSB[bk], zero_c.b], [p_.b])
                    else:
                        e_idx = (st + 2 * b) - row + 7
                        P.op("pe", lambda e, bk=bk, par=par, e_idx=e_idx: e.matmul(
                            psum[bk][:, :], ident.ap, Tb.ap[:, par::2, e_idx, :], start=True, stop=False),
                            [ident.b, Tb.b], [PSB[bk]])
                        for hp in range(8):
                            P.op("pe", lambda e, bk=bk, hp=hp, q_=q_, kt_=kt_, b=b: e.matmul(
                                psum[bk][:, hp * 64:(hp + 1) * 64], kt_.ap[:, hp, b * 128:(b + 1) * 128], q_.ap[:, hp, :],
                                start=False, stop=(hp == 7)), [kt_.b, q_.b], [PSB[bk]])
                        P.op("act", lambda e, bk=bk, p_=p_, c=mcol + b: e.activation(
                            out=p_.ap, in_=psum[bk][:, :], func=AF.Exp, bias=rowmask.ap[:, c:c + 1], scale=1.0),
                            [PSB[bk], rowmask.b], [p_.b])
                ob = OBANKS[ob_rr[0] % 2]
                ob_rr[0] += 1
                for hp in range(8):
                    h = 2 * hp + par
                    obk = ob[hp // 4]
                    for b in range(nb + 1):
                        ismeta = (b == nb)
                        p_ = pts[b]
                        if ismeta:
                            P.op("pe", lambda e, obk=obk, hp=hp, h=h, p_=p_, b=b, mrow=mrow: e.matmul(
                                psum[obk][0:64, (hp % 4) * 65:(hp % 4 + 1) * 65], p_.ap[:, hp * 64:(hp + 1) * 64],
                                vm[mrow].ap[:, h * 65:(h + 1) * 65], start=(b == 0), stop=True),
                                [p_.b, vm[mrow].b], [PSB[obk]])
                        else:
                            P.op("pe", lambda e, obk=obk, hp=hp, h=h, p_=p_, b=b, vw_=vw_: e.matmul(
                                psum[obk][0:64, (hp % 4) * 65:(hp % 4 + 1) * 65], p_.ap[:, hp * 64:(hp + 1) * 64],
                                vw_.ap[:, b, h * 65:(h + 1) * 65], start=(b == 0), stop=False),
                                [p_.b, vw_.b], [PSB[obk]])
                rc = rec[par]
                for half in range(2):
                    obk = ob[half]
                    ov = psum[obk][0:64, 0:260].rearrange("p (a b) -> p a b", b=65)
                    P.op("dve", lambda e, ov=ov, rc=rc, half=half: e.reciprocal(
                        out=rc.ap[:, half * 4:(half + 1) * 4].unsqueeze(2), in_=ov[:, :, 64:65]), [PSB[obk]], [rc.b])
                    h0 = par + 8 * half
                    P.op("dve", lambda e, ov=ov, rc=rc, half=half, oa_=oa_, h0=h0: e.tensor_tensor(
                        out=oa_.ap[:, h0:h0 + 7:2, :], in0=ov[:, :, 0:64],
                        in1=rc.ap[:, half * 4:(half + 1) * 4].unsqueeze(2).to_broadcast([64, 4, 64]), op=ALU.mult),
                        [PSB[obk], rc.b], [oa_.b])
            dma(OA[row * 64:(row + 1) * 64, :], oa_.ap.rearrange("p a b -> p (a b)"), [oa_.b], [P.D("OA", row)])
        P.barrier()

    def phase_pool():
        A.reset()
        W_ = NSLOT + 32
        vt = T([128, NSLOT], F32)
        dma(vt.ap, t_valid.to_broadcast([128, NSLOT]), [], [vt.b])
        ic = T([128, NSLOT], F32)
        ut = [T([128, W_], F32) for _ in range(2)]
        sa = T([128, W_], F32)
        sb = T([128, W_], F32)
        plt = [T([128, NSLOT], BF16) for _ in range(2)]
        for t_ in ut + [sa, sb]:
            P.op("dve", lambda e, t_=t_: e.memset(t_.ap, 0.0), [], [t_.b])
        allrows = range(NR)
        for c in range(8):
            g = c // 2
            u_ = ut[c % 2]
            pl_ = plt[c % 2]
            if c % 2 == 0:
                dma(ic.ap, t_invcnt[g:g + 1, :].to_broadcast([128, NSLOT]), [], [ic.b])
            dma(u_.ap[:, 16:16 + NSLOT], UT[c * 128:(c + 1) * 128, :], P.Ds("UT", allrows), [u_.b])
            P.op("dve", lambda e, u_=u_: e.tensor_tensor(out=sa.ap[:, 16:16 + NSLOT], in0=u_.ap[:, 16:16 + NSLOT], in1=vt.ap,
                                                         op=ALU.mult), [u_.b, vt.b], [sa.b])
            lo, hi = 8, W_ - 8
            P.op("dve", lambda e: e.tensor_tensor(out=sb.ap[:, lo:hi], in0=sa.ap[:, lo - 1:hi - 1], in1=sa.ap[:, lo:hi], op=ALU.add),
                 [sa.b], [sb.b])
            cur, oth = sb, sa
            for k in range(g):
                sh = 1 << k
                P.op("dve", lambda e, cur=cur, oth=oth, sh=sh: e.tensor_tensor(
                    out=oth.ap[:, lo:hi], in0=cur.ap[:, lo - sh:hi - sh], in1=cur.ap[:, lo + sh:hi + sh], op=ALU.add),
                    [cur.b], [oth.b])
                cur, oth = oth, cur
            P.op("dve", lambda e, cur=cur, oth=oth: e.tensor_tensor(out=oth.ap[:, 16:16 + NSLOT], in0=cur.ap[:, 16:16 + NSLOT],
                                                                    in1=ic.ap, op=ALU.mult), [cur.b, ic.b], [oth.b])
            P.op("dve", lambda e, oth=oth, u_=u_, pl_=pl_: e.tensor_tensor(
                out=pl_.ap, in0=oth.ap[:, 16:16 + NSLOT], in1=u_.ap[:, 16:16 + NSLOT], op=ALU.subtract),
                [oth.b, u_.b], [pl_.b])
            dma(PLT[c * 128:(c + 1) * 128, :], pl_.ap, [pl_.b], P.Ds("PLT", allrows))
        P.barrier()

    def phase_mo(layer, rows):
        A.reset()
        ga = T([128, 1024], F32); gp = T([128, 1024], F32); psc = T([128, 1024], F32)
        dma(ga.ap, g_attn[layer:layer + 1, :].to_broadcast([128, 1024]), [], [ga.b])
        dma(gp.ap, g_pool[layer:layer + 1, :].to_broadcast([128, 1024]), [], [gp.b])
        dma(psc.ap, pscale[layer:layer + 1, :].to_broadcast([128, 1024]), [], [psc.b])
        pw = T([128, 4, 2, 256], BF16)
        dma(pw.ap, dap(wb["pool_w"], layer * 4 * 65536, [[256, 128], [65536, 4], [128 * 256, 2], [1, 256]]),
            wdep("pool_w", layer * 128, layer * 128 + 128), [pw.b])
        oat = [T([128, 1024], F32) for _ in range(2)]
        plt = [T([128, 8, 128], BF16) for _ in range(2)]
        mx = [T([128, 1024], F32) for _ in range(2)]
        on = [T([128, D], BF16) for _ in range(2)]
        ot = [T([128, 16, 128], BF16) for _ in range(2)]
        junk = T([128, 1024], BF16)
        ss = [T([128, 2], F32) for _ in range(2)]
        rs = [T([128, 2], F32) for _ in range(2)]
        tiles = [(rows[i], rows[i + 1]) for i in range(0, len(rows), 2)]
        def ld(ti):
            oa_, pl_ = oat[ti % 2], plt[ti % 2]
            for j, r in enumerate(tiles[ti]):
                dma(oa_.ap[j * 64:(j + 1) * 64, :], OA[r * 64:(r + 1) * 64, :], [P.D("OA", r)], [oa_.b])
                dma(pl_.ap[:, :, j * 64:(j + 1) * 64], dap(PLT, r * 64, [[NSLOT, 128], [128 * NSLOT, 8], [1, 64]]),
                    [P.D("PLT", r)], [pl_.b])

        ld(0)
        for ti, (ra, rb) in enumerate(tiles):
            p = ti % 2
            oa_, pl_, mx_, on_, ot_, ss_, rs_ = oat[p], plt[p], mx[p], on[p], ot[p], ss[p], rs[p]
            if ti + 1 < len(tiles):
                ld(ti + 1)
            bk2 = [next_bank(), next_bank()]
            for g in range(4):
                bk = bk2[g // 2]
                for cc in range(2):
                    P.op("pe", lambda e, bk=bk, g=g, cc=cc, pl_=pl_: e.matmul(
                        psum[bk][:, (g % 2) * 256:(g % 2 + 1) * 256], pl_.ap[:, 2 * g + cc, :], pw.ap[:, g, cc, :],
                        start=(cc == 0), stop=(cc == 1)), [pl_.b, pw.b], [PSB[bk]])
            for hf in range(2):
                P.op("dve", lambda e, hf=hf, mx_=mx_, bk=bk2[hf]: e.tensor_tensor(
                    out=mx_.ap[:, hf * 512:(hf + 1) * 512], in0=psum[bk][:, :], in1=psc.ap[:, hf * 512:(hf + 1) * 512], op=ALU.mult),
                    [PSB[bk2[hf]], psc.b], [mx_.b])
            P.op("act", lambda e, oa_=oa_, ss_=ss_: e.activation(out=junk.ap, in_=oa_.ap, func=AF.Square, accum_out=ss_.ap[:, 0:1]),
                 [oa_.b], [junk.b, ss_.b])
            P.op("act", lambda e, mx_=mx_, ss_=ss_: e.activation(out=junk.ap, in_=mx_.ap, func=AF.Square, accum_out=ss_.ap[:, 1:2]),
                 [mx_.b], [junk.b, ss_.b])
            P.op("act", lambda e, ss_=ss_, rs_=rs_: e.activation(out=rs_.ap, in_=ss_.ap, func=AF.Sqrt, bias=eps_c.ap,
                                                                 scale=1.0 / 1024), [ss_.b, eps_c.b], [rs_.b])
            P.op("dve", lambda e, rs_=rs_: e.reciprocal(out=rs_.ap, in_=rs_.ap), [rs_.b], [rs_.b])
            P.op("dve", lambda e, oa_=oa_, rs_=rs_, on_=on_: e.scalar_tensor_tensor(
                out=on_.ap[:, 0:1024], in0=oa_.ap, scalar=rs_.ap[:, 0:1], in1=ga.ap, op0=ALU.mult, op1=ALU.mult),
                [oa_.b, rs_.b, ga.b], [on_.b])
            P.op("dve", lambda e, mx_=mx_, rs_=rs_, on_=on_: e.scalar_tensor_tensor(
                out=on_.ap[:, 1024:2048], in0=mx_.ap, scalar=rs_.ap[:, 1:2], in1=gp.ap, op0=ALU.mult, op1=ALU.mult),
                [mx_.b, rs_.b, gp.b], [on_.b])
            bks = [next_bank(), next_bank()]
            for kc in range(16):
                bk = bks[kc // 8]
                P.op("pe", lambda e, on_=on_, kc=kc, bk=bk: e.transpose(
                    out=psum[bk][:, :].bitcast(BF16)[:, (kc % 8) * 128:(kc % 8 + 1) * 128],
                    in_=on_.ap[:, kc * 128:(kc + 1) * 128], identity=ident.ap), [on_.b, ident.b], [PSB[bk]])
            P.op("act", lambda e, ot_=ot_, bk=bks[0]: e.activation(
                out=ot_.ap[:, 0:8, :], in_=psum[bk][:, :].bitcast(BF16).rearrange("p (a b) -> p a b", b=128), func=AF.Copy),
                [PSB[bks[0]]], [ot_.b])
            P.op("dve", lambda e, ot_=ot_, bk=bks[1]: e.tensor_copy(
                out=ot_.ap[:, 8:16, :], in_=psum[bk][:, :].bitcast(BF16).rearrange("p (a b) -> p a b", b=128)),
                [PSB[bks[1]]], [ot_.b])
            for j, r in enumerate((ra, rb)):
                dma(dap(OT, r * 64, [[NSLOT, 128], [128 * NSLOT, 16], [1, 64]]), ot_.ap[:, :, j * 64:(j + 1) * 64],
                    [ot_.b], [P.D("OT", r)])
        P.barrier()

    def phase_final():
        A.reset()
        gt = T([128, D], F32)
        dma(gt.ap, g_final.to_broadcast([128, D]), [], [gt.b])
        hb = [T([128, D], F32) for _ in range(2)]
        ob = [T([128, D], F32) for _ in range(2)]
        junk = T([128, D], BF16)
        ss = [T([128, 1], F32) for _ in range(2)]
        rs = [T([128, 1], F32) for _ in range(2)]
        tiles = [(L1_OUT[i], L1_OUT[i + 1]) for i in range(0, len(L1_OUT), 2)]
        def ld(ti):
            h_ = hb[ti % 2]
            for j, r in enumerate(tiles[ti]):
                dma(h_.ap[j * 64:(j + 1) * 64, :], H[r * 64:(r + 1) * 64, :], [P.D("H", r)], [h_.b])

        ld(0)
        for ti, (ra, rb) in enumerate(tiles):
            p = ti % 2
            h_, o_, ss_, rs_ = hb[p], ob[p], ss[p], rs[p]
            if ti + 1 < len(tiles):
                ld(ti + 1)
            P.op("act", lambda e, h_=h_, ss_=ss_: e.activation(out=junk.ap, in_=h_.ap, func=AF.Square, accum_out=ss_.ap),
                 [h_.b], [junk.b, ss_.b])
            P.op("act", lambda e, ss_=ss_, rs_=rs_: e.activation(out=rs_.ap, in_=ss_.ap, func=AF.Sqrt, bias=eps_c.ap,
                                                                 scale=1.0 / D), [ss_.b, eps_c.b], [rs_.b])
            P.op("dve", lambda e, rs_=rs_: e.reciprocal(out=rs_.ap, in_=rs_.ap), [rs_.b], [rs_.b])
            P.op("dve", lambda e, h_=h_, rs_=rs_, o_=o_: e.scalar_tensor_tensor(
                out=o_.ap, in0=h_.ap, scalar=rs_.ap, in1=gt.ap, op0=ALU.mult, op1=ALU.mult), [h_.b, rs_.b, gt.b], [o_.b])
            for j, r in enumerate((ra, rb)):
                if r <= 32:
                    dst = ys[(r - 1) * 64:r * 64, :]
                else:
                    dst = yp[(r - 43) * 64:(r - 42) * 64, :]
                dma(dst, o_.ap[j * 64:(j + 1) * 64, :], [o_.b], [])
        P.barrier()

    def mixer(layer, kv_rows, out_rows, src, src_name):
        WB = layer * D * 4096
        dep_in = (layer * WIN_R, (layer + 1) * WIN_R)
        phase_nt(kv_rows, src, src_name, ln_mix[layer:layer + 1, :])
        if stop_here(f"nt{layer}"): return True
        gk = groups_of(kv_rows)
        lin_b(gk, HNT, 16, [("w_in", dep_in, WB, 4096, 0)], 2048, epi_qk)
        lin_a(gk, HNT, 16, "w_in", dep_in, WB, 4096, 2048, 1024, 512, epi_v)
        lin_b(gk, HNT, 16, [("w_in", dep_in, WB, 4096, 3072)], 1024, epi_u)
        if stop_here(f"qkvu{layer}"): return True
        phase_att(layer)
        if stop_here(f"att{layer}"): return True
        phase_pool()
        if stop_here(f"pool{layer}"): return True
        orows = out_rows if len(out_rows) % 2 == 0 else out_rows + [65]
        phase_mo(layer, orows)
        if stop_here(f"mo{layer}"): return True
        lin_a(groups_of(orows), OT, 16, "w_out", (layer * D, (layer + 1) * D), layer * D * D, D, 0, D, 512,
              make_epi_res(src, src_name, 512))
        if stop_here(f"wout{layer}"): return True
        return False

    def run_all():
        if mixer(0, L0_ALL, L0_OUT, xs, "xs"): return
        phase_nt(L0_OUT, H, "H4", ln_ffn[0:1, :])
        g0 = groups_of(L0_OUT)
        lin_b(g0, HNT, 16, [("ffn_g", (0, DFF), 0, DFF, 0), ("ffn_u", (0, DFF), 0, DFF, 0)], DFF, epi_gu)
        lin_a(groups_of(L0_OUT, 10), ACTT, 44, "ffn_d", (0, DFF), 0, D, 0, D, 256, make_epi_res(H, "H", 256), nxg=2)
        if stop_here("ffn0"): return
        if mixer(1, L1_KV, L1_OUT, H, "H8"): return
        phase_nt(L1_OUT, H, "H4", ln_ffn[1:2, :], router=True)
        g1 = groups_of(L1_OUT)
        for ex_ in range(NE):
            dep = (ex_ * DFF, (ex_ + 1) * DFF)
            lin_b(g1, HNT, 16, [("moe_g", dep, ex_ * D * DFF, DFF, 0), ("moe_u", dep, ex_ * D * DFF, DFF, 0)], DFF, epi_gu)
            lin_a(groups_of(L1_OUT, 10), ACTT, 44, "moe_d", dep, ex_ * DFF * D, D, 0, D, 256,
                  make_epi_res(H, "H", 256, comb_e=ex_), nxg=2)
        phase_final()

    run_all()
    P.barrier()

    with nc.Block() as block:
        @block.sync
        def _(e):
            P.emit("sp", e)

        @block.gpsimd
        def _(e):
            P.emit("pool", e)

        @block.scalar
        def _(e):
            P.emit("act", e)

        @block.vector
        def _(e):
            P.emit("dve", e)

        @block.tensor
        def _(e):
            P.emit("pe", e)
    es.close()
    return nc


def make_in_maps(inp):
    f = np.float32
    xp = np.asarray(inp["x_prompt"], f)[0]
    xsm = np.asarray(inp["x_sample"], f)
    meta = np.asarray(inp["meta_tokens"], f)
    shared = {
        "w_in": np.ascontiguousarray(np.asarray(inp["w_in"], f)).reshape(-1, 2048),
        "w_out": np.ascontiguousarray(np.asarray(inp["w_out"], f)).reshape(-1, 2048),
        "pool_w": np.ascontiguousarray(np.asarray(inp["pool_w"], f)).reshape(-1, 2048),
        "ffn_g": np.ascontiguousarray(np.asarray(inp["ffn_w_gate"], f)).reshape(-1, 2048),
        "ffn_u": np.ascontiguousarray(np.asarray(inp["ffn_w_up"], f)).reshape(-1, 2048),
        "ffn_d": np.ascontiguousarray(np.asarray(inp["ffn_w_down"], f)).reshape(-1, 2048),
        "moe_g": np.ascontiguousarray(np.asarray(inp["moe_w_gate"], f)).reshape(-1, 2048),
        "moe_u": np.ascontiguousarray(np.asarray(inp["moe_w_up"], f)).reshape(-1, 2048),
        "moe_d": np.ascontiguousarray(np.asarray(inp["moe_w_down"], f)).reshape(-1, 2048),
        "rpb": np.ascontiguousarray(np.asarray(inp["rpb"], f)).reshape(2, -1),
        "mbT": np.ascontiguousarray(np.transpose(np.asarray(inp["meta_bias"], f), (0, 2, 1))),
        "ln_mix": np.asarray(inp["ln_mix"], f), "ln_ffn": np.asarray(inp["ln_ffn"], f),
        "g_attn": np.asarray(inp["g_attn_out"], f), "g_pool": np.asarray(inp["g_pool_out"], f),
        "pscale": np.asarray(inp["pool_scale"], f),
        "g_final": np.asarray(inp["g_final"], f).reshape(1, D),
        "routerT": np.ascontiguousarray(np.asarray(inp["router"], f)[0].T),
    }
    maps = []
    for c in range(8):
        x = np.zeros((NSLOT, D), f)
        x[48:64] = meta
        x[64:64 + 2048] = xsm[c]
        x[ROW_PM * 64 + 48:ROW_PM * 64 + 64] = meta
        x[34 * 64:35 * 64] = xp[0:64]
        for i in range(30):
            r = 16 * c - 8 + i
            b0 = (ROW_P0 + i) * 64
            if 0 <= r < 128:
                x[b0:b0 + 64] = xp[r * 64:(r + 1) * 64]
            elif r == -1:
                x[b0 + 48:b0 + 64] = meta
        m = dict(shared)
        m["xs"] = x
        m.update(host_tables(c))
        maps.append(m)
    return maps


_NC = None


def kernel(**inputs):
    global _NC
    if _NC is None:
        _NC = build()
    maps = make_in_maps(inputs)
    res = run_bass_kernel_spmd(_NC, maps, core_ids=list(range(8)))
    ysamp = np.stack([np.asarray(res.results[c]["ys"], np.float32) for c in range(8)], axis=0)
    yprm = np.concatenate([np.asarray(res.results[c]["yp"], np.float32) for c in range(8)], axis=0)[None]
    return (yprm, ysamp)
```

```python
import numpy as np
import concourse.bass as bass
import concourse.mybir as mybir
from concourse.bass_utils import run_bass_kernel_spmd

F32, BF16 = mybir.dt.float32, mybir.dt.bfloat16
AF = mybir.ActivationFunctionType
ALU = mybir.AluOpType

D = 2048
DFF = 5632
NE = 8
NR = 66
NSLOT = NR * 64
NEG = -30000.0
EPS = 1e-6
ROW_PM = 33
ROW_P0 = 35
DBG = {"stop": None, "dump": []}

L0_ALL = list(range(66))
L0_OUT = list(range(0, 34)) + list(range(39, 62)) + [65]
L1_KV = L0_OUT
L1_OUT = list(range(1, 33)) + list(range(43, 59))


def groups_of(rows, g=16):
    return [rows[i:i + g] for i in range(0, len(rows), g)]


def runs_of(rows):
    out = []
    for j, r in enumerate(rows):
        if out and out[-1][0] + out[-1][1] == r:
            out[-1][1] += 1
        else:
            out.append([r, 1, j])
    return [tuple(x) for x in out]


def att_plan():
    plan = []
    col = 2
    for layer in range(2):
        rows = []
        if layer == 0:
            rows += [0]
        rows += list(range(1, 33))
        if layer == 0:
            rows += [ROW_PM]
        prange = range(4, 27) if layer == 0 else range(8, 24)
        rows += [ROW_P0 + i for i in prange]
        for row in rows:
            if row == 0 or row == ROW_PM:
                plan.append((layer, row, 0, 0, row, col))
                continue
            if row <= 32:
                g = row - 1
                r0 = min(max(g - 4, 0), 24)
                plan.append((layer, row, 1 + r0, 4, 0, col))
                col += 4
                continue
            i = row - ROW_P0
            st, nb = i - 4, 4
            if i in (8, 9):
                nb = 6
            elif i in (10, 11):
                nb = 5
            elif i in (21, 22):
                st, nb = 16, 5
            elif i == 23:
                st, nb = 16, 6
            plan.append((layer, row, ROW_P0 + st, nb, ROW_PM, col))
            col += nb
    return plan, col


PLAN, NMASK = att_plan()


def host_tables(c):
    rowmask = np.zeros((128, NMASK), np.float32)
    rowmask[16:, 1] = NEG
    for (layer, row, st, nb, mrow, col) in PLAN:
        if row < ROW_P0:
            continue
        i = row - ROW_P0
        r = 16 * c - 8 + i
        for b in range(nb):
            for kh in range(2):
                j = st - ROW_P0 + 2 * b + kh
                rk = 16 * c - 8 + j
                if r < 0 or r > 127:
                    ok = not (c == 0 and r == -1)
                else:
                    r0 = min(max(r - 4, 0), 120)
                    ok = (r0 <= rk <= r0 + 7)
                if not ok:
                    rowmask[kh * 64:(kh + 1) * 64, col + b] = NEG
    valid = np.zeros((NSLOT,), np.float32)
    pos = np.zeros((NSLOT,), np.int64)
    slen = np.ones((NSLOT,), np.int64)
    sl = np.arange(64)
    valid[48:64] = 1; pos[48:64] = np.arange(16); slen[0:64] = 2064
    for k in range(1, 33):
        valid[k * 64:(k + 1) * 64] = 1
        pos[k * 64:(k + 1) * 64] = 16 + (k - 1) * 64 + sl
        slen[k * 64:(k + 1) * 64] = 2064
    LP = 16 + 8192
    b0 = ROW_PM * 64
    valid[b0 + 48:b0 + 64] = 1; pos[b0 + 48:b0 + 64] = np.arange(16); slen[b0:b0 + 64] = LP
    b0 = 34 * 64
    valid[b0:b0 + 64] = 1; pos[b0:b0 + 64] = 16 + sl; slen[b0:b0 + 64] = LP
    for i in range(30):
        r = 16 * c - 8 + i
        b0 = (ROW_P0 + i) * 64
        slen[b0:b0 + 64] = LP
        if 0 <= r < 128:
            valid[b0:b0 + 64] = 1
            pos[b0:b0 + 64] = 16 + r * 64 + sl
        elif r == -1:
            valid[b0 + 48:b0 + 64] = 1
            pos[b0 + 48:b0 + 64] = np.arange(16)
    invcnt = np.zeros((4, NSLOT), np.float32)
    for g, w in enumerate((2, 4, 8, 16)):
        lo = np.maximum(pos - w // 2, 0)
        hi = np.minimum(pos + w // 2, slen)
        cnt = np.where(valid > 0, hi - lo, w)
        invcnt[g] = 1.0 / cnt.astype(np.float32)
    colmask = np.zeros((128, 64), np.float32)
    for qc in range(64):
        cs = min(max(qc - 8, 0), 48)
        for kc in range(64):
            if not (cs <= kc < cs + 16):
                colmask[kc, qc] = NEG
                colmask[64 + kc, qc] = NEG
    jj = np.zeros((128, 64), np.float32)
    for q in range(64):
        jj[q, 63 - q] = 1.0
        jj[64 + q, 63 - q] = 1.0
    return dict(rowmask=rowmask, valid=valid.reshape(1, NSLOT), invcnt=invcnt, colmask=colmask, jj=jj,
                ident=np.eye(128, dtype=np.float32))


class Buf:
    __slots__ = ("w", "r")

    def __init__(self):
        self.w = None
        self.r = {}


class Q:
    def __init__(self, name, csem, dsems):
        self.name, self.csem, self.dsems = name, csem, dsems
        self.ccnt = 0
        self.dcnt = [0] * len(dsems)
        self.i = 0
        self.ops = []
        self.known = {}


class Prog:
    def __init__(self, nc, es):
        self.nc = nc
        self.q = {}
        for name, ndma, comp in (("sp", 8, False), ("pool", 2, False), ("act", 6, True), ("dve", 0, True), ("pe", 0, True)):
            dsems = [es.enter_context(nc.semaphore(f"d_{name}{k}")) for k in range(ndma)]
            csem = es.enter_context(nc.semaphore(f"c_{name}")) if comp else None
            self.q[name] = Q(name, csem, dsems)
        self.dbufs = {}
        self.allbufs = []

    def buf(self, persistent=False):
        b = Buf()
        if not persistent:
            self.allbufs.append(b)
        return b

    def D(self, name, key=0):
        k = (name, key)
        b = self.dbufs.get(k)
        if b is None:
            b = self.dbufs[k] = self.buf()
        return b

    def Ds(self, name, keys):
        return [self.D(name, k) for k in keys]

    def op(self, qn, fn, reads=(), writes=(), dma=None):
        q = self.q[qn]
        if dma is None:
            dma = q.csem is None
        waits = {}

        def need(ev):
            if ev is None:
                return
            sem, val = ev
            if qn == "pe" and sem is q.csem:
                return
            k = id(sem)
            if k not in waits or waits[k][1] < val:
                waits[k] = (sem, val)

        for b in reads:
            need(b.w)
        for b in writes:
            need(b.w)
            for ev in b.r.values():
                need(ev)
        if dma:
            k = q.i % len(q.dsems)
            q.i += 1
            sem = q.dsems[k]
            if q.dcnt[k] > 0:
                need((sem, q.dcnt[k]))
            q.dcnt[k] += 16
            ev = (sem, q.dcnt[k])
            inc = 16
        else:
            sem = q.csem
            q.ccnt += 1
            ev = (sem, q.ccnt)
            inc = 1
        wl = []
        for k2, (s_, v) in waits.items():
            if q.known.get(k2, 0) >= v:
                continue
            q.known[k2] = v
            wl.append((s_, v))
        q.ops.append((fn, wl, sem, inc))
        for b in reads:
            k3 = id(sem)
            if k3 not in b.r or b.r[k3][1] < ev[1]:
                b.r[k3] = ev
        for b in writes:
            b.w = ev
            b.r = {}
        return ev

    def barrier(self):
        evs = []
        for q in self.q.values():
            if q.name == "pool":
                continue
            if q.csem is not None and q.ccnt > 0:
                evs.append((q.csem, q.ccnt))
            for k, s_ in enumerate(q.dsems):
                if q.dcnt[k] > 0:
                    evs.append((s_, q.dcnt[k]))
        for q in self.q.values():
            if q.name == "pool":
                continue
            for (s_, v) in evs:
                if q.known.get(id(s_), 0) < v:
                    q.known[id(s_)] = v
                    q.ops.append((None, [(s_, v)], None, 0))
        for b in self.allbufs:
            b.w = None
            b.r = {}

    def emit(self, qn, e):
        q = self.q[qn]
        for fn, wl, sem, inc in q.ops:
            for s_, v in wl:
                e.wait_ge(s_, v)
            if fn is not None:
                fn(e).then_inc(sem, inc)
        if qn == "sp":
            for k, s_ in enumerate(q.dsems):
                if q.dcnt[k] > 0:
                    e.wait_ge(s_, q.dcnt[k])


class Arena:
    def __init__(self, ap_u8, nbytes):
        self.ap, self.n = ap_u8, nbytes
        self.base = 0
        self.off = 0

    def persist(self):
        self.base = self.off

    def reset(self):
        self.off = self.base

    def alloc(self, shape, dt):
        esz = 4 if dt == F32 else 2
        n = int(np.prod(shape[1:]))
        nb = (n * esz + 63) // 64 * 64
        assert self.off + nb <= self.n, f"SBUF arena overflow {self.off}+{nb}>{self.n}"
        a = self.ap[0:shape[0], self.off:self.off + nb].bitcast(dt)[:, 0:n]
        self.off += nb
        if len(shape) == 3:
            a = a.rearrange("p (a b) -> p a b", b=shape[2])
        elif len(shape) == 4:
            a = a.rearrange("p (a b c) -> p a b c", b=shape[2], c=shape[3])
        return a


def dap(t, off, dims):
    return bass.AP(tensor=t.tensor, offset=off, ap=[[int(s), int(n)] for s, n in dims])


def build():
    from contextlib import ExitStack
    nc = bass.Bass("TRN2", target_bir_lowering=False)
    es = ExitStack()
    es.enter_context(nc.allow_low_precision("bf16 matmul operands, fp32 accumulate"))
    es.enter_context(nc.allow_non_contiguous_dma(reason="small strided param loads"))

    def din(name, shape, dt=F32):
        return nc.dram_tensor(name, list(shape), dt, kind="ExternalInput").ap()

    def dscr(name, shape, dt):
        kind = "ExternalOutput" if name in DBG["dump"] else "Internal"
        return nc.dram_tensor(name, list(shape), dt, kind=kind).ap()

    xs = din("xs", [NSLOT, D])
    wsrc = {
        "w_in": din("w_in", [2 * D * 4096 // 2048, 2048]),
        "w_out": din("w_out", [2 * D, 2048]),
        "pool_w": din("pool_w", [2 * 4 * 256 * 256 // 2048, 2048]),
        "ffn_g": din("ffn_g", [DFF, 2048]), "ffn_u": din("ffn_u", [DFF, 2048]), "ffn_d": din("ffn_d", [DFF, 2048]),
        "moe_g": din("moe_g", [NE * DFF, 2048]), "moe_u": din("moe_u", [NE * DFF, 2048]),
        "moe_d": din("moe_d", [NE * DFF, 2048]),
    }
    rpb = din("rpb", [2, 16 * 15 * 31])
    mbT = din("mbT", [2, 16, 16])
    ln_mix = din("ln_mix", [2, D]); ln_ffn = din("ln_ffn", [2, D])
    g_attn = din("g_attn", [2, 1024]); g_pool = din("g_pool", [2, 1024]); pscale = din("pscale", [2, 1024])
    g_final = din("g_final", [1, D])
    routerT = din("routerT", [NE, D])
    t_rowmask = din("rowmask", [128, NMASK]); t_valid = din("valid", [1, NSLOT]); t_invcnt = din("invcnt", [4, NSLOT])
    t_colmask = din("colmask", [128, 64]); t_jj = din("jj", [128, 64]); t_ident = din("ident", [128, 128])
    ys = nc.dram_tensor("ys", [2048, D], F32, kind="ExternalOutput").ap()
    yp = nc.dram_tensor("yp", [1024, D], F32, kind="ExternalOutput").ap()

    wb = {k: dscr(k + "_b", list(v.shape), BF16) for k, v in wsrc.items()}
    H = dscr("H", [NSLOT, D], F32)
    HNT = dscr("HNT", [D, NSLOT], BF16)
    QKT = dscr("QKT", [D, NSLOT], BF16)
    VA = dscr("VA", [NSLOT, 1040], BF16)
    UT = dscr("UT", [1024, NSLOT], F32)
    PLT = dscr("PLT", [1024, NSLOT], BF16)
    OA = dscr("OA", [NSLOT, 1024], F32)
    OT = dscr("OT", [D, NSLOT], BF16)
    ACTT = dscr("ACTT", [DFF, NSLOT], BF16)
    RPAD = dscr("RPAD", [1, 64 + 7440 + 128], F32)

    ARENA_BYTES = 188 * 1024
    arena_t = es.enter_context(nc.sbuf_tensor("arena", [128, ARENA_BYTES], mybir.dt.uint8))
    A = Arena(arena_t[:, :], ARENA_BYTES)
    psum = [es.enter_context(nc.psum_tensor(f"ps{i}", [128, 512], F32)) for i in range(8)]
    P = Prog(nc, es)
    PSB = [P.buf() for _ in range(8)]

    class T:
        def __init__(self, shape, dt):
            self.ap = A.alloc(shape, dt)
            self.b = P.buf()

    def dma(out, in_, reads, writes, qn="sp"):
        return P.op(qn, lambda e: e.dma_start(out=out, in_=in_), reads, writes, dma=True)

    ident_f = T([128, 128], F32); ident = T([128, 128], BF16)
    jj_f = T([128, 64], F32); jj = T([128, 64], BF16)
    colmask = T([128, 64], F32)
    rowmask = T([128, NMASK], F32)
    COMB = T([128, 24, 8], F32)
    zero_c = T([128, 1], F32)
    dma(ident_f.ap, t_ident, [], [ident_f.b]); dma(jj_f.ap, t_jj, [], [jj_f.b])
    dma(colmask.ap, t_colmask, [], [colmask.b]); dma(rowmask.ap, t_rowmask, [], [rowmask.b])
    P.op("dve", lambda e: e.tensor_copy(out=ident.ap, in_=ident_f.ap), [ident_f.b], [ident.b])
    P.op("dve", lambda e: e.tensor_copy(out=jj.ap, in_=jj_f.ap), [jj_f.b], [jj.b])
    P.op("dve", lambda e: e.memset(zero_c.ap, 0.0), [], [zero_c.b])
    eps_c = T([128, 1], F32)
    P.op("dve", lambda e: e.memset(eps_c.ap, EPS), [], [eps_c.b])
    A.persist()

    WBUFS = {}

    def pe_cnt():
        return P.q["pe"].ccnt

    def cast_set(items, span=None):
        CH = 512
        chunks = []
        for (name, r0, r1) in items:
            WBUFS.setdefault((name, r0, r1), [])
            for a in range(r0, r1, CH):
                chunks.append((name, r0, r1, a, min(a + CH, r1)))
        n = len(chunks)
        for j, (name, r0, r1, a, b_) in enumerate(chunks):
            bb = P.buf(persistent=True)
            WBUFS[(name, r0, r1)].append(bb)
            reads = []
            if span is not None:
                c0, c1 = span
                v = c0 + int(0.9 * (c1 - c0) * j / n)
                if v > 0:
                    gb = P.buf(persistent=True)
                    gb.w = (P.q["pe"].csem, v)
                    reads = [gb]
            dma(wb[name][a:b_, :], wsrc[name][a:b_, :], reads, [bb], qn="pool")

    def wdep(name, r0, r1):
        return WBUFS[(name, r0, r1)]

    WIN_R = D * 4096 // 2048

    def moe_items(e_):
        return [(nm, e_ * DFF, (e_ + 1) * DFF) for nm in ("moe_g", "moe_u", "moe_d")]

    bank_rr = [0]

    def next_bank():
        i = bank_rr[0] % 8
        bank_rr[0] += 1
        return i

    def stop_here(tag):
        return DBG["stop"] == tag

    def phase_nt(rows, src, src_name, gain_ap, router=False):
        A.reset()
        gt = T([128, D], F32)
        dma(gt.ap, gain_ap.to_broadcast([128, D]), [], [gt.b])
        hb = [T([128, D], F32) for _ in range(2)]
        hnb = [T([128, D], BF16) for _ in range(2)]
        xt = [T([128, 16, 128], BF16) for _ in range(2)]
        junk = T([128, D], BF16)
        ss = [T([128, 1], F32) for _ in range(2)]
        rs = [T([128, 1], F32) for _ in range(2)]
        if router:
            rt = T([128, NE, D], F32)
            dma(rt.ap, dap(routerT, 0, [[0, 128], [D, NE], [1, D]]), [], [rt.b])
            hnf = T([128, D], F32)
            junk2 = T([128, D], F32)
            lg = T([128, 8], F32); m8 = T([128, 8], F32); nv1 = T([128, 1], F32)
            ex = T([128, 8], F32); mk = T([128, 8], F32); num = T([128, 8], F32); den = T([128, 1], F32)
        tiles = [(rows[i], rows[i + 1]) for i in range(0, len(rows), 2)]
        def ld(ti):
            ra, rb = tiles[ti]
            h_ = hb[ti % 2]
            dma(h_.ap[0:64, :], src[ra * 64:(ra + 1) * 64, :], [P.D(src_name, ra)], [h_.b])
            dma(h_.ap[64:128, :], src[rb * 64:(rb + 1) * 64, :], [P.D(src_name, rb)], [h_.b])

        ld(0)
        for ti, (ra, rb) in enumerate(tiles):
            p = ti % 2
            h_, hn_, xt_, ss_, rs_ = hb[p], hnb[p], xt[p], ss[p], rs[p]
            if ti + 1 < len(tiles):
                ld(ti + 1)
            P.op("act", lambda e, h_=h_, ss_=ss_: e.activation(out=junk.ap, in_=h_.ap, func=AF.Square, accum_out=ss_.ap),
                 [h_.b], [junk.b, ss_.b])
            P.op("act", lambda e, ss_=ss_, rs_=rs_: e.activation(out=rs_.ap, in_=ss_.ap, func=AF.Sqrt, bias=eps_c.ap,
                                                                 scale=1.0 / D), [ss_.b, eps_c.b], [rs_.b])
            P.op("dve", lambda e, rs_=rs_: e.reciprocal(out=rs_.ap, in_=rs_.ap), [rs_.b], [rs_.b])
            if not router:
                P.op("dve", lambda e, h_=h_, rs_=rs_, hn_=hn_: e.scalar_tensor_tensor(
                    out=hn_.ap, in0=h_.ap, scalar=rs_.ap, in1=gt.ap, op0=ALU.mult, op1=ALU.mult),
                    [h_.b, rs_.b, gt.b], [hn_.b])
            else:
                P.op("dve", lambda e, h_=h_, rs_=rs_: e.scalar_tensor_tensor(
                    out=hnf.ap, in0=h_.ap, scalar=rs_.ap, in1=gt.ap, op0=ALU.mult, op1=ALU.mult),
                    [h_.b, rs_.b, gt.b], [hnf.b])
                P.op("act", lambda e, hn_=hn_: e.activation(out=hn_.ap, in_=hnf.ap, func=AF.Copy), [hnf.b], [hn_.b])
                for ex_ in range(NE):
                    P.op("dve", lambda e, ex_=ex_: e.scalar_tensor_tensor(
                        out=junk2.ap, in0=hnf.ap, scalar=1.0, in1=rt.ap[:, ex_, :], op0=ALU.mult, op1=ALU.mult,
                        accum_out=lg.ap[:, ex_:ex_ + 1]), [hnf.b, rt.b], [junk2.b, lg.b])
                P.op("dve", lambda e: e.max(out=m8.ap, in_=lg.ap), [lg.b], [m8.b])
                P.op("dve", lambda e: e.tensor_scalar(out=nv1.ap, in0=m8.ap[:, 0:1], scalar1=-1.0, scalar2=None, op0=ALU.mult),
                     [m8.b], [nv1.b])
                P.op("act", lambda e: e.activation(out=ex.ap, in_=lg.ap, func=AF.Exp, bias=nv1.ap, scale=1.0),
                     [lg.b, nv1.b], [ex.b])
                P.op("dve", lambda e: e.tensor_scalar(out=mk.ap, in0=lg.ap, scalar1=m8.ap[:, 1:2], scalar2=None, op0=ALU.is_ge),
                     [lg.b, m8.b], [mk.b])
                P.op("dve", lambda e: e.scalar_tensor_tensor(out=num.ap, in0=ex.ap, scalar=1.0, in1=mk.ap, op0=ALU.mult,
                                                             op1=ALU.mult, accum_out=den.ap), [ex.b, mk.b], [num.b, den.b])
                P.op("dve", lambda e: e.reciprocal(out=den.ap, in_=den.ap), [den.b], [den.b])
                P.op("dve", lambda e, ti=ti: e.tensor_scalar(out=COMB.ap[:, ti, :], in0=num.ap, scalar1=den.ap, scalar2=None,
                                                             op0=ALU.mult), [num.b, den.b], [COMB.b])
            bks = [next_bank(), next_bank()]
            for kc in range(16):
                bk = bks[kc // 8]
                P.op("pe", lambda e, hn_=hn_, kc=kc, bk=bk: e.transpose(
                    out=psum[bk][:, :].bitcast(BF16)[:, (kc % 8) * 128:(kc % 8 + 1) * 128],
                    in_=hn_.ap[:, kc * 128:(kc + 1) * 128], identity=ident.ap), [hn_.b, ident.b], [PSB[bk]])
            P.op("act", lambda e, xt_=xt_, bk=bks[0]: e.activation(
                out=xt_.ap[:, 0:8, :], in_=psum[bk][:, :].bitcast(BF16).rearrange("p (a b) -> p a b", b=128), func=AF.Copy),
                [PSB[bks[0]]], [xt_.b])
            P.op("dve", lambda e, xt_=xt_, bk=bks[1]: e.tensor_copy(
                out=xt_.ap[:, 8:16, :], in_=psum[bk][:, :].bitcast(BF16).rearrange("p (a b) -> p a b", b=128)),
                [PSB[bks[1]]], [xt_.b])
            for j, r in enumerate((ra, rb)):
                dma(dap(HNT, r * 64, [[NSLOT, 128], [128 * NSLOT, 16], [1, 64]]), xt_.ap[:, :, j * 64:(j + 1) * 64],
                    [xt_.b], [P.D("HNT", r)])
        P.barrier()

    def load_xg(xg, xt_ap, f0, KC, grows):
        for (r0, n, j) in runs_of(grows):
            dma(xg.ap[:, :, j * 64:(j + n) * 64],
                dap(xt_ap, f0 * NSLOT + r0 * 64, [[NSLOT, 128], [128 * NSLOT, KC], [1, 64 * n]]),
                P.Ds(xt_ap.tensor.name, range(r0, r0 + n)), [xg.b])

    def lin_b(groups, xt_ap, KC, wspecs, nfeat, epi, FB=512):
        A.reset()
        TMAX = max(len(g) for g in groups) * 64
        nxg = 2 if KC * TMAX * 2 * 2 <= 70 * 1024 else 1
        xg = [T([128, KC, TMAX], BF16) for _ in range(nxg)]
        wbk = [[T([128, KC, FB], BF16) for _ in range(2)] for _ in wspecs]
        st = epi("alloc", None)
        tasks = [(gi, fb) for gi in range(len(groups)) for fb in range(nfeat // FB)]

        def loadx(ti):
            gi, fb = tasks[ti]
            if fb == 0:
                load_xg(xg[gi % nxg], xt_ap, 0, KC, groups[gi])

        def loads(ti):
            gi, fb = tasks[ti]
            for si, (wname, dep, base, ldw, col0) in enumerate(wspecs):
                w_ = wbk[si][ti % 2]
                dma(w_.ap, dap(wb[wname], base + col0 + fb * FB, [[ldw, 128], [128 * ldw, KC], [1, FB]]),
                    wdep(wname, *dep), [w_.b], qn="act")

        loadx(0)
        loads(0)
        for ti, (gi, fb) in enumerate(tasks):
            if ti + 1 < len(tasks):
                if nxg == 2:
                    loadx(ti + 1)
                loads(ti + 1)
            grows = groups[gi]
            xg_ = xg[gi % nxg]
            Tn = len(grows) * 64
            ws = [wbk[si][ti % 2] for si in range(len(wspecs))]
            for ftl in range(FB // 128):
                ft = fb * (FB // 128) + ftl
                for h0 in range(0, Tn, 512):
                    n = min(512, Tn - h0)
                    bks = []
                    for w_ in ws:
                        bk = next_bank()
                        bks.append(bk)
                        for kc in range(KC):
                            P.op("pe", lambda e, bk=bk, w_=w_, kc=kc, ftl=ftl, xg_=xg_, h0=h0, n=n: e.matmul(
                                psum[bk][:, 0:n], w_.ap[:, kc, ftl * 128:(ftl + 1) * 128], xg_.ap[:, kc, h0:h0 + n],
                                start=(kc == 0), stop=(kc == KC - 1)), [w_.b, xg_.b], [PSB[bk]])
                    epi("run", (st, ft, bks, n, grows[h0 // 64:(h0 + n) // 64]))
            if ti + 1 < len(tasks) and nxg == 1:
                loadx(ti + 1)
        P.barrier()

    def store_fm(dst_ap, dst_name, f0, stg, rows_half):
        for (r0, n, j) in runs_of(rows_half):
            dma(dap(dst_ap, f0 * NSLOT + r0 * 64, [[NSLOT, 128], [1, 64 * n]]), stg.ap[:, j * 64:(j + n) * 64],
                [stg.b], P.Ds(dst_name, range(r0, r0 + n)))

    def lin_a(groups, xt_ap, KC, wname, dep, base, ldw, col0, ncols, NB, epi, nxg=2):
        A.reset()
        TMAX = max(len(g) for g in groups) * 64
        xg = [T([128, KC, TMAX], BF16) for _ in range(nxg)]
        wbk = [T([128, KC, NB], BF16) for _ in range(2)]
        st = epi("alloc", None)
        tasks = [(gi, nb) for gi in range(len(groups)) for nb in range(ncols // NB)]
        tbase = [sum(len(g) for g in groups[:gi]) // 2 for gi in range(len(groups))]
        seq = [(ti, t) for ti, (gi, nb) in enumerate(tasks) for t in range(len(groups[gi]) // 2)]

        def loadx(ti):
            gi, nb = tasks[ti]
            if nb == 0:
                load_xg(xg[gi % nxg], xt_ap, 0, KC, groups[gi])

        def loads(ti):
            gi, nb = tasks[ti]
            w_ = wbk[ti % 2]
            dma(w_.ap, dap(wb[wname], base + col0 + nb * NB, [[ldw, 128], [128 * ldw, KC], [1, NB]]),
                wdep(wname, *dep), [w_.b], qn="act")

        def eload(k):
            if k < len(seq):
                ti, t = seq[k]
                gi, nb = tasks[ti]
                grows = groups[gi]
                epi("load", (st, nb, (grows[2 * t], grows[2 * t + 1]), k))

        loadx(0)
        loads(0)
        eload(0)
        eload(1)
        k = 0
        for ti, (gi, nb) in enumerate(tasks):
            if ti + 1 < len(tasks):
                if nxg == 2:
                    loadx(ti + 1)
                loads(ti + 1)
            grows = groups[gi]
            xg_ = xg[gi % nxg]
            w_ = wbk[ti % 2]
            for t in range(len(grows) // 2):
                eload(k + 2)
                bk = next_bank()
                for kc in range(KC):
                    P.op("pe", lambda e, bk=bk, w_=w_, kc=kc, xg_=xg_, t=t: e.matmul(
                        psum[bk][:, 0:NB], xg_.ap[:, kc, t * 128:(t + 1) * 128], w_.ap[:, kc, :],
                        start=(kc == 0), stop=(kc == KC - 1)), [w_.b, xg_.b], [PSB[bk]])
                epi("run", (st, nb, bk, (grows[2 * t], grows[2 * t + 1]), tbase[gi] + t, k))
                k += 1
            if ti + 1 < len(tasks) and nxg == 1:
                loadx(ti + 1)
        P.barrier()

    def epi_qk(mode, a):
        if mode == "alloc":
            return [T([128, 512], BF16) for _ in range(3)], [0]
        (stgs, cnt), ft, bks, n, rows_half = a
        stg = stgs[cnt[0] % 3]
        cnt[0] += 1
        sc = 0.125 if ft < 8 else 1.0
        if cnt[0] % 2:
            P.op("act", lambda e: e.activation(out=stg.ap[:, 0:n], in_=psum[bks[0]][:, 0:n], func=AF.Copy, scale=sc),
                 [PSB[bks[0]]], [stg.b])
        else:
            P.op("dve", lambda e: e.tensor_scalar(out=stg.ap[:, 0:n], in0=psum[bks[0]][:, 0:n], scalar1=sc, scalar2=None,
                                                  op0=ALU.mult), [PSB[bks[0]]], [stg.b])
        store_fm(QKT, "QKT", ft * 128, stg, rows_half)

    def epi_u(mode, a):
        if mode == "alloc":
            return [T([128, 512], F32) for _ in range(3)], [0]
        (stgs, cnt), ft, bks, n, rows_half = a
        stg = stgs[cnt[0] % 3]
        cnt[0] += 1
        if cnt[0] % 2:
            P.op("act", lambda e: e.activation(out=stg.ap[:, 0:n], in_=psum[bks[0]][:, 0:n], func=AF.Copy),
                 [PSB[bks[0]]], [stg.b])
        else:
            P.op("dve", lambda e: e.tensor_copy(out=stg.ap[:, 0:n], in_=psum[bks[0]][:, 0:n]), [PSB[bks[0]]], [stg.b])
        store_fm(UT, "UT", ft * 128, stg, rows_half)

    def epi_gu(mode, a):
        if mode == "alloc":
            return [T([128, 512], BF16) for _ in range(3)], [T([128, 512], F32) for _ in range(2)], [0]
        (stgs, tmps, cnt), ft, bks, n, rows_half = a
        stg = stgs[cnt[0] % 3]
        tmp = tmps[cnt[0] % 2]
        cnt[0] += 1
        P.op("act", lambda e: e.activation(out=tmp.ap[:, 0:n], in_=psum[bks[0]][:, 0:n], func=AF.Silu),
             [PSB[bks[0]]], [tmp.b])
        P.op("dve", lambda e: e.tensor_tensor(out=stg.ap[:, 0:n], in0=psum[bks[1]][:, 0:n], in1=tmp.ap[:, 0:n], op=ALU.mult),
             [PSB[bks[1]], tmp.b], [stg.b])
        store_fm(ACTT, "ACTT", ft * 128, stg, rows_half)

    def epi_v(mode, a):
        if mode == "alloc":
            vs = [T([128, 8, 65], BF16) for _ in range(3)]
            for v_ in vs:
                P.op("dve", lambda e, v_=v_: e.memset(v_.ap, 1.0), [], [v_.b])
            return vs, [0]
        if mode == "load":
            return
        (vs, cnt), nb, bk, (ra, rb), tix, kk = a
        v_ = vs[cnt[0] % 3]
        cnt[0] += 1
        src = psum[bk][:, :].rearrange("p (a b) -> p a b", b=64)
        if cnt[0] % 2:
            P.op("act", lambda e: e.activation(out=v_.ap[:, :, 0:64], in_=src, func=AF.Copy), [PSB[bk]], [v_.b])
        else:
            P.op("dve", lambda e: e.tensor_copy(out=v_.ap[:, :, 0:64], in_=src), [PSB[bk]], [v_.b])
        for j, r in enumerate((ra, rb)):
            dma(VA[r * 64:(r + 1) * 64, nb * 520:(nb + 1) * 520],
                v_.ap[j * 64:(j + 1) * 64, :, :].rearrange("p a b -> p (a b)"), [v_.b], [P.D("VA", r)])

    def make_epi_res(src, src_name, NB, comb_e=None, tile_base=0):
        def epi(mode, a):
            if mode == "alloc":
                return [T([128, NB], F32) for _ in range(4)], [T([128, NB], F32) for _ in range(3)]
            if mode == "load":
                (hr, os_), nb, (ra, rb), kk = a
                h_ = hr[kk % 4]
                for j, r in enumerate((ra, rb)):
                    dma(h_.ap[j * 64:(j + 1) * 64, :], src[r * 64:(r + 1) * 64, nb * NB:(nb + 1) * NB],
                        [P.D(src_name, (r, nb) if src_name == "H" else r)], [h_.b])
                return
            (hr, os_), nb, bk, (ra, rb), tix, kk = a
            h_ = hr[kk % 4]
            o_ = os_[kk % 3]
            if comb_e is None:
                P.op("dve", lambda e: e.tensor_tensor(out=o_.ap, in0=psum[bk][:, 0:NB], in1=h_.ap, op=ALU.add),
                     [PSB[bk], h_.b], [o_.b])
            else:
                P.op("dve", lambda e: e.scalar_tensor_tensor(out=o_.ap, in0=psum[bk][:, 0:NB],
                                                             scalar=COMB.ap[:, tix, comb_e:comb_e + 1], in1=h_.ap,
                                                             op0=ALU.mult, op1=ALU.add), [PSB[bk], h_.b, COMB.b], [o_.b])
            for j, r in enumerate((ra, rb)):
                dma(H[r * 64:(r + 1) * 64, nb * NB:(nb + 1) * NB], o_.ap[j * 64:(j + 1) * 64, :], [o_.b],
                    [P.D("H", (r, nb))])
        return epi

    def Hrow_bufs(r, nblk):
        return [P.D("H", (r, k)) for k in range(nblk)]

    def phase_att(layer):
        A.reset()
        Tb = T([128, 16, 14, 64], BF16)
        MB = T([128, 16, 64], BF16)
        P.op("dve", lambda e: e.memset(MB.ap, 0.0), [], [MB.b])
        zt = T([1, 128], F32)
        P.op("dve", lambda e: e.memset(zt.ap, 0.0), [], [zt.b])
        dma(RPAD[0:1, 0:64], zt.ap[0:1, 0:64], [zt.b], [P.D("RPAD")])
        dma(RPAD[0:1, 64 + 7440:64 + 7440 + 128], zt.ap[0:1, 0:128], [zt.b], [P.D("RPAD")])
        dma(RPAD[0:1, 64:64 + 7440], rpb[layer:layer + 1, :], [], [P.D("RPAD")])
        mbt = T([16, 16], F32)
        dma(mbt.ap, mbT[layer, :, :], [], [mbt.b])
        P.op("dve", lambda e: e.tensor_copy(out=MB.ap[0:16, :, :], in_=mbt.ap.unsqueeze(2).to_broadcast([16, 16, 64])), [mbt.b], [MB.b])
        mark = A.off
        hk = [T([128, 4, 14, 64], F32) for _ in range(2)]
        bd = [T([128, 4, 14, 128], BF16) for _ in range(2)]
        for b_ in bd:
            P.op("dve", lambda e, b_=b_: e.memset(b_.ap, 0.0), [], [b_.b])
        for hg in range(4):
            hk_, bd_ = hk[hg % 2], bd[hg % 2]
            for kh in range(2):
                for hl in range(4):
                    dma(hk_.ap[kh * 64:(kh + 1) * 64, hl, :, :],
                        dap(RPAD, 64 + (hg * 4 + hl) * 465 + kh * 31 - 48, [[1, 64], [31, 14], [1, 64]]),
                        [P.D("RPAD")], [hk_.b])
                P.op("dve", lambda e, kh=kh, hk_=hk_, bd_=bd_: e.tensor_copy(
                    out=bd_.ap[kh * 64:(kh + 1) * 64, :, :, kh * 64:(kh + 1) * 64], in_=hk_.ap[kh * 64:(kh + 1) * 64, :, :, :]),
                    [hk_.b], [bd_.b])
            for hl in range(4):
                for e0 in (0, 7):
                    bk = next_bank()
                    for ei in range(7):
                        P.op("pe", lambda e, bk=bk, hl=hl, ee=e0 + ei, ei=ei, bd_=bd_: e.matmul(
                            psum[bk][:, ei * 64:(ei + 1) * 64], bd_.ap[:, hl, ee, :], jj.ap, start=True, stop=True),
                            [bd_.b, jj.b], [PSB[bk]])
                    P.op("dve", lambda e, bk=bk, h=hg * 4 + hl, e0=e0: e.tensor_tensor(
                        out=Tb.ap[:, h, e0:e0 + 7, :], in0=psum[bk][:, 0:448].rearrange("p (a b) -> p a b", b=64),
                        in1=colmask.ap.unsqueeze(1).to_broadcast([128, 7, 64]), op=ALU.add),
                        [PSB[bk], colmask.b], [Tb.b])
        P.barrier()
        A.off = mark
        NKMAX = 12 * 64
        qt = [T([128, 8, 64], BF16) for _ in range(2)]
        qa = [[T([128, 8, 64], BF16) for _ in range(2)] for _ in range(2)]
        ktw = [T([128, 8, NKMAX], BF16) for _ in range(2)]
        vw = [T([128, 6, 1040], BF16) for _ in range(2)]
        ktm = {r: T([128, 8, 128], BF16) for r in (0, ROW_PM)}
        vm = {r: T([128, 1040], BF16) for r in (0, ROW_PM)}
        pt = [T([128, 512], BF16) for _ in range(8)]
        rec = [T([64, 8], F32) for _ in range(2)]
        oa = [T([64, 16, 64], F32) for _ in range(2)]
        for p_ in pt:
            P.op("dve", lambda e, p_=p_: e.memset(p_.ap, 0.0), [], [p_.b])
        for i_ in range(2):
            for par in range(2):
                q_ = qa[par][i_]
                P.op("dve", lambda e, q_=q_: e.memset(q_.ap, 0.0), [], [q_.b])
        for r in (0, ROW_PM):
            P.op("dve", lambda e, r=r: e.memset(ktm[r].ap, 0.0), [], [ktm[r].b])
            P.op("dve", lambda e, r=r: e.memset(vm[r].ap, 0.0), [], [vm[r].b])
            dma(ktm[r].ap[:, :, 0:16], dap(QKT, 1024 * NSLOT + r * 64 + 48, [[NSLOT, 128], [128 * NSLOT, 8], [1, 16]]),
                [P.D("QKT", r)], [ktm[r].b])
            dma(vm[r].ap[0:16, :], VA[r * 64 + 48:r * 64 + 64, :], [P.D("VA", r)], [vm[r].b])
        sb_rr = [0]
        pt_rr = [0]
        ob_rr = [0]
        SBANKS = (0, 1, 2, 3)
        OBANKS = ((4, 5), (6, 7))
        plan_l = [x for x in PLAN if x[0] == layer]

        def ld(it):
            (ly, row, st, nb, mrow, mcol) = plan_l[it]
            p = it % 2
            qt_, kt_, vw_ = qt[p], ktw[p], vw[p]
            dma(qt_.ap, dap(QKT, row * 64, [[NSLOT, 128], [128 * NSLOT, 8], [1, 64]]), [P.D("QKT", row)], [qt_.b])
            if nb > 0:
                dma(kt_.ap[:, :, 0:nb * 128], dap(QKT, 1024 * NSLOT + st * 64, [[NSLOT, 128], [128 * NSLOT, 8], [1, nb * 128]]),
                    P.Ds("QKT", range(st, st + 2 * nb)), [kt_.b])
                dma(vw_.ap[:, 0:nb, :], dap(VA, st * 64 * 1040, [[1040, 128], [128 * 1040, nb], [1, 1040]]),
                    P.Ds("VA", range(st, st + 2 * nb)), [vw_.b])

        ld(0)
        for it, (ly, row, st, nb, mrow, mcol) in enumerate(plan_l):
            p = it % 2
            qt_, kt_, vw_, oa_ = qt[p], ktw[p], vw[p], oa[p]
            if it + 1 < len(plan_l):
                ld(it + 1)
            for par in range(2):
                q_ = qa[par][p]
                P.op("dve", lambda e, q_=q_, qt_=qt_, par=par: e.tensor_copy(
                    out=q_.ap[par * 64:(par + 1) * 64, :, :], in_=qt_.ap[par * 64:(par + 1) * 64, :, :]), [qt_.b], [q_.b])
            for par in range(2):
                q_ = qa[par][p]
                pts = []
                for b in range(nb + 1):
                    ismeta = (b == nb)
                    bk = SBANKS[sb_rr[0] % 4]
                    sb_rr[0] += 1
                    p_ = pt[pt_rr[0] % 8]
                    pt_rr[0] += 1
                    pts.append(p_)
                    if ismeta:
                        P.op("pe", lambda e, bk=bk, par=par: e.matmul(
                            psum[bk][0:16, :], ident.ap[:, 0:16], MB.ap[:, par::2, :], start=True, stop=False),
                            [ident.b, MB.b], [PSB[bk]])
                        for hp in range(8):
                            P.op("pe", lambda e, bk=bk, hp=hp, q_=q_, mrow=mrow: e.matmul(
                                psum[bk][0:16, hp * 64:(hp + 1) * 64], ktm[mrow].ap[:, hp, 0:16], q_.ap[:, hp, :],
                                start=False, stop=(hp == 7)), [ktm[mrow].b, q_.b], [PSB[bk]])
                        P.op("act", lambda e, bk=bk, p_=p_: e.activation(
                            out=p_.ap[0:16, :], in_=psum[bk][0:16, :], func=AF.Exp, bias=zero_c.ap[0:16, :], scale=1.0),
                            [PSB[bk], zero_c.b], [p_.b])
                    else:
                        e_idx = (st + 2 * b) - row + 7
                        P.op("pe", lambda e, bk=bk, par=par, e_idx=e_idx: e.matmul(
                            psum[bk][:, :], ident.ap, Tb.ap[:, par::2, e_idx, :], start=True, stop=False),
                            [ident.b, Tb.b], [PSB[bk]])
                        for hp in range(8):
                            P.op("pe", lambda e, bk=bk, hp=hp, q_=q_, kt_=kt_, b=b: e.matmul(
                                psum[bk][:, hp * 64:(hp + 1) * 64], kt_.ap[:, hp, b * 128:(b + 1) * 128], q_.ap[:, hp, :],
                                start=False, stop=(hp == 7)), [kt_.b, q_.b], [PSB[bk]])
                        P.op("act", lambda e, bk=bk, p_=p_, c=mcol + b: e.activation(
                            out=p_.ap, in_=psum[bk][:, :], func=AF.Exp, bias=rowmask.ap[:, c:c + 1], scale=1.0),
                            [PSB[bk], rowmask.b], [p_.b])
                ob = OBANKS[ob_rr[0] % 2]
                ob_rr[0] += 1
                for hp in range(8):
                    h = 2 * hp + par
                    obk = ob[hp // 4]
                    for b in range(nb + 1):
                        ismeta = (b == nb)
                        p_ = pts[b]
                        if ismeta:
                            P.op("pe", lambda e, obk=obk, hp=hp, h=h, p_=p_, b=b, mrow=mrow: e.matmul(
                                psum[obk][0:64, (hp % 4) * 65:(hp % 4 + 1) * 65], p_.ap[:, hp * 64:(hp + 1) * 64],
                                vm[mrow].ap[:, h * 65:(h + 1) * 65], start=(b == 0), stop=True),
                                [p_.b, vm[mrow].b], [PSB[obk]])
                        else:
                            P.op("pe", lambda e, obk=obk, hp=hp, h=h, p_=p_, b=b, vw_=vw_: e.matmul(
                                psum[obk][0:64, (hp % 4) * 65:(hp % 4 + 1) * 65], p_.ap[:, hp * 64:(hp + 1) * 64],
                                vw_.ap[:, b, h * 65:(h + 1) * 65], start=(b == 0), stop=False),
                                [p_.b, vw_.b], [PSB[obk]])
                rc = rec[par]
                for half in range(2):
                    obk = ob[half]
                    ov = psum[obk][0:64, 0:260].rearrange("p (a b) -> p a b", b=65)
                    P.op("dve", lambda e, ov=ov, rc=rc, half=half: e.reciprocal(
                        out=rc.ap[:, half * 4:(half + 1) * 4].unsqueeze(2), in_=ov[:, :, 64:65]), [PSB[obk]], [rc.b])
                    h0 = par + 8 * half
                    P.op("dve", lambda e, ov=ov, rc=rc, half=half, oa_=oa_, h0=h0: e.tensor_tensor(
                        out=oa_.ap[:, h0:h0 + 7:2, :], in0=ov[:, :, 0:64],
                        in1=rc.ap[:, half * 4:(half + 1) * 4].unsqueeze(2).to_broadcast([64, 4, 64]), op=ALU.mult),
                        [PSB[obk], rc.b], [oa_.b])
            dma(OA[row * 64:(row + 1) * 64, :], oa_.ap.rearrange("p a b -> p (a b)"), [oa_.b], [P.D("OA", row)])
        P.barrier()

    def phase_pool():
        A.reset()
        W_ = NSLOT + 32
        vt = T([128, NSLOT], F32)
        dma(vt.ap, t_valid.to_broadcast([128, NSLOT]), [], [vt.b])
        ic = T([128, NSLOT], F32)
        ut = [T([128, W_], F32) for _ in range(2)]
        sa = T([128, W_], F32)
        sb = T([128, W_], F32)
        plt = [T([128, NSLOT], BF16) for _ in range(2)]
        for t_ in ut + [sa, sb]:
            P.op("dve", lambda e, t_=t_: e.memset(t_.ap, 0.0), [], [t_.b])
        allrows = range(NR)
        for c in range(8):
            g = c // 2
            u_ = ut[c % 2]
            pl_ = plt[c % 2]
            if c % 2 == 0:
                dma(ic.ap, t_invcnt[g:g + 1, :].to_broadcast([128, NSLOT]), [], [ic.b])
            dma(u_.ap[:, 16:16 + NSLOT], UT[c * 128:(c + 1) * 128, :], P.Ds("UT", allrows), [u_.b])
            P.op("dve", lambda e, u_=u_: e.tensor_tensor(out=sa.ap[:, 16:16 + NSLOT], in0=u_.ap[:, 16:16 + NSLOT], in1=vt.ap,
                                                         op=ALU.mult), [u_.b, vt.b], [sa.b])
            lo, hi = 8, W_ - 8
            P.op("dve", lambda e: e.tensor_tensor(out=sb.ap[:, lo:hi], in0=sa.ap[:, lo - 1:hi - 1], in1=sa.ap[:, lo:hi], op=ALU.add),
                 [sa.b], [sb.b])
            cur, oth = sb, sa
            for k in range(g):
                sh = 1 << k
                P.op("dve", lambda e, cur=cur, oth=oth, sh=sh: e.tensor_tensor(
                    out=oth.ap[:, lo:hi], in0=cur.ap[:, lo - sh:hi - sh], in1=cur.ap[:, lo + sh:hi + sh], op=ALU.add),
                    [cur.b], [oth.b])
                cur, oth = oth, cur
            P.op("dve", lambda e, cur=cur, oth=oth: e.tensor_tensor(out=oth.ap[:, 16:16 + NSLOT], in0=cur.ap[:, 16:16 + NSLOT],
                                                                    in1=ic.ap, op=ALU.mult), [cur.b, ic.b], [oth.b])
            P.op("dve", lambda e, oth=oth, u_=u_, pl_=pl_: e.tensor_tensor(
                out=pl_.ap, in0=oth.ap[:, 16:16 + NSLOT], in1=u_.ap[:, 16:16 + NSLOT], op=ALU.subtract),
                [oth.b, u_.b], [pl_.b])
            dma(PLT[c * 128:(c + 1) * 128, :], pl_.ap, [pl_.b], P.Ds("PLT", allrows))
        P.barrier()

    def phase_mo(layer, rows):
        A.reset()
        ga = T([128, 1024], F32); gp = T([128, 1024], F32); psc = T([128, 1024], F32)
        dma(ga.ap, g_attn[layer:layer + 1, :].to_broadcast([128, 1024]), [], [ga.b])
        dma(gp.ap, g_pool[layer:layer + 1, :].to_broadcast([128, 1024]), [], [gp.b])
        dma(psc.ap, pscale[layer:layer + 1, :].to_broadcast([128, 1024]), [], [psc.b])
        pw = T([128, 4, 2, 256], BF16)
        dma(pw.ap, dap(wb["pool_w"], layer * 4 * 65536, [[256, 128], [65536, 4], [128 * 256, 2], [1, 256]]),
            wdep("pool_w", layer * 128, layer * 128 + 128), [pw.b])
        oat = [T([128, 1024], F32) for _ in range(2)]
        plt = [T([128, 8, 128], BF16) for _ in range(2)]
        mx = [T([128, 1024], F32) for _ in range(2)]
        on = [T([128, D], BF16) for _ in range(2)]
        ot = [T([128, 16, 128], BF16) for _ in range(2)]
        junk = T([128, 1024], BF16)
        ss = [T([128, 2], F32) for _ in range(2)]
        rs = [T([128, 2], F32) for _ in range(2)]
        tiles = [(rows[i], rows[i + 1]) for i in range(0, len(rows), 2)]
        def ld(ti):
            oa_, pl_ = oat[ti % 2], plt[ti % 2]
            for j, r in enumerate(tiles[ti]):
                dma(oa_.ap[j * 64:(j + 1) * 64, :], OA[r * 64:(r + 1) * 64, :], [P.D("OA", r)], [oa_.b])
                dma(pl_.ap[:, :, j * 64:(j + 1) * 64], dap(PLT, r * 64, [[NSLOT, 128], [128 * NSLOT, 8], [1, 64]]),
                    [P.D("PLT", r)], [pl_.b])

        ld(0)
        for ti, (ra, rb) in enumerate(tiles):
            p = ti % 2
            oa_, pl_, mx_, on_, ot_, ss_, rs_ = oat[p], plt[p], mx[p], on[p], ot[p], ss[p], rs[p]
            if ti + 1 < len(tiles):
                ld(ti + 1)
            bk2 = [next_bank(), next_bank()]
            for g in range(4):
                bk = bk2[g // 2]
                for cc in range(2):
                    P.op("pe", lambda e, bk=bk, g=g, cc=cc, pl_=pl_: e.matmul(
                        psum[bk][:, (g % 2) * 256:(g % 2 + 1) * 256], pl_.ap[:, 2 * g + cc, :], pw.ap[:, g, cc, :],
                        start=(cc == 0), stop=(cc == 1)), [pl_.b, pw.b], [PSB[bk]])
            for hf in range(2):
                P.op("dve", lambda e, hf=hf, mx_=mx_, bk=bk2[hf]: e.tensor_tensor(
                    out=mx_.ap[:, hf * 512:(hf + 1) * 512], in0=psum[bk][:, :], in1=psc.ap[:, hf * 512:(hf + 1) * 512], op=ALU.mult),
                    [PSB[bk2[hf]], psc.b], [mx_.b])
            P.op("act", lambda e, oa_=oa_, ss_=ss_: e.activation(out=junk.ap, in_=oa_.ap, func=AF.Square, accum_out=ss_.ap[:, 0:1]),
                 [oa_.b], [junk.b, ss_.b])
            P.op("act", lambda e, mx_=mx_, ss_=ss_: e.activation(out=junk.ap, in_=mx_.ap, func=AF.Square, accum_out=ss_.ap[:, 1:2]),
                 [mx_.b], [junk.b, ss_.b])
            P.op("act", lambda e, ss_=ss_, rs_=rs_: e.activation(out=rs_.ap, in_=ss_.ap, func=AF.Sqrt, bias=eps_c.ap,
                                                                 scale=1.0 / 1024), [ss_.b, eps_c.b], [rs_.b])
            P.op("dve", lambda e, rs_=rs_: e.reciprocal(out=rs_.ap, in_=rs_.ap), [rs_.b], [rs_.b])
            P.op("dve", lambda e, oa_=oa_, rs_=rs_, on_=on_: e.scalar_tensor_tensor(
                out=on_.ap[:, 0:1024], in0=oa_.ap, scalar=rs_.ap[:, 0:1], in1=ga.ap, op0=ALU.mult, op1=ALU.mult),
                [oa_.b, rs_.b, ga.b], [on_.b])
            P.op("dve", lambda e, mx_=mx_, rs_=rs_, on_=on_: e.scalar_tensor_tensor(
                out=on_.ap[:, 1024:2048], in0=mx_.ap, scalar=rs_.ap[:, 1:2], in1=gp.ap, op0=ALU.mult, op1=ALU.mult),
                [mx_.b, rs_.b, gp.b], [on_.b])
            bks = [next_bank(), next_bank()]
            for kc in range(16):
                bk = bks[kc // 8]
                P.op("pe", lambda e, on_=on_, kc=kc, bk=bk: e.transpose(
                    out=psum[bk][:, :].bitcast(BF16)[:, (kc % 8) * 128:(kc % 8 + 1) * 128],
                    in_=on_.ap[:, kc * 128:(kc + 1) * 128], identity=ident.ap), [on_.b, ident.b], [PSB[bk]])
            P.op("act", lambda e, ot_=ot_, bk=bks[0]: e.activation(
                out=ot_.ap[:, 0:8, :], in_=psum[bk][:, :].bitcast(BF16).rearrange("p (a b) -> p a b", b=128), func=AF.Copy),
                [PSB[bks[0]]], [ot_.b])
            P.op("dve", lambda e, ot_=ot_, bk=bks[1]: e.tensor_copy(
                out=ot_.ap[:, 8:16, :], in_=psum[bk][:, :].bitcast(BF16).rearrange("p (a b) -> p a b", b=128)),
                [PSB[bks[1]]], [ot_.b])
            for j, r in enumerate((ra, rb)):
                dma(dap(OT, r * 64, [[NSLOT, 128], [128 * NSLOT, 16], [1, 64]]), ot_.ap[:, :, j * 64:(j + 1) * 64],
                    [ot_.b], [P.D("OT", r)])
        P.barrier()

    def phase_final():
        A.reset()
        gt = T([128, D], F32)
        dma(gt.ap, g_final.to_broadcast([128, D]), [], [gt.b])
        hb = [T([128, D], F32) for _ in range(2)]
        ob = [T([128, D], F32) for _ in range(2)]
        junk = T([128, D], BF16)
        ss = [T([128, 1], F32) for _ in range(2)]
        rs = [T([128, 1], F32) for _ in range(2)]
        tiles = [(L1_OUT[i], L1_OUT[i + 1]) for i in range(0, len(L1_OUT), 2)]
        def ld(ti):
            h_ = hb[ti % 2]
            for j, r in enumerate(tiles[ti]):
                dma(h_.ap[j * 64:(j + 1) * 64, :], H[r * 64:(r + 1) * 64, :], [P.D("H", r)], [h_.b])

        ld(0)
        for ti, (ra, rb) in enumerate(tiles):
            p = ti % 2
            h_, o_, ss_, rs_ = hb[p], ob[p], ss[p], rs[p]
            if ti + 1 < len(tiles):
                ld(ti + 1)
            P.op("act", lambda e, h_=h_, ss_=ss_: e.activation(out=junk.ap, in_=h_.ap, func=AF.Square, accum_out=ss_.ap),
                 [h_.b], [junk.b, ss_.b])
            P.op("act", lambda e, ss_=ss_, rs_=rs_: e.activation(out=rs_.ap, in_=ss_.ap, func=AF.Sqrt, bias=eps_c.ap,
                                                                 scale=1.0 / D), [ss_.b, eps_c.b], [rs_.b])
            P.op("dve", lambda e, rs_=rs_: e.reciprocal(out=rs_.ap, in_=rs_.ap), [rs_.b], [rs_.b])
            P.op("dve", lambda e, h_=h_, rs_=rs_, o_=o_: e.scalar_tensor_tensor(
                out=o_.ap, in0=h_.ap, scalar=rs_.ap, in1=gt.ap, op0=ALU.mult, op1=ALU.mult), [h_.b, rs_.b, gt.b], [o_.b])
            for j, r in enumerate((ra, rb)):
                if r <= 32:
                    dst = ys[(r - 1) * 64:r * 64, :]
                else:
                    dst = yp[(r - 43) * 64:(r - 42) * 64, :]
                dma(dst, o_.ap[j * 64:(j + 1) * 64, :], [o_.b], [])
        P.barrier()

    def mixer(layer, kv_rows, out_rows, src, src_name):
        WB = layer * D * 4096
        dep_in = (layer * WIN_R, (layer + 1) * WIN_R)
        phase_nt(kv_rows, src, src_name, ln_mix[layer:layer + 1, :])
        if stop_here(f"nt{layer}"): return True
        gk = groups_of(kv_rows)
        lin_b(gk, HNT, 16, [("w_in", dep_in, WB, 4096, 0)], 2048, epi_qk)
        lin_a(gk, HNT, 16, "w_in", dep_in, WB, 4096, 2048, 1024, 512, epi_v)
        lin_b(gk, HNT, 16, [("w_in", dep_in, WB, 4096, 3072)], 1024, epi_u)
        if stop_here(f"qkvu{layer}"): return True
        phase_att(layer)
        if stop_here(f"att{layer}"): return True
        phase_pool()
        if stop_here(f"pool{layer}"): return True
        orows = out_rows if len(out_rows) % 2 == 0 else out_rows + [65]
        phase_mo(layer, orows)
        if stop_here(f"mo{layer}"): return True
        lin_a(groups_of(orows), OT, 16, "w_out", (layer * D, (layer + 1) * D), layer * D * D, D, 0, D, 512,
              make_epi_res(src, src_name, 512))
        if stop_here(f"wout{layer}"): return True
        return False

    def run_all():
        cast_set([("w_in", 0, WIN_R), ("pool_w", 0, 128), ("w_out", 0, D)])
        c0 = pe_cnt()
        if mixer(0, L0_ALL, L0_OUT, xs, "xs"): return
        c1 = pe_cnt()
        cast_set([("ffn_g", 0, DFF), ("ffn_u", 0, DFF), ("ffn_d", 0, DFF)], (c0, c1))
        phase_nt(L0_OUT, H, "H4", ln_ffn[0:1, :])
        g0 = groups_of(L0_OUT)
        lin_b(g0, HNT, 16, [("ffn_g", (0, DFF), 0, DFF, 0), ("ffn_u", (0, DFF), 0, DFF, 0)], DFF, epi_gu)
        lin_a(groups_of(L0_OUT, 10), ACTT, 44, "ffn_d", (0, DFF), 0, D, 0, D, 256, make_epi_res(H, "H", 256), nxg=2)
        if stop_here("ffn0"): return
        c2 = pe_cnt()
        cast_set([("w_in", WIN_R, 2 * WIN_R), ("pool_w", 128, 256), ("w_out", D, 2 * D)], (c1, c2))
        if mixer(1, L1_KV, L1_OUT, H, "H8"): return
        c3 = pe_cnt()
        cast_set(moe_items(0), (c2, c3))
        phase_nt(L1_OUT, H, "H4", ln_ffn[1:2, :], router=True)
        g1 = groups_of(L1_OUT)
        for ex_ in range(NE):
            dep = (ex_ * DFF, (ex_ + 1) * DFF)
            cs = pe_cnt()
            lin_b(g1, HNT, 16, [("moe_g", dep, ex_ * D * DFF, DFF, 0), ("moe_u", dep, ex_ * D * DFF, DFF, 0)], DFF, epi_gu)
            lin_a(groups_of(L1_OUT, 10), ACTT, 44, "moe_d", dep, ex_ * DFF * D, D, 0, D, 256,
                  make_epi_res(H, "H", 256, comb_e=ex_), nxg=2)
            ce = pe_cnt()
            if ex_ + 1 < NE:
                cast_set(moe_items(ex_ + 1), (cs, ce))
        phase_final()

    run_all()
    P.barrier()

    with nc.Block() as block:
        @block.sync
        def _(e):
            P.emit("sp", e)

        @block.gpsimd
        def _(e):
            P.emit("pool", e)

        @block.scalar
        def _(e):
            P.emit("act", e)

        @block.vector
        def _(e):
            P.emit("dve", e)

        @block.tensor
        def _(e):
            P.emit("pe", e)
    es.close()
    return nc


def make_in_maps(inp):
    f = np.float32
    xp = np.asarray(inp["x_prompt"], f)[0]
    xsm = np.asarray(inp["x_sample"], f)
    meta = np.asarray(inp["meta_tokens"], f)
    shared = {
        "w_in": np.ascontiguousarray(np.asarray(inp["w_in"], f)).reshape(-1, 2048),
        "w_out": np.ascontiguousarray(np.asarray(inp["w_out"], f)).reshape(-1, 2048),
        "pool_w": np.ascontiguousarray(np.asarray(inp["pool_w"], f)).reshape(-1, 2048),
        "ffn_g": np.ascontiguousarray(np.asarray(inp["ffn_w_gate"], f)).reshape(-1, 2048),
        "ffn_u": np.ascontiguousarray(np.asarray(inp["ffn_w_up"], f)).reshape(-1, 2048),
        "ffn_d": np.ascontiguousarray(np.asarray(inp["ffn_w_down"], f)).reshape(-1, 2048),
        "moe_g": np.ascontiguousarray(np.asarray(inp["moe_w_gate"], f)).reshape(-1, 2048),
        "moe_u": np.ascontiguousarray(np.asarray(inp["moe_w_up"], f)).reshape(-1, 2048),
        "moe_d": np.ascontiguousarray(np.asarray(inp["moe_w_down"], f)).reshape(-1, 2048),
        "rpb": np.ascontiguousarray(np.asarray(inp["rpb"], f)).reshape(2, -1),
        "mbT": np.ascontiguousarray(np.transpose(np.asarray(inp["meta_bias"], f), (0, 2, 1))),
        "ln_mix": np.asarray(inp["ln_mix"], f), "ln_ffn": np.asarray(inp["ln_ffn"], f),
        "g_attn": np.asarray(inp["g_attn_out"], f), "g_pool": np.asarray(inp["g_pool_out"], f),
        "pscale": np.asarray(inp["pool_scale"], f),
        "g_final": np.asarray(inp["g_final"], f).reshape(1, D),
        "routerT": np.ascontiguousarray(np.asarray(inp["router"], f)[0].T),
    }
    maps = []
    for c in range(8):
        x = np.zeros((NSLOT, D), f)
        x[48:64] = meta
        x[64:64 + 2048] = xsm[c]
        x[ROW_PM * 64 + 48:ROW_PM * 64 + 64] = meta
        x[34 * 64:35 * 64] = xp[0:64]
        for i in range(30):
            r = 16 * c - 8 + i
            b0 = (ROW_P0 + i) * 64
            if 0 <= r < 128:
                x[b0:b0 + 64] = xp[r * 64:(r + 1) * 64]
            elif r == -1:
                x[b0 + 48:b0 + 64] = meta
        m = dict(shared)
        m["xs"] = x
        m.update(host_tables(c))
        maps.append(m)
    return maps


_NC = None


def kernel(**inputs):
    global _NC
    if _NC is None:
        _NC = build()
    maps = make_in_maps(inputs)
    res = run_bass_kernel_spmd(_NC, maps, core_ids=list(range(8)))
    ysamp = np.stack([np.asarray(res.results[c]["ys"], np.float32) for c in range(8)], axis=0)
    yprm = np.concatenate([np.asarray(res.results[c]["yp"], np.float32) for c in range(8)], axis=0)[None]
    return (yprm, ysamp)
```
